# Optimizing a Trainium2 kernel written in Bass

```python
import jax
import jax.numpy as jnp
from jax import lax
import numpy as np

D_MODEL = 1024
BATCH = 2
SEQ = 8192
DEPTH = 1

MEM_LEN = 256
EPS = 1e-6

GLA_HEADS = 4
GLA_DK = D_MODEL // 8
GLA_DV = D_MODEL // 4
GLA_GATE_RANK = 16
GLA_GATE_TEMP = 16.0
GLA_CHUNK = 64

DSA_HEADS = 8
DSA_KV_HEADS = 2
DSA_HEAD_DIM = 128
IDX_HEADS = 8
IDX_DIM = 64
IDX_TOPK_MAX = 256
Q_BLOCK = 128

MEM_HEADS = 4
MEM_HEAD_DIM = D_MODEL // 4

N_GROUPS = 4
EXPERTS_PER_GROUP = 4
N_EXPERTS = N_GROUPS * EXPERTS_PER_GROUP
EXPERT_TOPK = 2
D_EXPERT = D_MODEL // 4

N_BRANCH = 3
IN_SPLITS = (
    GLA_HEADS * GLA_DK,
    GLA_HEADS * GLA_DK,
    GLA_HEADS * GLA_DV,
    GLA_HEADS * GLA_DV,
    GLA_GATE_RANK,
    DSA_HEADS * DSA_HEAD_DIM,
    DSA_KV_HEADS * DSA_HEAD_DIM,
    DSA_KV_HEADS * DSA_HEAD_DIM,
    IDX_HEADS * IDX_DIM,
    IDX_DIM,
    IDX_HEADS,
    MEM_HEADS * MEM_HEAD_DIM,
    N_BRANCH * D_MODEL,
)
D_IN = sum(IN_SPLITS)

kernel_name = 'hybrid_gla_dsa_memxattn_hmoe'


def _rmsnorm(x, g):
    xf = x.astype(jnp.float32)
    y = xf * lax.rsqrt(jnp.mean(xf * xf, axis=-1, keepdims=True) + EPS)
    return y.astype(x.dtype) * g


def _split_in(z):
    offs = []
    acc = 0
    for w in IN_SPLITS[:-1]:
        acc += w
        offs.append(acc)
    return jnp.split(z, offs, axis=-1)


def _gla(q, k, v, g_out, a_low, w_a2, b_a2, norm_g):
    B, T = q.shape[:2]
    C = GLA_CHUNK
    N = T // C
    shp_k = (B, N, C, GLA_HEADS, GLA_DK)
    qf = q.astype(jnp.float32).reshape(shp_k) * GLA_DK ** -0.5
    kf = k.astype(jnp.float32).reshape(shp_k)
    vf = v.astype(jnp.float32).reshape(B, N, C, GLA_HEADS, GLA_DV)
    log_a = jax.nn.log_sigmoid((a_low @ w_a2 + b_a2).astype(jnp.float32)) / GLA_GATE_TEMP
    cum = jnp.cumsum(log_a.reshape(shp_k), axis=2)
    last = cum[:, :, -1:]
    q_dec = qf * jnp.exp(cum)
    k_inv = kf * jnp.exp(-cum)
    k_end = kf * jnp.exp(last - cum)
    causal = jnp.tril(jnp.ones((C, C), dtype=bool))
    att = jnp.where(causal, jnp.einsum('bnihd,bnjhd->bnhij', q_dec, k_inv), 0.0)
    o_intra = jnp.einsum('bnhij,bnjhv->bnihv', att, vf)
    upd = jnp.einsum('bnjhd,bnjhv->nbhdv', k_end, vf)
    decay = jnp.exp(last[:, :, 0]).transpose(1, 0, 2, 3)

    def step(state, inp):
        dec, u = inp
        return dec[..., None] * state + u, state

    s0 = jnp.zeros((B, GLA_HEADS, GLA_DK, GLA_DV), jnp.float32)
    _, s_prev = lax.scan(step, s0, (decay, upd))
    o_inter = jnp.einsum('bnihd,nbhdv->bnihv', q_dec, s_prev)
    o = (o_intra + o_inter).reshape(B, T, GLA_HEADS, GLA_DV)
    o = _rmsnorm(o, norm_g.astype(jnp.float32)).reshape(B, T, GLA_HEADS * GLA_DV)
    return (o * jax.nn.silu(g_out.astype(jnp.float32))).astype(q.dtype)


def _dsa(q, k, v, q_idx, k_idx, w_idx, q_gain, k_gain, kidx_gain):
    B, T = q.shape[:2]
    topk = min(IDX_TOPK_MAX, T // 4)
    grp = DSA_HEADS // DSA_KV_HEADS
    q = _rmsnorm(q.reshape(B, T, DSA_HEADS, DSA_HEAD_DIM), q_gain) * DSA_HEAD_DIM ** -0.5
    k = _rmsnorm(k.reshape(B, T, DSA_KV_HEADS, DSA_HEAD_DIM), k_gain)
    v = v.reshape(B, T, DSA_KV_HEADS, DSA_HEAD_DIM)
    q_idx = q_idx.reshape(B, T, IDX_HEADS, IDX_DIM)
    k_idx = _rmsnorm(k_idx, kidx_gain)
    w_idx = w_idx * (IDX_HEADS ** -0.5 * IDX_DIM ** -0.5)
    key_pos = jnp.arange(T)

    def block(i):
        start = i * Q_BLOCK
        qb = lax.dynamic_slice_in_dim(q, start, Q_BLOCK, axis=1)
        qib = lax.dynamic_slice_in_dim(q_idx, start, Q_BLOCK, axis=1)
        wb = lax.dynamic_slice_in_dim(w_idx, start, Q_BLOCK, axis=1)
        qpos = start + jnp.arange(Q_BLOCK)
        rel = jax.nn.relu(jnp.einsum('bqhd,bsd->bqhs', qib, k_idx).astype(jnp.float32))
        score = jnp.einsum('bqh,bqhs->bqs', wb.astype(jnp.float32), rel)
        allowed = key_pos[None, :] <= qpos[:, None]
        score = jnp.where(allowed, score, -jnp.inf)
        _, idx = lax.top_k(score, topk)
        valid = idx <= qpos[None, :, None]
        kg = jax.vmap(lambda kk, ii: kk[ii])(k, idx)
        vg = jax.vmap(lambda vv, ii: vv[ii])(v, idx)
        qg = qb.reshape(B, Q_BLOCK, DSA_KV_HEADS, grp, DSA_HEAD_DIM)
        logits = jnp.einsum('bqgrd,bqkgd->bqgrk', qg, kg).astype(jnp.float32)
        logits = jnp.where(valid[:, :, None, None, :], logits, -jnp.inf)
        p = jax.nn.softmax(logits, axis=-1).astype(v.dtype)
        o = jnp.einsum('bqgrk,bqkgd->bqgrd', p, vg)
        return o.reshape(B, Q_BLOCK, DSA_HEADS * DSA_HEAD_DIM)

    out = lax.map(block, jnp.arange(T // Q_BLOCK))
    return out.transpose(1, 0, 2, 3).reshape(B, T, DSA_HEADS * DSA_HEAD_DIM)


def _memory_attn(q, mem_h, w_kv, q_gain, k_gain):
    B, T = q.shape[:2]
    M = mem_h.shape[1]
    q = _rmsnorm(q.reshape(B, T, MEM_HEADS, MEM_HEAD_DIM), q_gain) * MEM_HEAD_DIM ** -0.5
    kv = (mem_h @ w_kv).reshape(B, M, 2, MEM_HEADS, MEM_HEAD_DIM)
    k = _rmsnorm(kv[:, :, 0], k_gain)
    v = kv[:, :, 1]
    logits = jnp.einsum('bthd,bmhd->bhtm', q, k).astype(jnp.float32)
    p = jax.nn.softmax(logits, axis=-1).astype(v.dtype)
    o = jnp.einsum('bhtm,bmhd->bthd', p, v)
    return o.reshape(B, T, MEM_HEADS * MEM_HEAD_DIM)


def _hier_moe(h, w_r1, b_r1, w_r2, b_r2, w_gate, w_up, w_down):
    B, T, D = h.shape
    hf = h.reshape(B * T, D)
    g_logits = (hf @ w_r1 + b_r1).astype(jnp.float32)
    g_prob = jax.nn.softmax(g_logits, axis=-1)
    g_sel = jnp.argmax(g_logits, axis=-1)
    g_w = jnp.take_along_axis(g_prob, g_sel[:, None], axis=-1)
    e_logits = (hf @ w_r2 + b_r2).astype(jnp.float32).reshape(-1, N_GROUPS, EXPERTS_PER_GROUP)
    e_logits = jnp.take_along_axis(e_logits, g_sel[:, None, None], axis=1)[:, 0]
    e_prob = jax.nn.softmax(e_logits, axis=-1)
    top_p, top_i = lax.top_k(e_prob, EXPERT_TOPK)
    top_p = top_p / jnp.sum(top_p, axis=-1, keepdims=True)
    expert_id = g_sel[:, None] * EXPERTS_PER_GROUP + top_i
    combine = jnp.sum(jax.nn.one_hot(expert_id, N_EXPERTS, dtype=jnp.float32)
                      * (g_w * top_p)[..., None], axis=1)
    hid = jax.nn.silu(jnp.einsum('nd,edf->nef', hf, w_gate)) * jnp.einsum('nd,edf->nef', hf, w_up)
    hid = hid * combine[..., None].astype(hid.dtype)
    out = jnp.einsum('nef,efd->nd', hid, w_down)
    return out.reshape(B, T, D)


def setup_inputs(seed: int = 0) -> dict:
    key = jax.random.key(seed)
    ks = iter(jax.random.split(key, 40))
    L = DEPTH

    def w(shape, fan_in):
        return jax.random.normal(next(ks), shape, jnp.float32) * fan_in ** -0.5

    def gain(shape):
        return 1.0 + 0.02 * jax.random.normal(next(ks), shape, jnp.float32)

    def bias(shape, s=0.01):
        return s * jax.random.normal(next(ks), shape, jnp.float32)

    return {
        'x': jax.random.normal(next(ks), (BATCH, SEQ, D_MODEL), jnp.float32),
        'mem': jax.random.normal(next(ks), (BATCH, MEM_LEN, D_MODEL), jnp.float32),
        'g_mix': gain((L, D_MODEL)),
        'g_mem': gain((L, D_MODEL)),
        'w_in': w((L, D_MODEL, D_IN), D_MODEL),
        'w_gla_a2': w((L, GLA_GATE_RANK, GLA_HEADS * GLA_DK), GLA_GATE_RANK),
        'b_gla_a2': bias((L, GLA_HEADS * GLA_DK), 0.1),
        'gla_norm': gain((L, GLA_DV)),
        'w_mem_kv': w((L, D_MODEL, 2 * MEM_HEADS * MEM_HEAD_DIM), D_MODEL),
        'dsa_q_norm': gain((L, DSA_HEAD_DIM)),
        'dsa_k_norm': gain((L, DSA_HEAD_DIM)),
        'idx_k_norm': gain((L, IDX_DIM)),
        'mem_q_norm': gain((L, MEM_HEAD_DIM)),
        'mem_k_norm': gain((L, MEM_HEAD_DIM)),
        'b_gate': bias((L, N_BRANCH * D_MODEL), 0.1),
        'w_o_gla': w((L, GLA_HEADS * GLA_DV, D_MODEL), GLA_HEADS * GLA_DV),
        'w_o_dsa': w((L, DSA_HEADS * DSA_HEAD_DIM, D_MODEL), DSA_HEADS * DSA_HEAD_DIM),
        'w_o_mem': w((L, MEM_HEADS * MEM_HEAD_DIM, D_MODEL), MEM_HEADS * MEM_HEAD_DIM),
        'w_out': w((L, D_MODEL, D_MODEL), D_MODEL),
        'g_ffn': gain((L, D_MODEL)),
        'w_r1': w((L, D_MODEL, N_GROUPS), D_MODEL),
        'b_r1': bias((L, N_GROUPS)),
        'w_r2': w((L, D_MODEL, N_EXPERTS), D_MODEL),
        'b_r2': bias((L, N_EXPERTS)),
        'w_gate': w((L, N_EXPERTS, D_MODEL, D_EXPERT), D_MODEL),
        'w_up': w((L, N_EXPERTS, D_MODEL, D_EXPERT), D_MODEL),
        'w_down': w((L, N_EXPERTS, D_EXPERT, D_MODEL), D_EXPERT),
    }


def reference(x, mem, g_mix, g_mem, w_in, w_gla_a2, b_gla_a2, gla_norm, w_mem_kv,
              dsa_q_norm, dsa_k_norm, idx_k_norm, mem_q_norm, mem_k_norm, b_gate,
              w_o_gla, w_o_dsa, w_o_mem, w_out, g_ffn, w_r1, b_r1, w_r2, b_r2,
              w_gate, w_up, w_down):
    B, T, D = x.shape
    for l in range(DEPTH):
        h = _rmsnorm(x, g_mix[l])
        (gq, gk, gv, gg, ga, dq, dk, dv, iq, ik, iw, mq, gates) = _split_in(h @ w_in[l])
        o_gla = _gla(gq, gk, gv, gg, ga, w_gla_a2[l], b_gla_a2[l], gla_norm[l])
        o_dsa = _dsa(dq, dk, dv, iq, ik, iw, dsa_q_norm[l], dsa_k_norm[l], idx_k_norm[l])
        mem_h = _rmsnorm(mem, g_mem[l])
        o_mem = _memory_attn(mq, mem_h, w_mem_kv[l], mem_q_norm[l], mem_k_norm[l])
        gate = jax.nn.sigmoid((gates + b_gate[l]).astype(jnp.float32)).astype(x.dtype)
        gate = gate.reshape(B, T, N_BRANCH, D)
        merged = (gate[:, :, 0] * (o_gla @ w_o_gla[l])
                  + gate[:, :, 1] * (o_dsa @ w_o_dsa[l])
                  + gate[:, :, 2] * (o_mem @ w_o_mem[l]))
        x = x + merged @ w_out[l]
        x = x + _hier_moe(_rmsnorm(x, g_ffn[l]), w_r1[l], b_r1[l], w_r2[l], b_r2[l],
                          w_gate[l], w_up[l], w_down[l])
    return x
```

```python
import numpy as np
import ml_dtypes
from contextlib import ExitStack
import concourse.bass as bass
import concourse.mybir as mybir
from concourse.bass_utils import run_bass_kernel_spmd

F32 = mybir.dt.float32
BF16 = mybir.dt.bfloat16
AF = mybir.ActivationFunctionType
ALU = mybir.AluOpType
AX = mybir.AxisListType

D = 1024
D_IN = 9304
EPS = 1e-6
OFF = dict(gq=0, gk=512, gv=1024, gg=2048, ga=3072, dq=3088, dk=4112, dv=4368, iq=4624,
           ik=5136, iw=5200, mq=5208, gates=6232)
NBIS = 14
NEG = -30000.0


class T:
    def __init__(self, h):
        self.h = h
        self.w = None
        self.r = {}
        self.dsem = None
        self.dcnt = 0
        self.wx = []

    def __getitem__(self, k):
        return self.h[k]


class Rot:
    def __init__(self, tiles):
        self.t = tiles
        self.i = 0

    def next(self):
        t = self.t[self.i % len(self.t)]
        self.i += 1
        return t


class JunkRot:
    def __init__(self, tiles):
        self.t = tiles
        self.i = 0

    def advance(self):
        self.i += 1
        return self.t[self.i % len(self.t)]

    def __getitem__(self, k):
        return self.t[self.i % len(self.t)].h[k]


class KB:
    def __init__(self, nc):
        self.nc = nc
        self.eng = {'pe': nc.tensor, 'act': nc.scalar, 'dve': nc.vector, 'pool': nc.gpsimd, 'sp': nc.sync}
        self.sem = {k: nc.alloc_semaphore("s_" + k) for k in self.eng}
        self.cnt = {k: 0 for k in self.eng}
        self.waited = {k: {} for k in self.eng}
        self.dma_evs = {}
        self.nsem = 5
        self.free_sems = []
        self.free_sems_sw = []
        self.dma_tiles = []
        self.temp_recs = []
        self.nops = 0

    def _deps(self, r, w):
        deps = []
        for t in r:
            if t.w is not None:
                deps.append(t.w)
            deps.extend(t.wx)
        for t in w:
            if t.w is not None:
                deps.append(t.w)
            deps.extend(t.wx)
            deps.extend(t.r.values())
        return deps

    def _wait(self, e, deps):
        wd = self.waited[e]
        best = {}
        for (sem, sid, val, prod) in deps:
            if prod == e and e == 'pe':
                continue
            if wd.get(sid, 0) >= val:
                continue
            if sid not in best or best[sid][1] < val:
                best[sid] = (sem, val)
        for sid, (sem, val) in best.items():
            self.eng[e].wait_ge(sem, val)
            wd[sid] = val

    def op(self, e, fn, r=(), w=()):
        w = [x.advance() if isinstance(x, JunkRot) else x for x in w]
        self._wait(e, self._deps(r, w))
        ins = fn()
        self.cnt[e] += 1
        ins.then_inc(self.sem[e], 1)
        ev = (self.sem[e], e, self.cnt[e], e)
        for t in r:
            t.r[e] = ev
        for t in w:
            t.w = ev
            t.wx = []
            t.r = {}
        self.nops += 1
        return ev

    def dma(self, q, out_ap, in_ap, sb_t, load=True):
        def new_rec():
            pool_ = self.free_sems_sw if q == 'pool' else self.free_sems
            if pool_:
                return pool_.pop()
            r_ = [self.nc.alloc_semaphore("d%d" % self.nsem), "d%d" % self.nsem, 0, q]
            self.nsem += 1
            return r_

        fresh = (q == 'pool' and load and sb_t.dsem is not None and sb_t.w is not None
                 and sb_t.w[3] == 'dma' and not sb_t.r)
        if fresh:
            rec = new_rec()
            self.temp_recs.append(rec)
        else:
            deps = self._deps((), (sb_t,)) if load else self._deps((sb_t,), ())
            self._wait(q, deps)
            if sb_t.dsem is not None and sb_t.dsem[3] != q:
                self.temp_recs.append(sb_t.dsem)
                sb_t.dsem = new_rec()
            if sb_t.dsem is None:
                sb_t.dsem = new_rec()
                self.dma_tiles.append(sb_t)
            rec = sb_t.dsem
        ins = self.eng[q].dma_start(out=out_ap, in_=in_ap)
        rec[2] += 16
        ins.then_inc(rec[0], 16)
        ev = (rec[0], rec[1], rec[2], 'dma')
        if load:
            if fresh:
                sb_t.wx = sb_t.wx + [sb_t.w]
            else:
                sb_t.wx = []
            sb_t.w = ev
            sb_t.r = {}
        else:
            sb_t.r['dma%d' % rec[2]] = ev
        self.dma_evs[rec[1]] = ev
        return ev

    def barrier(self):
        self._wait('sp', list(self.dma_evs.values()))
        evs = [(self.sem[k], k, self.cnt[k], k) for k in self.eng if k != 'sp' and self.cnt[k] > 0]
        self._wait('sp', evs)
        self.eng['sp'].sem_inc(self.sem['sp'], 1)
        self.cnt['sp'] += 1
        ev = (self.sem['sp'], 'sp', self.cnt['sp'], 'sp')
        for k in self.eng:
            if k != 'sp':
                self._wait(k, [ev])
        for t in self.dma_tiles:
            self.temp_recs.append(t.dsem)
            t.dsem = None
        self.dma_tiles = []
        for rec in self.temp_recs:
            (self.free_sems_sw if rec[3] == 'pool' else self.free_sems).append(rec)
        self.temp_recs = []
        self.dma_evs = {}


def pipeline(gens, depth=2, lag=4):
    active = []
    idx = 0
    n = len(gens)
    while idx < n or active:
        if idx < n and len(active) < depth and (not active or active[-1][1] >= lag):
            active.append([gens[idx], 0])
            idx += 1
        for a in list(active):
            try:
                next(a[0])
                a[1] += 1
            except StopIteration:
                active.remove(a)


def pipeline2(items, depth, lag, ahead):
    n = len(items)
    pre_res = {}

    def ensure(k):
        if k < n and k not in pre_res:
            pre_res[k] = items[k][0]()

    active = []
    idx = 0
    while idx < n or active:
        if idx < n and len(active) < depth and (not active or active[-1][1] >= lag):
            for k in range(idx, idx + ahead + 1):
                ensure(k)
            active.append([items[idx][1](pre_res.pop(idx)), 0])
            idx += 1
        for a in list(active):
            try:
                next(a[0])
                a[1] += 1
            except StopIteration:
                active.remove(a)


def build_nc(TT, dbg=False):
    NB = TT // 128
    NOWN = NB // 4
    NTILE = NOWN // 4
    nc = bass.Bass("TRN2", target_bir_lowering=False)
    kb = KB(nc)
    V_, A_, P_, G_ = nc.vector, nc.scalar, nc.tensor, nc.gpsimd

    def din(name, shape, dt=F32):
        return nc.dram_tensor(name, list(shape), dt, kind="ExternalInput")

    x_full = din("x_full", [TT, D]).ap()
    x_own = din("x_own", [NOWN * 128, D]).ap()
    mem = din("mem", [256, D]).ap()
    cmask_d = din("cmask", [128, 512]).ap()
    wsel_d = din("wsel", [128, 4]).ap()
    w_in = din("w_in", [D, D_IN]).ap()
    w_a2 = din("w_gla_a2", [16, 512]).ap()
    w_mem_kv = din("w_mem_kv", [D, 2048]).ap()
    w_o = [din(n, [D, D]).ap() for n in ("w_o_gla", "w_o_dsa", "w_o_mem")]
    w_out = din("w_out", [D, D]).ap()
    w_r1 = din("w_r1", [D, 4]).ap()
    w_r2 = din("w_r2", [D, 16]).ap()
    w_gate = din("w_gate", [16, D, 256]).ap()
    w_up = din("w_up", [16, D, 256]).ap()
    w_down = din("w_down", [16, 256, D]).ap()
    vecs = {}
    for n, L in (("g_mix", 1024), ("g_mem", 1024), ("g_ffn", 1024), ("gla_norm", 256), ("dsa_q_norm", 128),
                 ("dsa_k_norm", 128), ("idx_k_norm", 64), ("mem_q_norm", 256), ("mem_k_norm", 256),
                 ("b_gla_a2", 512), ("b_gate", 3072), ("b_r1", 4), ("b_r2", 16)):
        vecs[n] = din(n, [1, L])
    c_ident = din("c_ident", [128, 128], BF16).ap()
    c_i4 = din("c_i4", [128, 512], BF16).ap()
    c_ones = din("c_ones", [128, 128], BF16).ap()
    c_lmt = din("c_lmt", [128, 128]).ap()
    c_umt = din("c_umt", [128, 128]).ap()
    c_caus4 = din("c_caus4", [128, 512]).ap()
    c_sel16 = din("c_sel16", [16, 2048], BF16).ap()
    c_bis = din("c_bis", [128, NBIS]).ap()
    out_own = nc.dram_tensor("out_own", [NOWN * 128, D], F32, kind="ExternalOutput").ap()
    dbgs = {}
    ogla_scr = nc.dram_tensor("ogla_scr", [NOWN, 128, 1024], BF16, kind="Internal").ap()

    def bcast(name, L):
        return bass.AP(tensor=vecs[name], offset=0, ap=[[0, 128], [1, L]])

    top = ExitStack()

    uid = [0]

    def sb(stack, name, shape, dt):
        uid[0] += 1
        return T(stack.enter_context(nc.sbuf_tensor("%s_%d" % (name, uid[0]), list(shape), dt)))

    def ps(stack, name, shape, dt=F32):
        uid[0] += 1
        return T(stack.enter_context(nc.psum_tensor("%s_%d" % (name, uid[0]), list(shape), dt)))

    def wchunks(src2d, col0, ncol):
        return src2d.rearrange("(c p) n -> p c n", p=128)[:, :, col0:col0 + ncol]

    with top:
        ident = sb(top, "ident", [128, 128], BF16)
        i4 = sb(top, "i4", [128, 512], BF16)
        ones = sb(top, "ones", [128, 128], BF16)
        wsel = sb(top, "wselt", [128, 4], F32)
        memKT = sb(top, "memKT", [128, 8, 256], BF16)
        memV = sb(top, "memV", [128, 2, 1024], BF16)

        for t_, src_ in ((ident, c_ident), (i4, c_i4), (ones, c_ones), (wsel, wsel_d)):
            kb.dma('sp', t_[:], src_, t_)

        def load_gain(t_, name, L, rep=1, scale=None):
            for i in range(rep):
                kb.dma('sp', t_[:, i * L:(i + 1) * L], bcast(name, L), t_)
            if scale is not None:
                kb.op('dve', lambda: V_.tensor_scalar(out=t_[:], in0=t_[:], scalar1=scale, scalar2=None,
                                                      op0=ALU.mult), r=[t_], w=[t_])

        def rstd_from_ss(ss, n, width, tmp):
            kb.op('act', lambda: A_.activation(out=ss, in_=ss, func=AF.Ln, bias=EPS, scale=1.0 / width), r=[tmp], w=[tmp])
            kb.op('act', lambda: A_.activation(out=ss, in_=ss, func=AF.Exp, scale=-0.5), r=[tmp], w=[tmp])

        def norm_only(xt, gain, junk, st, hb):
            kb.op('act', lambda: A_.activation(out=junk[:], in_=xt[:], func=AF.Square, accum_out=st[:, 0:1]),
                  r=[xt], w=[junk, st])
            rstd_from_ss(st[:, 0:1], 1, 1024.0, st)
            kb.op('dve', lambda: V_.scalar_tensor_tensor(out=hb[:], in0=xt[:], scalar=st[:, 0:1], in1=gain[:],
                                                         op0=ALU.mult, op1=ALU.mult), r=[xt, st, gain], w=[hb])

        def transp_T(hb, PT, hT_ap, hT_t):
            for c in range(8):
                kb.op('pe', lambda c=c: P_.transpose(out=PT[:, c, :], in_=hb[:, c * 128:(c + 1) * 128],
                                                     identity=ident[:]), r=[hb, ident], w=[PT])
            kb.op('act', lambda: A_.copy(out=hT_ap, in_=PT[:]), r=[PT], w=[hT_t])

        def norm_T(xt, gain, junk, st, hb, PT, hT_ap, hT_t):
            norm_only(xt, gain, junk, st, hb)
            transp_T(hb, PT, hT_ap, hT_t)

        def proj(hT_ap_fn, hT_t, W, col0, ncol, PJ_t, PJ_ap=None):
            o = PJ_t[:, 0:ncol] if PJ_ap is None else PJ_ap
            for c in range(8):
                kb.op('pe', lambda c=c: P_.matmul(o, lhsT=hT_ap_fn(c), rhs=W[:, c, col0:col0 + ncol],
                                                  start=(c == 0), stop=(c == 7)), r=[hT_t, W], w=[PJ_t])

        with ExitStack() as ph:
            g_mem = sb(ph, "g_memt", [128, 1024], F32)
            mkg4 = sb(ph, "mkg4", [128, 1024], F32)
            xt = sb(ph, "p0_x", [128, 1024], F32)
            junk = JunkRot([sb(ph, "p0_junk%d" % i, [128, 1024], BF16) for i in range(3)])
            st = sb(ph, "p0_st", [128, 8], F32)
            hb = sb(ph, "p0_hb", [128, 1024], BF16)
            mhT = sb(ph, "p0_mhT", [128, 8, 256], BF16)
            wkv = [sb(ph, "p0_wkv%d" % i, [128, 8, 512], BF16) for i in range(2)]
            kfs = [sb(ph, "p0_kf%d" % i, [128, 1024], F32) for i in range(2)]
            kbf = sb(ph, "p0_kbf", [128, 1024], BF16)
            PT = ps(ph, "p0_PT", [128, 8, 128], BF16)
            PJ = [ps(ph, "p0_PJ%d" % i, [128, 512]) for i in range(2)]
            kb.dma('sp', g_mem[:], bcast("g_mem", 1024), g_mem)
            for i in range(4):
                kb.dma('sp', mkg4[:, i * 256:(i + 1) * 256], bcast("mem_k_norm", 256), mkg4)
            for mb in range(2):
                kb.dma('sp', xt[:], mem[mb * 128:(mb + 1) * 128, :], xt)
                norm_T(xt, g_mem, junk, st, hb, PT, mhT[:, :, mb * 128:(mb + 1) * 128], mhT)
            wrot = Rot(wkv)
            pjrot = Rot(PJ)
            for nt in range(4):
                wt = wrot.next()
                kb.dma('pool', wt[:], wchunks(w_mem_kv, nt * 512, 512), wt)
                for mb in range(2):
                    pj = pjrot.next()
                    kf = kfs[mb]
                    proj(lambda c, mb=mb: mhT[:, c, mb * 128:(mb + 1) * 128], mhT, wt, 0, 512, pj)
                    if nt < 2:
                        kb.op('act', lambda pj=pj, nt=nt, kf=kf: A_.copy(out=kf[:, nt * 512:(nt + 1) * 512], in_=pj[:]),
                              r=[pj], w=[kf])
                        if nt == 1:
                            for h in range(4):
                                kb.op('act', lambda h=h, kf=kf: A_.activation(out=junk[:, 0:256], in_=kf[:, h * 256:(h + 1) * 256],
                                                                       func=AF.Square, accum_out=st[:, h:h + 1]),
                                      r=[kf], w=[junk, st])
                            rstd_from_ss(st[:, 0:4], 4, 256.0, st)
                            for h in range(4):
                                kb.op('dve', lambda h=h, kf=kf: V_.scalar_tensor_tensor(
                                    out=kbf[:, h * 256:(h + 1) * 256], in0=kf[:, h * 256:(h + 1) * 256],
                                    scalar=st[:, h:h + 1], in1=mkg4[:, h * 256:(h + 1) * 256],
                                    op0=ALU.mult, op1=ALU.mult), r=[kf, st, mkg4], w=[kbf])
                            for c8 in range(8):
                                kb.op('pe', lambda c8=c8: P_.transpose(out=PT[:, c8, :], in_=kbf[:, c8 * 128:(c8 + 1) * 128],
                                                                       identity=ident[:]), r=[kbf, ident], w=[PT])
                            kb.op('act', lambda mb=mb: A_.copy(out=memKT[:, :, mb * 128:(mb + 1) * 128], in_=PT[:]),
                                  r=[PT], w=[memKT])
                    else:
                        kb.op('act', lambda pj=pj, nt=nt, mb=mb: A_.copy(
                            out=memV[:, mb, (nt - 2) * 512:(nt - 1) * 512], in_=pj[:]), r=[pj], w=[memV])
            kb.barrier()


        with ExitStack() as ph:
            Wg = sb(ph, "g_W", [128, 8, 3088], BF16)
            caus4 = sb(ph, "caus4", [128, 512], F32)
            g_mix = sb(ph, "g_mixt", [128, 1024], F32)
            gnorm = sb(ph, "gnormt", [128, 256], F32)
            kb.dma('sp', caus4[:], c_caus4, caus4)
            load_gain(g_mix, "g_mix", 1024)
            load_gain(gnorm, "gla_norm", 256)
            wa2 = sb(ph, "g_wa2", [17, 512], BF16)
            lmt = sb(ph, "g_lmt", [128, 128], F32)
            umt = sb(ph, "g_umt", [128, 128], F32)
            negc = sb(ph, "g_negc", [128, 2], F32)
            S = sb(ph, "g_S", [128, 1024], F32)
            Ssel = sb(ph, "g_Ssel", [128, 1024], F32)
            Sbf = sb(ph, "g_Sbf", [128, 1024], BF16)
            xts = Rot([sb(ph, "g_x%d" % i, [128, 1024], F32) for i in range(4)])
            junk = JunkRot([sb(ph, "g_junk%d" % i, [128, 256], BF16) for i in range(3)])
            sts = Rot([sb(ph, "g_st%d" % i, [128, 8], F32) for i in range(4)])
            hbs = Rot([sb(ph, "g_hb%d" % i, [128, 1024], BF16) for i in range(4)])
            hTs = Rot([sb(ph, "g_hT%d" % i, [128, 8, 128], BF16) for i in range(4)])
            kfs = Rot([sb(ph, "g_kf%d" % i, [128, 512], F32) for i in range(4)])
            qf = sb(ph, "g_qf", [128, 512], F32)
            vbs = Rot([sb(ph, "g_vb%d" % i, [128, 1024], BF16) for i in range(4)])
            sg = sb(ph, "g_sg", [128, 1024], F32)
            aTs = Rot([sb(ph, "g_aT%d" % i, [17, 128], BF16) for i in range(4)])
            spbs = Rot([sb(ph, "g_sp%d" % i, [128, 512], F32) for i in range(4)])
            E1s = Rot([sb(ph, "g_E1%d" % i, [128, 512], F32) for i in range(4)])
            E2 = sb(ph, "g_E2", [128, 512], F32)
            decs = Rot([sb(ph, "g_dec%d" % i, [128, 4], F32) for i in range(4)])
            kends = Rot([sb(ph, "g_kend%d" % i, [128, 512], BF16) for i in range(4)])
            qdec = sb(ph, "g_qdec", [128, 512], BF16)
            qkT = sb(ph, "g_qkT", [128, 8, 128], BF16)
            attT = sb(ph, "g_attT", [128, 512], BF16)
            ogl = sb(ph, "g_ogl", [128, 1024], BF16)
            ogTs = Rot([sb(ph, "g_ogT%d" % i, [128, 8, 128], BF16) for i in range(2)])
            PJs = Rot([ps(ph, "g_PJ%d" % i, [128, 512]) for i in range(2)])
            PA = ps(ph, "g_PA", [128, 512])
            PB = ps(ph, "g_PB", [128, 512])
            PU = [ps(ph, "g_PU%d" % i, [128, 512]) for i in range(2)]
            PT = ps(ph, "g_PT", [128, 8, 128], BF16)
            PS = ps(ph, "g_PS", [128, 512])

            for i in range(7):
                c0 = i * 512
                n = min(512, 3088 - c0)
                kb.dma('pool', Wg[:, :, c0:c0 + n], wchunks(w_in, c0, n), Wg)
            kb.dma('pool', wa2[0:16, :], w_a2, wa2)
            kb.dma('pool', wa2[16:17, :], vecs["b_gla_a2"].ap(), wa2)
            kb.dma('sp', lmt[:], c_lmt, lmt)
            kb.dma('sp', umt[:], c_umt, umt)
            kb.op('dve', lambda: V_.memset(negc[:], -1.0 / 16.0), w=[negc])
            kb.op('dve', lambda: V_.memset(S[:], 0.0), w=[S])
            for aT_ in aTs.t:
                kb.op('pool', lambda aT_=aT_: G_.memset(aT_[:], 1.0), w=[aT_])

            def g_pre(xsrc):
                def f():
                    xt = xts.next(); st = sts.next(); hb = hbs.next()
                    kb.dma('sp', xt[:], xsrc, xt)
                    norm_only(xt, g_mix, hb, st, hb)
                    return (st, hb)
                return f

            def gla_block(pre, own, p, slot):
                st, hb = pre
                hT = hTs.next()
                kf = kfs.next(); vb = vbs.next(); aT = aTs.next(); spb = spbs.next(); E1 = E1s.next(); E3 = E1
                dec = decs.next(); kend = kends.next(); kinv = kend
                transp_T(hb, PT, hT[:], hT)
                yield
                hf = lambda c: hT[:, c, :]
                for c in range(8):
                    kb.op('pe', lambda c=c: P_.matmul(PS[0:16, 0:128], lhsT=Wg[:, c, OFF['ga']:OFF['ga'] + 16],
                                                      rhs=hT[:, c, :], start=(c == 0), stop=(c == 7)),
                          r=[hT, Wg], w=[PS])
                kb.op('act', lambda: A_.copy(out=aT[0:16, :], in_=PS[0:16, 0:128]), r=[PS], w=[aT])
                pj = PJs.next()
                proj(hf, hT, Wg, OFF['gk'], 512, pj)
                kb.op('dve', lambda: V_.tensor_copy(out=kf[:], in_=pj[:]), r=[pj], w=[kf])
                yield
                kb.op('pe', lambda: P_.matmul(PA[:], lhsT=aT[:], rhs=wa2[:], start=True, stop=True),
                      r=[aT, wa2], w=[PA])
                pj = PJs.next()
                proj(hf, hT, Wg, OFF['gv'], 512, pj)
                kb.op('dve', lambda pj=pj: V_.tensor_copy(out=vb[:, 0:512], in_=pj[:]), r=[pj], w=[vb])
                yield
                kb.op('act', lambda: A_.activation(out=spb[:], in_=PA[:], func=AF.Exp, scale=-1.0), r=[PA], w=[spb])
                kb.op('act', lambda: A_.activation(out=spb[:], in_=spb[:], func=AF.Ln, bias=1.0), r=[spb], w=[spb])
                pj = PJs.next()
                proj(hf, hT, Wg, OFF['gv'] + 512, 512, pj)
                kb.op('dve', lambda pj=pj: V_.tensor_copy(out=vb[:, 512:1024], in_=pj[:]), r=[pj], w=[vb])
                yield
                if own:
                    pj = PJs.next()
                    proj(hf, hT, Wg, OFF['gq'], 512, pj)
                    kb.op('act', lambda: A_.copy(out=qf[:], in_=pj[:]), r=[pj], w=[qf])

                    def g_proj(nt):
                        pj = PJs.next()
                        proj(hf, hT, Wg, OFF['gg'] + nt * 512, 512, pj)
                        kb.op('act', lambda: A_.activation(out=sg[:, nt * 512:(nt + 1) * 512], in_=pj[:], func=AF.Silu),
                              r=[pj], w=[sg])
                yield
                if not own:
                    kb.op('pe', lambda: P_.matmul(PB[:], lhsT=umt[:], rhs=spb[:], start=True, stop=True),
                          r=[umt, spb], w=[PB])
                    for h in range(4):
                        kb.op('pe', lambda h=h: P_.matmul(PS[:, 128 + 2 * h:130 + 2 * h], lhsT=spb[:, h * 128:(h + 1) * 128],
                                                          rhs=negc[:], start=True, stop=True), r=[spb, negc], w=[PS])
                    yield
                    kb.op('act', lambda: A_.activation(out=E3[:], in_=PB[:], func=AF.Exp), r=[PB], w=[E3])
                    kb.op('act', lambda: A_.activation(out=dec[:], in_=PS[:, 128:136:2], func=AF.Exp), r=[PS], w=[dec])
                    kb.op('dve', lambda: V_.tensor_tensor(out=kend[:], in0=kf[:], in1=E3[:], op=ALU.mult),
                          r=[kf, E3], w=[kend])
                    for h in range(4):
                        kb.op('pe', lambda h=h: P_.matmul(PU[h // 2][:, (h % 2) * 256:(h % 2 + 1) * 256],
                                                          lhsT=kend[:, h * 128:(h + 1) * 128],
                                                          rhs=vb[:, h * 256:(h + 1) * 256], start=True, stop=True),
                              r=[kend, vb], w=[PU[h // 2]])
                    yield
                    if p == 0:
                        kb.op('dve', lambda: V_.tensor_scalar(out=Ssel[:], in0=S[:], scalar1=wsel[:, 0:1], scalar2=None,
                                                              op0=ALU.mult), r=[S, wsel], w=[Ssel])
                    else:
                        kb.op('dve', lambda: V_.scalar_tensor_tensor(out=Ssel[:], in0=S[:], scalar=wsel[:, p:p + 1],
                                                                     in1=Ssel[:], op0=ALU.mult, op1=ALU.add),
                              r=[S, wsel, Ssel], w=[Ssel])
                    for h in range(4):
                        kb.op('dve', lambda h=h: V_.scalar_tensor_tensor(
                            out=S[:, h * 256:(h + 1) * 256], in0=S[:, h * 256:(h + 1) * 256], scalar=dec[:, h:h + 1],
                            in1=PU[h // 2][:, (h % 2) * 256:(h % 2 + 1) * 256], op0=ALU.mult, op1=ALU.add),
                            r=[S, dec, PU[h // 2]], w=[S])
                else:
                    kb.op('pe', lambda: P_.matmul(PB[:], lhsT=lmt[:], rhs=spb[:], start=True, stop=True),
                          r=[lmt, spb], w=[PB])
                    yield
                    kb.op('act', lambda: A_.activation(out=E1[:], in_=PB[:], func=AF.Exp), r=[PB], w=[E1])
                    kb.op('act', lambda: A_.activation(out=E2[:], in_=PB[:], func=AF.Exp, scale=-1.0), r=[PB], w=[E2])
                    g_proj(0)
                    kb.op('dve', lambda: V_.scalar_tensor_tensor(out=qdec[:], in0=qf[:], scalar=128.0 ** -0.5, in1=E1[:],
                                                                 op0=ALU.mult, op1=ALU.mult), r=[qf, E1], w=[qdec])
                    kb.op('dve', lambda: V_.tensor_tensor(out=kinv[:], in0=kf[:], in1=E2[:], op=ALU.mult),
                          r=[kf, E2], w=[kinv])
                    g_proj(1)
                    for h in range(4):
                        kb.op('pe', lambda h=h: P_.transpose(out=PT[:, h, :], in_=qdec[:, h * 128:(h + 1) * 128],
                                                             identity=ident[:]), r=[qdec, ident], w=[PT])
                        kb.op('pe', lambda h=h: P_.transpose(out=PT[:, 4 + h, :], in_=kinv[:, h * 128:(h + 1) * 128],
                                                             identity=ident[:]), r=[kinv, ident], w=[PT])
                    yield
                    kb.op('act', lambda: A_.copy(out=qkT[:], in_=PT[:]), r=[PT], w=[qkT])
                    for h in range(4):
                        kb.op('pe', lambda h=h: P_.matmul(PB[:, h * 128:(h + 1) * 128], lhsT=qkT[:, 4 + h, :],
                                                          rhs=qkT[:, h, :], start=True, stop=True), r=[qkT], w=[PB])
                    kb.op('dve', lambda: V_.tensor_tensor(out=attT[:], in0=PB[:], in1=caus4[:], op=ALU.mult),
                          r=[PB, caus4], w=[attT])
                    yield
                    kb.op('pool', lambda: G_.tensor_copy(out=Sbf[:], in_=Ssel[:]), r=[Ssel], w=[Sbf])
                    for h in range(4):
                        o_ap = PU[h // 2][:, (h % 2) * 256:(h % 2 + 1) * 256]
                        kb.op('pe', lambda h=h, o_ap=o_ap: P_.matmul(o_ap, lhsT=attT[:, h * 128:(h + 1) * 128],
                                                                     rhs=vb[:, h * 256:(h + 1) * 256], start=True, stop=False),
                              r=[attT, vb], w=[PU[h // 2]])
                        kb.op('pe', lambda h=h, o_ap=o_ap: P_.matmul(o_ap, lhsT=qkT[:, h, :],
                                                                     rhs=Sbf[:, h * 256:(h + 1) * 256], start=False, stop=True),
                              r=[qkT, Sbf], w=[PU[h // 2]])
                    for h in range(4):
                        kb.op('act', lambda h=h: A_.activation(out=junk[:, 0:256], in_=PU[h // 2][:, (h % 2) * 256:(h % 2 + 1) * 256],
                                                               func=AF.Square, accum_out=st[:, 4 + h:5 + h]),
                              r=[PU[h // 2]], w=[junk, st])
                    yield
                    rstd_from_ss(st[:, 4:8], 4, 256.0, st)
                    for h in range(4):
                        kb.op('pool', lambda h=h: G_.tensor_tensor(out=sg[:, h * 256:(h + 1) * 256], in0=sg[:, h * 256:(h + 1) * 256],
                                                                   in1=gnorm[:], op=ALU.mult), r=[sg, gnorm], w=[sg])
                    for h in range(4):
                        kb.op('dve', lambda h=h: V_.scalar_tensor_tensor(
                            out=ogl[:, h * 256:(h + 1) * 256], in0=PU[h // 2][:, (h % 2) * 256:(h % 2 + 1) * 256],
                            scalar=st[:, 4 + h:5 + h], in1=sg[:, h * 256:(h + 1) * 256], op0=ALU.mult, op1=ALU.mult),
                            r=[PU[h // 2], st, sg], w=[ogl])
                    for c in range(8):
                        kb.op('pe', lambda c=c: P_.transpose(out=PT[:, c, :], in_=ogl[:, c * 128:(c + 1) * 128],
                                                             identity=ident[:]), r=[ogl, ident], w=[PT])
                    ogT = ogTs.next()
                    kb.op('act', lambda: A_.copy(out=ogT[:], in_=PT[:]), r=[PT], w=[ogT])
                    kb.dma('sp', ogla_scr[slot], ogT[:].rearrange("p a b -> p (a b)"), ogT, load=False)

            items = []
            for m in range(NOWN):
                for p in range(4):
                    blk = 4 * m + p
                    items.append((g_pre(x_full[blk * 128:(blk + 1) * 128, :]),
                                  lambda pre, p=p, m=m: gla_block(pre, False, p, m)))
                items.append((g_pre(x_own[m * 128:(m + 1) * 128, :]), lambda pre, m=m: gla_block(pre, True, 0, m)))
            pipeline2(items, depth=2, lag=4, ahead=2)
            kb.barrier()

        KT = sb(top, "KT", [128, 2, TT], BF16)
        VV = sb(top, "VV", [128, NB, 256], BF16)
        kidxT = sb(top, "kidxT", [128, TT // 2], BF16)
        with ExitStack() as ph:
            Wk = sb(ph, "k_W", [128, 8, 576], BF16)
            g_mix = sb(ph, "g_mixt", [128, 1024], F32)
            kg2 = sb(ph, "kg2", [128, 256], F32)
            ikg = sb(ph, "ikg", [128, 128], F32)
            load_gain(g_mix, "g_mix", 1024)
            load_gain(kg2, "dsa_k_norm", 128, rep=2)
            load_gain(ikg, "idx_k_norm", 64, rep=2)
            xts = Rot([sb(ph, "k_x%d" % i, [128, 1024], F32) for i in range(4)])
            junk = JunkRot([sb(ph, "k_junk%d" % i, [128, 1024], BF16) for i in range(3)])
            sts = Rot([sb(ph, "k_st%d" % i, [128, 8], F32) for i in range(4)])
            hbs = Rot([sb(ph, "k_hb%d" % i, [128, 1024], BF16) for i in range(4)])
            hTs = Rot([sb(ph, "k_hT%d" % i, [128, 8, 128], BF16) for i in range(3)])
            knbs = Rot([sb(ph, "k_knb%d" % i, [128, 256], BF16) for i in range(3)])
            ikbs = Rot([sb(ph, "k_ikb%d" % i, [128, 128], BF16) for i in range(3)])
            PJs = Rot([ps(ph, "k_PJ%d" % i, [128, 512]) for i in range(2)])
            PI = ps(ph, "k_PI", [128, 512])
            PT = ps(ph, "k_PT", [128, 8, 128], BF16)
            PT2 = ps(ph, "k_PT2", [128, 4, 128], BF16)
            kb.dma('pool', Wk[:, :, 0:512], wchunks(w_in, OFF['dk'], 512), Wk)
            kb.dma('pool', Wk[:, :, 512:576], wchunks(w_in, OFF['ik'], 64), Wk)
            def k_pre(blk):
                def f():
                    xt = xts.next(); st = sts.next(); hb = hbs.next()
                    kb.dma('sp', xt[:], x_full[blk * 128:(blk + 1) * 128, :], xt)
                    norm_only(xt, g_mix, junk, st, hb)
                    return (st, hb)
                return f

            def k_block(blk, pre):
                st, hb = pre
                hT = hTs.next(); knb = knbs.next(); ikb = ikbs.next()
                transp_T(hb, PT, hT[:], hT)
                yield
                hf = lambda c, hT=hT: hT[:, c, :]
                pj = PJs.next()
                proj(hf, hT, Wk, 0, 512, pj)
                proj(hf, hT, Wk, 512, 64, PI)
                yield
                kb.op('act', lambda pj=pj, blk=blk: A_.copy(out=VV[:, blk, :], in_=pj[:, 256:512]), r=[pj], w=[])
                for g in range(2):
                    kb.op('act', lambda g=g, pj=pj, st=st: A_.activation(out=junk[:, 0:128], in_=pj[:, g * 128:(g + 1) * 128],
                                                                         func=AF.Square, accum_out=st[:, 4 + g:5 + g]),
                          r=[pj], w=[junk, st])
                kb.op('act', lambda st=st: A_.activation(out=junk[:, 0:64], in_=PI[:, 0:64], func=AF.Square,
                                                         accum_out=st[:, 6:7]), r=[PI], w=[junk, st])
                yield
                rstd_from_ss(st[:, 4:6], 2, 128.0, st)
                rstd_from_ss(st[:, 6:7], 1, 64.0, st)
                for g in range(2):
                    kb.op('dve', lambda g=g, pj=pj, st=st: V_.scalar_tensor_tensor(
                        out=knb[:, g * 128:(g + 1) * 128], in0=pj[:, g * 128:(g + 1) * 128], scalar=st[:, 4 + g:5 + g],
                        in1=kg2[:, g * 128:(g + 1) * 128], op0=ALU.mult, op1=ALU.mult), r=[pj, st, kg2], w=[knb])
                for hh in range(2):
                    kb.op('dve', lambda st=st, hh=hh: V_.scalar_tensor_tensor(
                        out=ikb[:, hh * 64:(hh + 1) * 64], in0=PI[:, 0:64], scalar=st[:, 6:7], in1=ikg[:, 0:64],
                        op0=ALU.mult, op1=ALU.mult), r=[PI, st, ikg], w=[ikb])
                yield
                for g in range(2):
                    kb.op('pe', lambda g=g: P_.transpose(out=PT2[:, g, :], in_=knb[:, g * 128:(g + 1) * 128],
                                                         identity=ident[:]), r=[knb, ident], w=[PT2])
                kb.op('pe', lambda: P_.transpose(out=PT2[:, 2, :], in_=ikb[:], identity=ident[:]),
                      r=[ikb, ident], w=[PT2])
                kb.op('act', lambda blk=blk: A_.copy(out=KT[:, :, blk * 128:(blk + 1) * 128], in_=PT2[:, 0:2, :]),
                      r=[PT2], w=[])
                hp = 0 if blk < NB // 2 else 64
                bl = blk if blk < NB // 2 else blk - NB // 2
                kb.op('act', lambda bl=bl, hp=hp: A_.copy(out=kidxT[hp:hp + 64, bl * 128:(bl + 1) * 128],
                                                          in_=PT2[hp:hp + 64, 2, :]), r=[PT2], w=[])
            pipeline2([(k_pre(blk), lambda pre, blk=blk: k_block(blk, pre)) for blk in range(NB)], depth=2, lag=2, ahead=2)
            if dbg:
                dbgs['KT'] = nc.dram_tensor("dbg_KT", [128, 2 * TT], BF16, kind="ExternalOutput")
                kb.dma('sp', dbgs['KT'].ap(), KT[:].rearrange("p a b -> p (a b)"), KT, load=False)
                dbgs['kidxT'] = nc.dram_tensor("dbg_kidxT", [128, TT // 2], BF16, kind="ExternalOutput")
                kb.dma('sp', dbgs['kidxT'].ap(), kidxT[:], kidxT, load=False)
            kb.barrier()

        U8 = mybir.dt.uint8
        for j in range(NTILE):
            with ExitStack() as tl:
                hTo = sb(tl, "c_hTo", [128, 8, 512], BF16)
                OdT = sb(tl, "c_OdT", [128, 8, 512], BF16)
                with ExitStack() as ph:
                    g_mix = sb(ph, "g_mixt", [128, 1024], F32)
                    xt = sb(ph, "c0_x", [128, 1024], F32)
                    junk = JunkRot([sb(ph, "c0_junk%d" % i, [128, 1024], BF16) for i in range(3)])
                    st = sb(ph, "c0_st", [128, 8], F32)
                    hbs4 = [sb(ph, "c0_hb%d" % i, [128, 1024], BF16) for i in range(4)]
                    xts2 = Rot([xt, sb(ph, "c0_x2", [128, 1024], F32)])
                    sts4 = [sb(ph, "c0_st%d" % i, [128, 8], F32) for i in range(4)]
                    PTs = Rot([ps(ph, "c0_PT%d" % i, [128, 8, 128], BF16) for i in range(2)])
                    load_gain(g_mix, "g_mix", 1024)
                    for s in range(4):
                        m = 4 * j + s
                        xt_ = xts2.next()
                        kb.dma('sp', xt_[:], x_own[m * 128:(m + 1) * 128, :], xt_)
                        norm_only(xt_, g_mix, junk, sts4[s], hbs4[s])
                    for s in range(4):
                        transp_T(hbs4[s], PTs.next(), hTo[:, :, s * 128:(s + 1) * 128], hTo)
                    kb.barrier()
                with ExitStack() as c1:
                    QT = sb(c1, "c1_QT", [128, 8, 512], BF16)
                    IQT = sb(c1, "c1_IQT", [128, 8, 512], BF16)
                    wt = sb(c1, "c1_wt", [128, 4, 8], F32)
                    wabs = sb(c1, "c1_wabs", [128, 4, 8], F32)
                    wsgn = sb(c1, "c1_wsgn", [128, 4, 8], F32)
                    with ExitStack() as ph:
                        qg8 = sb(ph, "qg8", [128, 1024], F32)
                        wbufs = Rot([sb(ph, "c1a_w%d" % i, [128, 8, 512], BF16) for i in range(2)])
                        wiw = sb(ph, "c1a_wiw", [128, 8, 8], BF16)
                        qn = sb(ph, "c1a_qn", [128, 512], BF16)
                        iqd = sb(ph, "c1a_iqd", [128, 8, 2, 64], BF16)
                        junk = JunkRot([sb(ph, "c1a_junk%d" % i, [128, 128], BF16) for i in range(3)])
                        st = sb(ph, "c1a_st", [128, 8], F32)
                        PJs = Rot([ps(ph, "c1a_PJ%d" % i, [128, 512]) for i in range(2)])
                        PJw = ps(ph, "c1a_PJw", [128, 512])
                        PT = ps(ph, "c1a_PT", [128, 8, 128], BF16)
                        load_gain(qg8, "dsa_q_norm", 128, rep=8, scale=128.0 ** -0.5)
                        kb.dma('pool', wiw[:], wchunks(w_in, OFF['iw'], 8), wiw)
                        pend = None
                        for wi, col0 in enumerate((OFF['dq'], OFF['dq'] + 512, OFF['iq'])):
                            wb = wbufs.next()
                            kb.dma('pool', wb[:], wchunks(w_in, col0, 512), wb)
                            for s in range(4):
                                hf = lambda c, s=s: hTo[:, c, s * 128:(s + 1) * 128]
                                pj = PJs.next()
                                proj(hf, hTo, wb, 0, 512, pj)
                                if pend is not None:
                                    pend()
                                def post(wi=wi, s=s, pj=pj, hf=hf):
                                    if wi < 2:
                                        for hh in range(4):
                                            kb.op('act', lambda hh=hh, pj=pj: A_.activation(
                                                out=junk[:], in_=pj[:, hh * 128:(hh + 1) * 128], func=AF.Square,
                                                accum_out=st[:, hh:hh + 1]), r=[pj], w=[junk, st])
                                        rstd_from_ss(st[:, 0:4], 4, 128.0, st)
                                        for hh in range(4):
                                            kb.op('dve', lambda hh=hh, pj=pj, wi=wi: V_.scalar_tensor_tensor(
                                                out=qn[:, hh * 128:(hh + 1) * 128], in0=pj[:, hh * 128:(hh + 1) * 128],
                                                scalar=st[:, hh:hh + 1], in1=qg8[:, (wi * 4 + hh) * 128:(wi * 4 + hh + 1) * 128],
                                                op0=ALU.mult, op1=ALU.mult), r=[pj, st, qg8], w=[qn])
                                        for hh in range(4):
                                            kb.op('pe', lambda hh=hh: P_.transpose(out=PT[:, hh, :], in_=qn[:, hh * 128:(hh + 1) * 128],
                                                                                   identity=ident[:]), r=[qn, ident], w=[PT])
                                        kb.op('act', lambda wi=wi, s=s: A_.copy(out=QT[:, wi * 4:wi * 4 + 4, s * 128:(s + 1) * 128],
                                                                                in_=PT[:, 0:4, :]), r=[PT], w=[QT])
                                    else:
                                        pv = pj[:, :].rearrange("p (h d) -> p h d", h=8)
                                        for dd in range(2):
                                            kb.op('act', lambda dd=dd, pv=pv: A_.copy(out=iqd[:, :, dd, :], in_=pv), r=[pj], w=[iqd])
                                        for h in range(8):
                                            kb.op('pe', lambda h=h: P_.transpose(
                                                out=PT[:, h, :], in_=iqd[:, h, :, :].rearrange("p a b -> p (a b)"),
                                                identity=ident[:]), r=[iqd, ident], w=[PT])
                                        kb.op('act', lambda s=s: A_.copy(out=IQT[:, :, s * 128:(s + 1) * 128], in_=PT[:]),
                                              r=[PT], w=[IQT])
                                        pj2 = PJw
                                        proj(hf, hTo, wiw, 0, 8, pj2)
                                        kb.op('dve', lambda s=s, pj2=pj2: V_.tensor_scalar(
                                            out=wt[:, s, :], in0=pj2[:, 0:8], scalar1=(8.0 ** -0.5) * (64.0 ** -0.5), scalar2=None,
                                            op0=ALU.mult), r=[pj2], w=[wt])
                                        kb.op('dve', lambda s=s: V_.tensor_scalar(out=wsgn[:, s, :], in0=wt[:, s, :], scalar1=0.0, scalar2=2.0,
                                                                                   op0=ALU.is_ge, op1=ALU.mult), r=[wt], w=[wsgn])
                                        kb.op('dve', lambda s=s: V_.tensor_scalar(out=wsgn[:, s, :], in0=wsgn[:, s, :], scalar1=-1.0, scalar2=None,
                                                                                   op0=ALU.add), r=[wsgn], w=[wsgn])
                                        kb.op('dve', lambda s=s: V_.tensor_tensor(out=wabs[:, s, :], in0=wt[:, s, :], in1=wsgn[:, s, :], op=ALU.mult),
                                              r=[wt, wsgn], w=[wabs])
                                pend = post
                        pend()
                        kb.barrier()
                    with ExitStack() as ph:
                        NKT = 4 * (j + 1)
                        scs = [sb(ph, "c1b_sc%d" % i, [128, NKT * 512], F32) for i in range(2)]
                        jk = sb(ph, "c1b_jk", [128, 2048], U8)
                        cmask = sb(ph, "cmaskt", [128, 512], F32)
                        cbis = sb(ph, "cbis", [128, NBIS], F32)
                        Rs = Rot([sb(ph, "c1b_R%d" % i, [128, 512], BF16) for i in range(2)])
                        Ps = Rot([sb(ph, "c1b_P%d" % i, [128, 512], BF16) for i in range(3)])
                        nmt = [sb(ph, "c1b_nm%d" % i, [128, 512], BF16) for i in range(NKT)]
                        Dm = sb(ph, "c1b_Dm", [128, 8, 128], BF16)
                        rec = sb(ph, "c1b_rec", [128, 512], F32)
                        bss = [sb(ph, "c1b_bs%d" % i, [128, 8], F32) for i in range(2)]
                        dl = sb(ph, "c1b_dl", [128, NBIS], F32)
                        PSIs = Rot([ps(ph, "c1b_PI%d" % i, [128, 512]) for i in range(2)])
                        PSC = ps(ph, "c1b_PC", [128, 512])
                        PSSs = Rot([ps(ph, "c1b_PS%d" % i, [128, 512]) for i in range(3)])
                        PSO = ps(ph, "c1b_PO", [128, 512])
                        PSM = ps(ph, "c1b_PM", [128, 512])
                        kb.dma('sp', cmask[:], cmask_d, cmask)
                        kb.dma('sp', cbis[:], c_bis, cbis)

                        def c1_index(s):
                            m = 4 * j + s
                            scores = scs[s % 2]
                            tok = slice(s * 128, (s + 1) * 128)
                            for h in range(8):
                                kb.op('dve', lambda: V_.tensor_scalar(out=Dm[:, h, :], in0=ident[:], scalar1=wsgn[:, s, h:h + 1],
                                                                      scalar2=None, op0=ALU.mult), r=[ident, wsgn], w=[Dm])
                            yield 0.5
                            pend = None
                            for kt in range(m + 1):
                                hp = 0 if kt * 512 < TT // 2 else 64
                                kc = kt * 512 - (0 if hp == 0 else TT // 2)
                                for h in range(8):
                                    psi = PSIs.next()
                                    kb.op('pe', lambda: P_.matmul(
                                        psi[:], lhsT=IQT[hp:hp + 64, h, tok], rhs=kidxT[hp:hp + 64, kc:kc + 512],
                                        start=True, stop=True), r=[IQT, kidxT], w=[psi])
                                    R = Rs.next()
                                    kb.op('act', lambda: A_.activation(out=R[:], in_=psi[:], func=AF.Relu, scale=wabs[:, s, h:h + 1]),
                                          r=[psi, wabs], w=[R])
                                    if pend is not None:
                                        pend()
                                    def acc(h=h, R=R, kt=kt):
                                        kb.op('pe', lambda: P_.matmul(PSC[:], lhsT=Dm[:, h, :], rhs=R[:], start=(h == 0), stop=(h == 7)),
                                              r=[Dm, R], w=[PSC])
                                        if h == 7:
                                            kb.op('act', lambda: A_.copy(out=scores[:, kt * 512:(kt + 1) * 512], in_=PSC[:]),
                                                  r=[PSC], w=[scores])
                                    pend = acc
                                    if h % 4 == 3:
                                        yield 2.7
                            pend()
                            yield 0.5

                        def c1_bisect(s):
                            m = 4 * j + s
                            nk = 512 * (m + 1)
                            scores = scs[s % 2]
                            bs = bss[s % 2]
                            kb.op('dve', lambda: V_.tensor_reduce(out=bs[:, 0:1], in_=scores[:, 0:nk], axis=AX.X, op=ALU.max,
                                                                  apply_absolute_value=True), r=[scores], w=[bs])
                            kb.op('dve', lambda: V_.tensor_tensor(out=scores[:, m * 512:(m + 1) * 512],
                                                                  in0=scores[:, m * 512:(m + 1) * 512], in1=cmask[:], op=ALU.add),
                                  r=[scores, cmask], w=[scores])
                            kb.op('dve', lambda: V_.tensor_scalar(out=dl[:], in0=cbis[:], scalar1=bs[:, 0:1], scalar2=None,
                                                                  op0=ALU.mult), r=[cbis, bs], w=[dl])
                            kb.op('dve', lambda: V_.tensor_scalar(out=bs[:, 1:2], in0=bs[:, 0:1], scalar1=-1.0, scalar2=None,
                                                                  op0=ALU.mult), r=[bs], w=[bs])
                            yield nk / 850.0 + 1.5
                            for k in range(NBIS):
                                kb.op('dve', lambda: V_.tensor_tensor(out=bs[:, 2:3], in0=bs[:, 1:2], in1=dl[:, k:k + 1],
                                                                      op=ALU.add), r=[bs, dl], w=[bs])
                                for ci, c0 in enumerate(range(0, nk, 2048)):
                                    cw = min(2048, nk - c0)
                                    kb.op('dve', lambda: V_.tensor_scalar(
                                        out=jk[:, 0:cw], in0=scores[:, c0:c0 + cw], scalar1=bs[:, 2:3],
                                        scalar2=(None if ci == 0 else bs[:, 3:4]), op0=ALU.is_ge, op1=ALU.add,
                                        accum_out=bs[:, 3:4]), r=[scores, bs], w=[jk, bs])
                                kb.op('dve', lambda: V_.scalar_tensor_tensor(out=bs[:, 4:5], in0=bs[:, 3:4], scalar=255.5,
                                                                             in1=dl[:, k:k + 1], op0=ALU.is_ge, op1=ALU.mult),
                                      r=[bs, dl], w=[bs])
                                kb.op('dve', lambda: V_.tensor_tensor(out=bs[:, 1:2], in0=bs[:, 1:2], in1=bs[:, 4:5], op=ALU.add),
                                      r=[bs], w=[bs])
                                yield nk / 850.0 + 1.3

                        def c1_nm(s):
                            m = 4 * j + s
                            scores = scs[s % 2]
                            bs = bss[s % 2]
                            for kt in range(m + 1):
                                kb.op('dve', lambda: V_.tensor_scalar(
                                    out=nmt[kt][:], in0=scores[:, kt * 512:(kt + 1) * 512], scalar1=bs[:, 1:2], scalar2=NEG,
                                    op0=ALU.is_lt, op1=ALU.mult), r=[scores, bs], w=[nmt[kt]])

                        def c1_attn(s):
                            m = 4 * j + s
                            tok = slice(s * 128, (s + 1) * 128)
                            nkb = 4 * (m + 1)
                            for g in range(2):
                                pend = None
                                for kb_ in range(nkb):
                                    nm = nmt[kb_ // 4]
                                    pss = PSSs.next()
                                    kb.op('pe', lambda: P_.matmul(
                                        pss[:], lhsT=nm[:, (kb_ % 4) * 128:(kb_ % 4 + 1) * 128], rhs=i4[:], start=True, stop=False),
                                        r=[nm, i4], w=[pss])
                                    kb.op('pe', lambda: P_.matmul(
                                        pss[:], lhsT=KT[:, g, kb_ * 128:(kb_ + 1) * 128], rhs=QT[:, 4 * g:4 * g + 4, tok],
                                        start=False, stop=True), r=[KT, QT], w=[pss])
                                    Pb = Ps.next()
                                    kb.op('act', lambda: A_.activation(out=Pb[:], in_=pss[:], func=AF.Exp), r=[pss], w=[Pb])
                                    if pend is not None:
                                        pend()
                                    def pv(kb_=kb_, Pb=Pb, g=g):
                                        kb.op('pe', lambda: P_.matmul(
                                            PSO[:], lhsT=VV[:, kb_, g * 128:(g + 1) * 128], rhs=Pb[:],
                                            start=(kb_ == 0), stop=(kb_ == nkb - 1)), r=[VV, Pb], w=[PSO])
                                        kb.op('pe', lambda: P_.matmul(
                                            PSM[:], lhsT=ones[:], rhs=Pb[:], start=(kb_ == 0), stop=(kb_ == nkb - 1)),
                                            r=[ones, Pb], w=[PSM])
                                    pend = pv
                                    yield 1.1
                                pend()
                                kb.op('dve', lambda: V_.reciprocal(out=rec[:], in_=PSM[:]), r=[PSM], w=[rec])
                                kb.op('dve', lambda: V_.tensor_tensor(
                                    out=OdT[:, 4 * g:4 * g + 4, tok], in0=PSO[:, :].rearrange("p (h q) -> p h q", h=4),
                                    in1=rec[:, :].rearrange("p (h q) -> p h q", h=4), op=ALU.mult), r=[PSO, rec], w=[OdT])
                                yield 1.0

                        def weave3(gens):
                            acc_t = [0.0 for _ in gens]
                            live = [g is not None for g in gens]
                            while any(live):
                                i = min((k for k in range(len(gens)) if live[k]), key=lambda k: acc_t[k])
                                try:
                                    acc_t[i] += next(gens[i])
                                except StopIteration:
                                    live[i] = False

                        for t in range(-2, 4):
                            gl = []
                            if 0 <= t + 2 < 4:
                                gl.append(c1_index(t + 2))
                            if 0 <= t + 1 < 4:
                                gl.append(c1_bisect(t + 1))
                            if 0 <= t < 4:
                                gl.append(c1_attn(t))
                            weave3(gl)
                            if 0 <= t + 1 < 4:
                                c1_nm(t + 1)
                        if dbg and j == 0:
                            dbgs['OdT'] = nc.dram_tensor("dbg_OdT", [128, 8 * 512], BF16, kind="ExternalOutput")
                            kb.dma('sp', dbgs['OdT'].ap(), OdT[:].rearrange("p a b -> p (a b)"), OdT, load=False)
                        kb.barrier()
                OmT = sb(tl, "c_OmT", [128, 8, 512], BF16)
                x1 = sb(tl, "c_x1", [128, 4, 1024], F32)
                s23 = ExitStack()
                c3wbufs = Rot([sb(s23, "c3_w%d" % i, [128, 8, 1024], BF16) for i in range(2)])
                c3bgs = Rot([sb(s23, "c3_bg%d" % i, [1, 512], BF16) for i in range(2)])
                def c3_load(br, nt):
                    wbuf = c3wbufs.next(); bg = c3bgs.next()
                    kb.dma('pool', wbuf[:, :, 0:512], wchunks(w_in, OFF['gates'] + br * 1024 + nt * 512, 512), wbuf)
                    kb.dma('pool', wbuf[:, :, 512:1024], wchunks(w_o[br], nt * 512, 512), wbuf)
                    kb.dma('pool', bg[:], vecs["b_gate"].ap()[:, br * 1024 + nt * 512:br * 1024 + (nt + 1) * 512], bg)
                    return wbuf, bg

                def c3_load_out():
                    wbuf = c3wbufs.next()
                    kb.dma('pool', wbuf[:, :, 0:512], wchunks(w_out, 0, 512), wbuf)
                    kb.dma('pool', wbuf[:, :, 512:1024], wchunks(w_out, 512, 512), wbuf)
                    return wbuf

                c3_first = c3_load(0, 0)
                with ExitStack() as ph:
                    mqg4 = sb(ph, "mqg4", [128, 1024], F32)
                    wbufs = Rot([sb(ph, "c2_w%d" % i, [128, 8, 512], BF16) for i in range(2)])
                    mqf = sb(ph, "c2_mqf", [128, 4, 1024], F32)
                    mqb = sb(ph, "c2_mqb", [128, 1024], BF16)
                    MQT = sb(ph, "c2_MQT", [128, 8, 512], BF16)
                    junk = JunkRot([sb(ph, "c2_junk%d" % i, [128, 256], BF16) for i in range(3)])
                    st = sb(ph, "c2_st", [128, 8], F32)
                    Pms = Rot([sb(ph, "c2_P%d" % i, [128, 512], BF16) for i in range(2)])
                    rec = sb(ph, "c2_rec", [128, 512], F32)
                    PJs = Rot([ps(ph, "c2_PJ%d" % i, [128, 512]) for i in range(2)])
                    PT = ps(ph, "c2_PT", [128, 8, 128], BF16)
                    PSs = Rot([ps(ph, "c2_PS%d" % i, [128, 512]) for i in range(2)])
                    PO2 = [ps(ph, "c2_PO%d" % i, [128, 512]) for i in range(2)]
                    PM2 = ps(ph, "c2_PM", [128, 512])
                    load_gain(mqg4, "mem_q_norm", 256, rep=4, scale=256.0 ** -0.5)
                    for nt in range(2):
                        wb = wbufs.next()
                        kb.dma('pool', wb[:], wchunks(w_in, OFF['mq'] + nt * 512, 512), wb)
                        for s in range(4):
                            pj = PJs.next()
                            proj(lambda c, s=s: hTo[:, c, s * 128:(s + 1) * 128], hTo, wb, 0, 512, pj)
                            kb.op('act', lambda pj=pj, s=s, nt=nt: A_.copy(out=mqf[:, s, nt * 512:(nt + 1) * 512], in_=pj[:]),
                                  r=[pj], w=[mqf])
                    for s in range(4):
                        for h in range(4):
                            kb.op('act', lambda h=h, s=s: A_.activation(out=junk[:], in_=mqf[:, s, h * 256:(h + 1) * 256],
                                                                        func=AF.Square, accum_out=st[:, h:h + 1]),
                                  r=[mqf], w=[junk, st])
                        rstd_from_ss(st[:, 0:4], 4, 256.0, st)
                        for h in range(4):
                            kb.op('dve', lambda h=h, s=s: V_.scalar_tensor_tensor(
                                out=mqb[:, h * 256:(h + 1) * 256], in0=mqf[:, s, h * 256:(h + 1) * 256], scalar=st[:, h:h + 1],
                                in1=mqg4[:, h * 256:(h + 1) * 256], op0=ALU.mult, op1=ALU.mult), r=[mqf, st, mqg4], w=[mqb])
                        for c8 in range(8):
                            kb.op('pe', lambda c8=c8: P_.transpose(out=PT[:, c8, :], in_=mqb[:, c8 * 128:(c8 + 1) * 128],
                                                                   identity=ident[:]), r=[mqb, ident], w=[PT])
                        kb.op('act', lambda s=s: A_.copy(out=MQT[:, :, s * 128:(s + 1) * 128], in_=PT[:]), r=[PT], w=[MQT])
                    for h in range(4):
                        pend = None
                        for mc in range(2):
                            pss = PSs.next()
                            for c in range(2):
                                kb.op('pe', lambda pss=pss, h=h, mc=mc, c=c: P_.matmul(
                                    pss[:], lhsT=memKT[:, h * 2 + c, mc * 128:(mc + 1) * 128], rhs=MQT[:, h * 2 + c, :],
                                    start=(c == 0), stop=(c == 1)), r=[memKT, MQT], w=[pss])
                            Pm = Pms.next()
                            kb.op('act', lambda pss=pss, Pm=Pm: A_.activation(out=Pm[:], in_=pss[:], func=AF.Exp),
                                  r=[pss], w=[Pm])
                            if pend is not None:
                                pend()
                            def pvm(Pm=Pm, h=h, mc=mc):
                                for vc in range(2):
                                    kb.op('pe', lambda vc=vc: P_.matmul(
                                        PO2[vc][:], lhsT=memV[:, mc, h * 256 + vc * 128:h * 256 + (vc + 1) * 128], rhs=Pm[:],
                                        start=(mc == 0), stop=(mc == 1)), r=[memV, Pm], w=[PO2[vc]])
                                kb.op('pe', lambda: P_.matmul(PM2[:], lhsT=ones[:], rhs=Pm[:], start=(mc == 0),
                                                              stop=(mc == 1)), r=[ones, Pm], w=[PM2])
                            pend = pvm
                        pend()
                        kb.op('dve', lambda: V_.reciprocal(out=rec[:], in_=PM2[:]), r=[PM2], w=[rec])
                        for vc in range(2):
                            kb.op('dve', lambda h=h, vc=vc: V_.tensor_tensor(out=OmT[:, h * 2 + vc, :], in0=PO2[vc][:], in1=rec[:],
                                                                             op=ALU.mult), r=[PO2[vc], rec], w=[OmT])
                    kb.barrier()
                with ExitStack() as ph:
                    gt = sb(ph, "c3_gt", [128, 512], F32)
                    tmp = sb(ph, "c3_tmp", [128, 512], F32)
                    mbf = sb(ph, "c3_mbf", [128, 1024], BF16)
                    mT = sb(ph, "c3_mT", [128, 8, 128], BF16)
                    xo = sb(ph, "c3_xo", [128, 1024], F32)
                    ogs = [sb(ph, "c3_og%d" % i, [128, 8, 128], BF16) for i in range(4)]
                    for s in range(4):
                        kb.dma('sp', ogs[s][:].rearrange("p a b -> p (a b)"), ogla_scr[4 * j + s], ogs[s])
                    PGs = Rot([ps(ph, "c3_PG%d" % i, [128, 512]) for i in range(2)])
                    PPs = Rot([ps(ph, "c3_PP%d" % i, [128, 512]) for i in range(2)])
                    PT = ps(ph, "c3_PT", [128, 8, 128], BF16)
                    chunks = [(br, nt) for br in range(3) for nt in range(2)]
                    nxt = c3_first
                    for ci, (br, nt) in enumerate(chunks):
                        if True:
                            wbuf, bg = nxt
                            nxt = c3_load(*chunks[ci + 1]) if ci + 1 < len(chunks) else (c3_load_out(), None)
                            for s in range(4):
                                tok = slice(s * 128, (s + 1) * 128)
                                pg = PGs.next()
                                for c in range(8):
                                    kb.op('pe', lambda pg=pg, c=c, tok=tok: P_.matmul(pg[:], lhsT=hTo[:, c, tok], rhs=wbuf[:, c, 0:512],
                                                                                      start=(c == 0), stop=False), r=[hTo, wbuf], w=[pg])
                                kb.op('pe', lambda pg=pg: P_.matmul(pg[:], lhsT=ones[0:1, :], rhs=bg[:], start=False, stop=True),
                                      r=[ones, bg], w=[pg])
                                kb.op('act', lambda pg=pg: A_.activation(out=gt[:], in_=pg[:], func=AF.Sigmoid), r=[pg], w=[gt])
                                pp = PPs.next()
                                for c in range(8):
                                    if br == 0:
                                        lh = ogs[s][:, c, :]
                                        lt = ogs[s]
                                    elif br == 1:
                                        lh = OdT[:, c, tok]
                                        lt = OdT
                                    else:
                                        lh = OmT[:, c, tok]
                                        lt = OmT
                                    kb.op('pe', lambda pp=pp, c=c, lh=lh: P_.matmul(pp[:], lhsT=lh, rhs=wbuf[:, c, 512:1024],
                                                                                    start=(c == 0), stop=(c == 7)), r=[lt, wbuf], w=[pp])
                                dst = x1[:, s, nt * 512:(nt + 1) * 512]
                                if br == 0:
                                    kb.op('dve', lambda pp=pp, dst=dst: V_.tensor_tensor(out=dst, in0=gt[:], in1=pp[:], op=ALU.mult),
                                          r=[gt, pp], w=[x1])
                                else:
                                    kb.op('dve', lambda pp=pp: V_.tensor_tensor(out=tmp[:], in0=gt[:], in1=pp[:], op=ALU.mult),
                                          r=[gt, pp], w=[tmp])
                                    kb.op('dve', lambda dst=dst: V_.tensor_tensor(out=dst, in0=dst, in1=tmp[:], op=ALU.add),
                                          r=[tmp, x1], w=[x1])
                    wbuf = nxt[0]
                    mbfs = [mbf, sb(ph, "c3_mbf2", [128, 1024], BF16)]
                    mTs = [mT, sb(ph, "c3_mT2", [128, 8, 128], BF16)]

                    def c3_prep(s):
                        mb_, mt_ = mbfs[s % 2], mTs[s % 2]
                        kb.op('act', lambda: A_.copy(out=mb_[:], in_=x1[:, s, :]), r=[x1], w=[mb_])
                        for c in range(8):
                            kb.op('pe', lambda c=c: P_.transpose(out=PT[:, c, :], in_=mb_[:, c * 128:(c + 1) * 128],
                                                                 identity=ident[:]), r=[mb_, ident], w=[PT])
                        kb.op('act', lambda: A_.copy(out=mt_[:], in_=PT[:]), r=[PT], w=[mt_])

                    c3_prep(0)
                    for s in range(4):
                        m = 4 * j + s
                        if s + 1 < 4:
                            c3_prep(s + 1)
                        mt_ = mTs[s % 2]
                        kb.dma('sp', xo[:], x_own[m * 128:(m + 1) * 128, :], xo)
                        for nt in range(2):
                            pp = PPs.next()
                            for c in range(8):
                                kb.op('pe', lambda pp=pp, c=c, nt=nt: P_.matmul(pp[:], lhsT=mt_[:, c, :], rhs=wbuf[:, c, nt * 512:(nt + 1) * 512],
                                                                                start=(c == 0), stop=(c == 7)), r=[mt_, wbuf], w=[pp])
                            kb.op('dve', lambda pp=pp, s=s, nt=nt: V_.tensor_tensor(
                                out=x1[:, s, nt * 512:(nt + 1) * 512], in0=xo[:, nt * 512:(nt + 1) * 512], in1=pp[:], op=ALU.add),
                                r=[xo, pp], w=[x1])
                    if dbg and j == 0:
                        dbgs['x1'] = nc.dram_tensor("dbg_x1", [128, 4096], F32, kind="ExternalOutput")
                        kb.dma('sp', dbgs['x1'].ap(), x1[:].rearrange("p a b -> p (a b)"), x1, load=False)
                    kb.barrier()
                s23.close()
                with ExitStack() as ph:
                    g_ffn = sb(ph, "g_ffnt", [128, 1024], F32)
                    xnT = sb(ph, "c4_xnT", [128, 8, 512], BF16)
                    junk = JunkRot([sb(ph, "c4_junk%d" % i, [128, 1024], BF16) for i in range(3)])
                    hbs4 = [sb(ph, "c4_hb%d" % i, [128, 1024], BF16) for i in range(4)]
                    sts4 = [sb(ph, "c4_st%d" % i, [128, 8], F32) for i in range(4)]
                    wr = sb(ph, "c4_wr", [128, 8, 20], BF16)
                    brow = sb(ph, "c4_brow", [1, 20], BF16)
                    lgs4 = [sb(ph, "c4_lg%d" % i, [128, 20], F32) for i in range(4)]
                    rts4 = [sb(ph, "c4_rt%d" % i, [128, 64], F32) for i in range(4)]
                    comb = sb(ph, "c4_comb", [128, 4, 16], F32)
                    wgs = Rot([sb(ph, "c4_wg%d" % i, [128, 8, 256], BF16) for i in range(3)])
                    wus = Rot([sb(ph, "c4_wu%d" % i, [128, 8, 256], BF16) for i in range(3)])
                    wds = Rot([sb(ph, "c4_wd%d" % i, [128, 2, 1024], BF16) for i in range(3)])
                    sgs = Rot([sb(ph, "c4_sg%d" % i, [128, 512], F32) for i in range(2)])
                    hid = [sb(ph, "c4_hid%d" % i, [128, 512], BF16) for i in range(2)]
                    PGs = Rot([ps(ph, "c4_PG%d" % i, [128, 512]) for i in range(2)])
                    PUs = Rot([ps(ph, "c4_PU%d" % i, [128, 512]) for i in range(2)])
                    PDs = Rot([ps(ph, "c4_PD%d" % i, [128, 512]) for i in range(2)])
                    PT = ps(ph, "c4_PT", [128, 8, 128], BF16)
                    PR = ps(ph, "c4_PR", [128, 512])
                    load_gain(g_ffn, "g_ffn", 1024)
                    kb.dma('pool', wr[:, :, 0:4], wchunks(w_r1, 0, 4), wr)
                    kb.dma('pool', wr[:, :, 4:20], wchunks(w_r2, 0, 16), wr)
                    kb.dma('pool', brow[:, 0:4], vecs["b_r1"].ap(), brow)
                    kb.dma('pool', brow[:, 4:20], vecs["b_r2"].ap(), brow)
                    BIG = 1.0e4

                    def c4_head(s):
                        hb = hbs4[s]; st = sts4[s]; lg = lgs4[s]; rt = rts4[s]
                        PRs = PR[:, 32 * s:32 * s + 20]
                        tok = slice(s * 128, (s + 1) * 128)
                        kb.op('act', lambda s=s: A_.activation(out=junk[:], in_=x1[:, s, :], func=AF.Square, accum_out=st[:, 0:1]),
                              r=[x1], w=[junk, st])
                        rstd_from_ss(st[:, 0:1], 1, 1024.0, st)
                        yield
                        kb.op('dve', lambda s=s: V_.scalar_tensor_tensor(out=hb[:], in0=x1[:, s, :], scalar=st[:, 0:1], in1=g_ffn[:],
                                                                         op0=ALU.mult, op1=ALU.mult), r=[x1, st, g_ffn], w=[hb])
                        yield
                        for c in range(8):
                            kb.op('pe', lambda c=c: P_.transpose(out=PT[:, c, :], in_=hb[:, c * 128:(c + 1) * 128],
                                                                 identity=ident[:]), r=[hb, ident], w=[PT])
                        kb.op('act', lambda tok=tok: A_.copy(out=xnT[:, :, tok], in_=PT[:]), r=[PT], w=[xnT])
                        yield
                        for c in range(8):
                            kb.op('pe', lambda c=c, tok=tok: P_.matmul(PRs, lhsT=xnT[:, c, tok], rhs=wr[:, c, :],
                                                                       start=(c == 0), stop=False), r=[xnT, wr], w=[PR])
                        kb.op('pe', lambda: P_.matmul(PRs, lhsT=ones[0:1, :], rhs=brow[:], start=False, stop=True),
                              r=[ones, brow], w=[PR])
                        kb.op('dve', lambda: V_.tensor_copy(out=lg[:], in_=PRs), r=[PR], w=[lg])
                        yield
                        dv = lambda fn, r=(), w=(): kb.op('dve', fn, r=[lg, rt] + list(r), w=[rt] + list(w))
                        dv(lambda: V_.tensor_reduce(out=rt[:, 0:1], in_=lg[:, 0:4], axis=AX.X, op=ALU.max))
                        yield
                        dv(lambda: V_.tensor_scalar(out=rt[:, 1:2], in0=rt[:, 0:1], scalar1=-1.0, scalar2=None, op0=ALU.mult))
                        yield
                        kb.op('act', lambda: A_.activation(out=rt[:, 48:52], in_=lg[:, 0:4], func=AF.Exp, bias=rt[:, 1:2],
                                                           accum_out=rt[:, 2:3]), r=[lg, rt], w=[rt])
                        yield
                        dv(lambda: V_.reciprocal(out=rt[:, 3:4], in_=rt[:, 2:3]))
                        yield
                        dv(lambda: V_.tensor_scalar(out=rt[:, 4:8], in0=lg[:, 0:4], scalar1=rt[:, 0:1], scalar2=None, op0=ALU.is_ge))
                        yield
                        dv(lambda: V_.tensor_scalar(out=rt[:, 8:12], in0=rt[:, 4:8], scalar1=BIG, scalar2=-BIG, op0=ALU.mult,
                                                    op1=ALU.add))
                        yield
                        for g in range(4):
                            dv(lambda g=g: V_.tensor_scalar(out=rt[:, 12 + 4 * g:16 + 4 * g], in0=lg[:, 4 + 4 * g:8 + 4 * g],
                                                            scalar1=rt[:, 8 + g:9 + g], scalar2=None, op0=ALU.add))
                            yield
                        dv(lambda: V_.tensor_reduce(out=rt[:, 28:29], in_=rt[:, 12:28], axis=AX.X, op=ALU.max))
                        yield
                        dv(lambda: V_.tensor_scalar(out=rt[:, 29:30], in0=rt[:, 28:29], scalar1=-1.0, scalar2=None, op0=ALU.mult))
                        yield
                        dv(lambda: V_.tensor_scalar(out=rt[:, 30:46], in0=rt[:, 12:28], scalar1=rt[:, 28:29], scalar2=-BIG,
                                                    op0=ALU.is_ge, op1=ALU.mult))
                        yield
                        dv(lambda: V_.tensor_tensor(out=rt[:, 30:46], in0=rt[:, 30:46], in1=rt[:, 12:28], op=ALU.add))
                        yield
                        dv(lambda: V_.tensor_reduce(out=rt[:, 46:47], in_=rt[:, 30:46], axis=AX.X, op=ALU.max))
                        yield
                        kb.op('act', lambda: A_.activation(out=rt[:, 48:64], in_=rt[:, 12:28], func=AF.Exp, bias=rt[:, 29:30]),
                              r=[rt], w=[rt])
                        yield
                        dv(lambda: V_.scalar_tensor_tensor(out=rt[:, 48:64], in0=rt[:, 12:28], scalar=rt[:, 46:47], in1=rt[:, 48:64],
                                                           op0=ALU.is_ge, op1=ALU.mult))
                        yield
                        dv(lambda: V_.tensor_reduce(out=rt[:, 47:48], in_=rt[:, 48:64], axis=AX.X, op=ALU.add))
                        yield
                        dv(lambda: V_.reciprocal(out=rt[:, 47:48], in_=rt[:, 47:48]))
                        yield
                        dv(lambda: V_.tensor_tensor(out=rt[:, 47:48], in0=rt[:, 47:48], in1=rt[:, 3:4], op=ALU.mult))
                        yield
                        dv(lambda s=s: V_.tensor_scalar(out=comb[:, s, :], in0=rt[:, 48:64], scalar1=rt[:, 47:48], scalar2=None,
                                                        op0=ALU.mult), w=[comb])
                        yield
                    pipeline([c4_head(s) for s in range(4)], depth=4, lag=0)
                    hidA = [hid, [sb(ph, "c4_hidB%d" % i, [128, 512], BF16) for i in range(2)]]
                    wsets = {}

                    def c4_load(e):
                        wg_ = wgs.next(); wu_ = wus.next(); wd_ = wds.next()
                        kb.dma('pool', wg_[:], w_gate[e].rearrange("(c p) n -> p c n", p=128), wg_)
                        kb.dma('pool', wu_[:], w_up[e].rearrange("(c p) n -> p c n", p=128), wu_)
                        kb.dma('pool', wd_[:], w_down[e].rearrange("(c p) n -> p c n", p=128), wd_)
                        wsets[e] = (wg_, wu_, wd_)

                    def c4_gu(e, fc):
                        wg_, wu_, _ = wsets[e]
                        hd = hidA[e % 2][fc]
                        pg = PGs.next(); pu = PUs.next()
                        for c in range(8):
                            kb.op('pe', lambda c=c: P_.matmul(
                                pg[:], lhsT=wg_[:, c, fc * 128:(fc + 1) * 128], rhs=xnT[:, c, :], start=(c == 0), stop=(c == 7)),
                                r=[wg_, xnT], w=[pg])
                        for c in range(8):
                            kb.op('pe', lambda c=c: P_.matmul(
                                pu[:], lhsT=wu_[:, c, fc * 128:(fc + 1) * 128], rhs=xnT[:, c, :], start=(c == 0), stop=(c == 7)),
                                r=[wu_, xnT], w=[pu])
                        sg_ = sgs.next()
                        kb.op('act', lambda: A_.activation(out=sg_[:], in_=pg[:], func=AF.Silu), r=[pg], w=[sg_])
                        kb.op('dve', lambda: V_.tensor_tensor(out=hd[:], in0=sg_[:], in1=pu[:], op=ALU.mult),
                              r=[sg_, pu], w=[hd])

                    def c4_down(e, slots):
                        _, _, wd_ = wsets[e]
                        for s in slots:
                            tok = slice(s * 128, (s + 1) * 128)
                            for nt in range(2):
                                pd = PDs.next()
                                for fc in range(2):
                                    hd = hidA[e % 2][fc]
                                    kb.op('pe', lambda fc=fc, hd=hd: P_.matmul(
                                        pd[:], lhsT=hd[:, tok], rhs=wd_[:, fc, nt * 512:(nt + 1) * 512], start=(fc == 0), stop=(fc == 1)),
                                        r=[hd, wd_], w=[pd])
                                kb.op('dve', lambda s=s, nt=nt: V_.scalar_tensor_tensor(
                                    out=x1[:, s, nt * 512:(nt + 1) * 512], in0=pd[:], scalar=comb[:, s, e:e + 1],
                                    in1=x1[:, s, nt * 512:(nt + 1) * 512], op0=ALU.mult, op1=ALU.add), r=[pd, comb, x1], w=[x1])

                    c4_load(0)
                    c4_load(1)
                    c4_gu(0, 0)
                    c4_gu(0, 1)
                    for e in range(16):
                        if e + 2 < 16:
                            c4_load(e + 2)
                        if e + 1 < 16:
                            c4_gu(e + 1, 0)
                        c4_down(e, [0, 1])
                        if e + 1 < 16:
                            c4_gu(e + 1, 1)
                        c4_down(e, [2, 3])
                    for s in range(4):
                        m = 4 * j + s
                        kb.dma('sp', out_own[m * 128:(m + 1) * 128, :], x1[:, s, :], x1, load=False)
                    kb.barrier()
    return nc, kb, dbgs


def _consts():
    bf = ml_dtypes.bfloat16
    c = {}
    c["c_ident"] = np.eye(128, dtype=np.float32).astype(bf)
    c["c_i4"] = np.tile(np.eye(128, dtype=np.float32), (1, 4)).astype(bf)
    c["c_ones"] = np.ones((128, 128), np.float32).astype(bf)
    j = np.arange(128)[:, None]
    i = np.arange(128)[None, :]
    c["c_lmt"] = np.where(j <= i, -1.0 / 16.0, 0.0).astype(np.float32)
    c["c_umt"] = np.where(j > i, -1.0 / 16.0, 0.0).astype(np.float32)
    c["c_caus4"] = np.tile(np.where(j <= i, 1.0, 0.0), (1, 4)).astype(np.float32)
    sel = np.zeros((16, 16, 128), np.float32)
    for e in range(16):
        sel[e, e, :] = 1.0
    c["c_sel16"] = sel.reshape(16, 2048).astype(bf)
    c["c_bis"] = np.tile((2.0 ** (1.0 - np.arange(1, NBIS + 1)))[None, :], (128, 1)).astype(np.float32)
    return c


_CACHE = {}


def kernel(x, mem, g_mix, g_mem, w_in, w_gla_a2, b_gla_a2, gla_norm, w_mem_kv,
           dsa_q_norm, dsa_k_norm, idx_k_norm, mem_q_norm, mem_k_norm, b_gate,
           w_o_gla, w_o_dsa, w_o_mem, w_out, g_ffn, w_r1, b_r1, w_r2, b_r2,
           w_gate, w_up, w_down, _dbg=False):
    f = lambda a: np.ascontiguousarray(np.asarray(a, dtype=np.float32))
    x = f(x)
    B, TT, _ = x.shape
    key = (TT, _dbg)
    if key not in _CACHE:
        _CACHE[key] = build_nc(TT, dbg=_dbg)
    nc, kb, dbgs = _CACHE[key]
    NB = TT // 128
    consts = _consts()
    shared = {
        "w_in": f(w_in)[0], "w_gla_a2": f(w_gla_a2)[0], "w_mem_kv": f(w_mem_kv)[0],
        "w_o_gla": f(w_o_gla)[0], "w_o_dsa": f(w_o_dsa)[0], "w_o_mem": f(w_o_mem)[0], "w_out": f(w_out)[0],
        "w_r1": f(w_r1)[0], "w_r2": f(w_r2)[0], "w_gate": f(w_gate)[0], "w_up": f(w_up)[0], "w_down": f(w_down)[0],
        "g_mix": f(g_mix), "g_mem": f(g_mem), "g_ffn": f(g_ffn), "gla_norm": f(gla_norm), "dsa_q_norm": f(dsa_q_norm),
        "dsa_k_norm": f(dsa_k_norm), "idx_k_norm": f(idx_k_norm), "mem_q_norm": f(mem_q_norm), "mem_k_norm": f(mem_k_norm),
        "b_gla_a2": f(b_gla_a2), "b_gate": f(b_gate), "b_r1": f(b_r1), "b_r2": f(b_r2),
    }
    shared.update(consts)
    memf = f(mem)
    in_maps = []
    tpos = np.arange(128)[:, None]
    spos = np.arange(128)[None, :]
    for c in range(8):
        b, r = c // 4, c % 4
        xb = x[b].reshape(NB, 128, D)
        own = np.ascontiguousarray(xb[r::4].reshape(-1, D))
        cm = np.zeros((128, 512), np.float32)
        for p in range(4):
            if p == r:
                cm[:, p * 128:(p + 1) * 128] = np.where(spos <= tpos, 0.0, -1e30)
            elif p > r:
                cm[:, p * 128:(p + 1) * 128] = -1e30
        ws = np.zeros((128, 4), np.float32)
        ws[:, r] = 1.0
        d = dict(shared)
        d.update({"x_full": x[b], "x_own": own, "mem": memf[b], "cmask": cm, "wsel": ws})
        in_maps.append(d)
    res = run_bass_kernel_spmd(nc, in_maps, core_ids=list(range(8)))
    out = np.zeros((B, NB, 128, D), np.float32)
    for c in range(8):
        b, r = c // 4, c % 4
        out[b, r::4] = res.results[c]["out_own"].reshape(NB // 4, 128, D)
    if _dbg:
        kernel.last = res
    return out.reshape(B, TT, D)
```

```python
import numpy as np
import ml_dtypes
from contextlib import ExitStack
import concourse.bass as bass
import concourse.mybir as mybir
from concourse.bass_utils import run_bass_kernel_spmd

F32 = mybir.dt.float32
BF16 = mybir.dt.bfloat16
AF = mybir.ActivationFunctionType
ALU = mybir.AluOpType
AX = mybir.AxisListType

D = 1024
D_IN = 9304
EPS = 1e-6
OFF = dict(gq=0, gk=512, gv=1024, gg=2048, ga=3072, dq=3088, dk=4112, dv=4368, iq=4624,
           ik=5136, iw=5200, mq=5208, gates=6232)
NBIS = 14
NEG = -30000.0


class T:
    def __init__(self, h):
        self.h = h
        self.w = None
        self.r = {}
        self.dsem = None
        self.dcnt = 0
        self.wx = []

    def __getitem__(self, k):
        return self.h[k]


class Rot:
    def __init__(self, tiles):
        self.t = tiles
        self.i = 0

    def next(self):
        t = self.t[self.i % len(self.t)]
        self.i += 1
        return t


class JunkRot:
    def __init__(self, tiles):
        self.t = tiles
        self.i = 0

    def advance(self):
        self.i += 1
        return self.t[self.i % len(self.t)]

    def __getitem__(self, k):
        return self.t[self.i % len(self.t)].h[k]


class KB:
    def __init__(self, nc):
        self.nc = nc
        self.eng = {'pe': nc.tensor, 'act': nc.scalar, 'dve': nc.vector, 'pool': nc.gpsimd, 'sp': nc.sync}
        self.sem = {k: nc.alloc_semaphore("s_" + k) for k in self.eng}
        self.cnt = {k: 0 for k in self.eng}
        self.waited = {k: {} for k in self.eng}
        self.dma_evs = {}
        self.nsem = 5
        self.free_sems = []
        self.free_sems_sw = []
        self.dma_tiles = []
        self.temp_recs = []
        self.nops = 0

    def _deps(self, r, w):
        deps = []
        for t in r:
            if t.w is not None:
                deps.append(t.w)
            deps.extend(t.wx)
        for t in w:
            if t.w is not None:
                deps.append(t.w)
            deps.extend(t.wx)
            deps.extend(t.r.values())
        return deps

    def _wait(self, e, deps):
        wd = self.waited[e]
        best = {}
        for (sem, sid, val, prod) in deps:
            if prod == e and e == 'pe':
                continue
            if wd.get(sid, 0) >= val:
                continue
            if sid not in best or best[sid][1] < val:
                best[sid] = (sem, val)
        for sid, (sem, val) in best.items():
            self.eng[e].wait_ge(sem, val)
            wd[sid] = val

    def op(self, e, fn, r=(), w=()):
        w = [x.advance() if isinstance(x, JunkRot) else x for x in w]
        self._wait(e, self._deps(r, w))
        ins = fn()
        self.cnt[e] += 1
        ins.then_inc(self.sem[e], 1)
        ev = (self.sem[e], e, self.cnt[e], e)
        for t in r:
            t.r[e] = ev
        for t in w:
            t.w = ev
            t.wx = []
            t.r = {}
        self.nops += 1
        return ev

    def dma(self, q, out_ap, in_ap, sb_t, load=True):
        def new_rec():
            pool_ = self.free_sems_sw if q == 'pool' else self.free_sems
            if pool_:
                return pool_.pop()
            r_ = [self.nc.alloc_semaphore("d%d" % self.nsem), "d%d" % self.nsem, 0, q]
            self.nsem += 1
            return r_

        fresh = (q == 'pool' and load and sb_t.dsem is not None and sb_t.w is not None
                 and sb_t.w[3] == 'dma' and not sb_t.r)
        if fresh:
            rec = new_rec()
            self.temp_recs.append(rec)
        else:
            deps = self._deps((), (sb_t,)) if load else self._deps((sb_t,), ())
            self._wait(q, deps)
            if sb_t.dsem is not None and sb_t.dsem[3] != q:
                self.temp_recs.append(sb_t.dsem)
                sb_t.dsem = new_rec()
            if sb_t.dsem is None:
                sb_t.dsem = new_rec()
                self.dma_tiles.append(sb_t)
            rec = sb_t.dsem
        ins = self.eng[q].dma_start(out=out_ap, in_=in_ap)
        rec[2] += 16
        ins.then_inc(rec[0], 16)
        ev = (rec[0], rec[1], rec[2], 'dma')
        if load:
            if fresh:
                sb_t.wx = sb_t.wx + [sb_t.w]
            else:
                sb_t.wx = []
            sb_t.w = ev
            sb_t.r = {}
        else:
            sb_t.r['dma%d' % rec[2]] = ev
        self.dma_evs[rec[1]] = ev
        return ev

    def barrier(self):
        self._wait('sp', list(self.dma_evs.values()))
        evs = [(self.sem[k], k, self.cnt[k], k) for k in self.eng if k != 'sp' and self.cnt[k] > 0]
        self._wait('sp', evs)
        self.eng['sp'].sem_inc(self.sem['sp'], 1)
        self.cnt['sp'] += 1
        ev = (self.sem['sp'], 'sp', self.cnt['sp'], 'sp')
        for k in self.eng:
            if k != 'sp':
                self._wait(k, [ev])
        for t in self.dma_tiles:
            self.temp_recs.append(t.dsem)
            t.dsem = None
        self.dma_tiles = []
        for rec in self.temp_recs:
            (self.free_sems_sw if rec[3] == 'pool' else self.free_sems).append(rec)
        self.temp_recs = []
        self.dma_evs = {}


def pipeline(gens, depth=2, lag=4):
    active = []
    idx = 0
    n = len(gens)
    while idx < n or active:
        if idx < n and len(active) < depth and (not active or active[-1][1] >= lag):
            active.append([gens[idx], 0])
            idx += 1
        for a in list(active):
            try:
                next(a[0])
                a[1] += 1
            except StopIteration:
                active.remove(a)


def pipeline2(items, depth, lag, ahead):
    n = len(items)
    pre_res = {}

    def ensure(k):
        if k < n and k not in pre_res:
            pre_res[k] = items[k][0]()

    active = []
    idx = 0
    while idx < n or active:
        if idx < n and len(active) < depth and (not active or active[-1][1] >= lag):
            for k in range(idx, idx + ahead + 1):
                ensure(k)
            active.append([items[idx][1](pre_res.pop(idx)), 0])
            idx += 1
        for a in list(active):
            try:
                next(a[0])
                a[1] += 1
            except StopIteration:
                active.remove(a)


def build_nc(TT, dbg=False):
    NB = TT // 128
    NOWN = NB // 4
    NTILE = NOWN // 4
    nc = bass.Bass("TRN2", target_bir_lowering=False)
    kb = KB(nc)
    V_, A_, P_, G_ = nc.vector, nc.scalar, nc.tensor, nc.gpsimd

    def din(name, shape, dt=F32):
        return nc.dram_tensor(name, list(shape), dt, kind="ExternalInput")

    x_full = din("x_full", [TT, D]).ap()
    x_own = din("x_own", [NOWN * 128, D]).ap()
    mem = din("mem", [256, D]).ap()
    cmask_d = din("cmask", [128, 512]).ap()
    wsel_d = din("wsel", [128, 4]).ap()
    w_in = din("w_in", [D, D_IN]).ap()
    w_a2 = din("w_gla_a2", [16, 512]).ap()
    w_mem_kv = din("w_mem_kv", [D, 2048]).ap()
    w_o = [din(n, [D, D]).ap() for n in ("w_o_gla", "w_o_dsa", "w_o_mem")]
    w_out = din("w_out", [D, D]).ap()
    w_r1 = din("w_r1", [D, 4]).ap()
    w_r2 = din("w_r2", [D, 16]).ap()
    w_gate = din("w_gate", [16, D, 256]).ap()
    w_up = din("w_up", [16, D, 256]).ap()
    w_down = din("w_down", [16, 256, D]).ap()
    vecs = {}
    for n, L in (("g_mix", 1024), ("g_mem", 1024), ("g_ffn", 1024), ("gla_norm", 256), ("dsa_q_norm", 128),
                 ("dsa_k_norm", 128), ("idx_k_norm", 64), ("mem_q_norm", 256), ("mem_k_norm", 256),
                 ("b_gla_a2", 512), ("b_gate", 3072), ("b_r1", 4), ("b_r2", 16)):
        vecs[n] = din(n, [1, L])
    c_ident = din("c_ident", [128, 128], BF16).ap()
    c_i4 = din("c_i4", [128, 512], BF16).ap()
    c_ones = din("c_ones", [128, 128], BF16).ap()
    c_lmt = din("c_lmt", [128, 128]).ap()
    c_umt = din("c_umt", [128, 128]).ap()
    c_caus4 = din("c_caus4", [128, 512]).ap()
    c_sel16 = din("c_sel16", [16, 2048], BF16).ap()
    c_bis = din("c_bis", [128, NBIS]).ap()
    out_own = nc.dram_tensor("out_own", [NOWN * 128, D], F32, kind="ExternalOutput").ap()
    dbgs = {}
    ogla_scr = nc.dram_tensor("ogla_scr", [NOWN, 128, 1024], BF16, kind="Internal").ap()

    def bcast(name, L):
        return bass.AP(tensor=vecs[name], offset=0, ap=[[0, 128], [1, L]])

    top = ExitStack()

    uid = [0]

    def sb(stack, name, shape, dt):
        uid[0] += 1
        return T(stack.enter_context(nc.sbuf_tensor("%s_%d" % (name, uid[0]), list(shape), dt)))

    def ps(stack, name, shape, dt=F32):
        uid[0] += 1
        return T(stack.enter_context(nc.psum_tensor("%s_%d" % (name, uid[0]), list(shape), dt)))

    def wchunks(src2d, col0, ncol):
        return src2d.rearrange("(c p) n -> p c n", p=128)[:, :, col0:col0 + ncol]

    with top:
        ident = sb(top, "ident", [128, 128], BF16)
        i4 = sb(top, "i4", [128, 512], BF16)
        ones = sb(top, "ones", [128, 128], BF16)
        wsel = sb(top, "wselt", [128, 4], F32)
        memKT = sb(top, "memKT", [128, 8, 256], BF16)
        memV = sb(top, "memV", [128, 2, 1024], BF16)

        for t_, src_ in ((ident, c_ident), (i4, c_i4), (ones, c_ones), (wsel, wsel_d)):
            kb.dma('sp', t_[:], src_, t_)

        def load_gain(t_, name, L, rep=1, scale=None):
            for i in range(rep):
                kb.dma('sp', t_[:, i * L:(i + 1) * L], bcast(name, L), t_)
            if scale is not None:
                kb.op('dve', lambda: V_.tensor_scalar(out=t_[:], in0=t_[:], scalar1=scale, scalar2=None,
                                                      op0=ALU.mult), r=[t_], w=[t_])

        def rstd_from_ss(ss, n, width, tmp):
            kb.op('act', lambda: A_.activation(out=ss, in_=ss, func=AF.Ln, bias=EPS, scale=1.0 / width), r=[tmp], w=[tmp])
            kb.op('act', lambda: A_.activation(out=ss, in_=ss, func=AF.Exp, scale=-0.5), r=[tmp], w=[tmp])

        def norm_only(xt, gain, junk, st, hb):
            kb.op('act', lambda: A_.activation(out=junk[:], in_=xt[:], func=AF.Square, accum_out=st[:, 0:1]),
                  r=[xt], w=[junk, st])
            rstd_from_ss(st[:, 0:1], 1, 1024.0, st)
            kb.op('dve', lambda: V_.scalar_tensor_tensor(out=hb[:], in0=xt[:], scalar=st[:, 0:1], in1=gain[:],
                                                         op0=ALU.mult, op1=ALU.mult), r=[xt, st, gain], w=[hb])

        def transp_T(hb, PT, hT_ap, hT_t):
            for c in range(8):
                kb.op('pe', lambda c=c: P_.transpose(out=PT[:, c, :], in_=hb[:, c * 128:(c + 1) * 128],
                                                     identity=ident[:]), r=[hb, ident], w=[PT])
            kb.op('act', lambda: A_.copy(out=hT_ap, in_=PT[:]), r=[PT], w=[hT_t])

        def norm_T(xt, gain, junk, st, hb, PT, hT_ap, hT_t):
            norm_only(xt, gain, junk, st, hb)
            transp_T(hb, PT, hT_ap, hT_t)

        def proj(hT_ap_fn, hT_t, W, col0, ncol, PJ_t, PJ_ap=None):
            o = PJ_t[:, 0:ncol] if PJ_ap is None else PJ_ap
            for c in range(8):
                kb.op('pe', lambda c=c: P_.matmul(o, lhsT=hT_ap_fn(c), rhs=W[:, c, col0:col0 + ncol],
                                                  start=(c == 0), stop=(c == 7)), r=[hT_t, W], w=[PJ_t])

        with ExitStack() as ph:
            g_mem = sb(ph, "g_memt", [128, 1024], F32)
            mkg4 = sb(ph, "mkg4", [128, 1024], F32)
            xt = sb(ph, "p0_x", [128, 1024], F32)
            junk = JunkRot([sb(ph, "p0_junk%d" % i, [128, 1024], BF16) for i in range(3)])
            st = sb(ph, "p0_st", [128, 8], F32)
            hb = sb(ph, "p0_hb", [128, 1024], BF16)
            mhT = sb(ph, "p0_mhT", [128, 8, 256], BF16)
            wkv = [sb(ph, "p0_wkv%d" % i, [128, 8, 512], BF16) for i in range(2)]
            kfs = [sb(ph, "p0_kf%d" % i, [128, 1024], F32) for i in range(2)]
            kbf = sb(ph, "p0_kbf", [128, 1024], BF16)
            PT = ps(ph, "p0_PT", [128, 8, 128], BF16)
            PJ = [ps(ph, "p0_PJ%d" % i, [128, 512]) for i in range(2)]
            kb.dma('sp', g_mem[:], bcast("g_mem", 1024), g_mem)
            for i in range(4):
                kb.dma('sp', mkg4[:, i * 256:(i + 1) * 256], bcast("mem_k_norm", 256), mkg4)
            for mb in range(2):
                kb.dma('sp', xt[:], mem[mb * 128:(mb + 1) * 128, :], xt)
                norm_T(xt, g_mem, junk, st, hb, PT, mhT[:, :, mb * 128:(mb + 1) * 128], mhT)
            wrot = Rot(wkv)
            pjrot = Rot(PJ)
            for nt in range(4):
                wt = wrot.next()
                kb.dma('pool', wt[:], wchunks(w_mem_kv, nt * 512, 512), wt)
                for mb in range(2):
                    pj = pjrot.next()
                    kf = kfs[mb]
                    proj(lambda c, mb=mb: mhT[:, c, mb * 128:(mb + 1) * 128], mhT, wt, 0, 512, pj)
                    if nt < 2:
                        kb.op('act', lambda pj=pj, nt=nt, kf=kf: A_.copy(out=kf[:, nt * 512:(nt + 1) * 512], in_=pj[:]),
                              r=[pj], w=[kf])
                        if nt == 1:
                            for h in range(4):
                                kb.op('act', lambda h=h, kf=kf: A_.activation(out=junk[:, 0:256], in_=kf[:, h * 256:(h + 1) * 256],
                                                                       func=AF.Square, accum_out=st[:, h:h + 1]),
                                      r=[kf], w=[junk, st])
                            rstd_from_ss(st[:, 0:4], 4, 256.0, st)
                            for h in range(4):
                                kb.op('dve', lambda h=h, kf=kf: V_.scalar_tensor_tensor(
                                    out=kbf[:, h * 256:(h + 1) * 256], in0=kf[:, h * 256:(h + 1) * 256],
                                    scalar=st[:, h:h + 1], in1=mkg4[:, h * 256:(h + 1) * 256],
                                    op0=ALU.mult, op1=ALU.mult), r=[kf, st, mkg4], w=[kbf])
                            for c8 in range(8):
                                kb.op('pe', lambda c8=c8: P_.transpose(out=PT[:, c8, :], in_=kbf[:, c8 * 128:(c8 + 1) * 128],
                                                                       identity=ident[:]), r=[kbf, ident], w=[PT])
                            kb.op('act', lambda mb=mb: A_.copy(out=memKT[:, :, mb * 128:(mb + 1) * 128], in_=PT[:]),
                                  r=[PT], w=[memKT])
                    else:
                        kb.op('act', lambda pj=pj, nt=nt, mb=mb: A_.copy(
                            out=memV[:, mb, (nt - 2) * 512:(nt - 1) * 512], in_=pj[:]), r=[pj], w=[memV])
            kb.barrier()


        with ExitStack() as ph:
            Wg = sb(ph, "g_W", [128, 8, 3088], BF16)
            caus4 = sb(ph, "caus4", [128, 512], F32)
            g_mix = sb(ph, "g_mixt", [128, 1024], F32)
            gnorm = sb(ph, "gnormt", [128, 256], F32)
            kb.dma('sp', caus4[:], c_caus4, caus4)
            load_gain(g_mix, "g_mix", 1024)
            load_gain(gnorm, "gla_norm", 256)
            wa2 = sb(ph, "g_wa2", [17, 512], BF16)
            lmt = sb(ph, "g_lmt", [128, 128], F32)
            umt = sb(ph, "g_umt", [128, 128], F32)
            negc = sb(ph, "g_negc", [128, 2], F32)
            S = sb(ph, "g_S", [128, 1024], F32)
            Ssel = sb(ph, "g_Ssel", [128, 1024], F32)
            Sbf = sb(ph, "g_Sbf", [128, 1024], BF16)
            xts = Rot([sb(ph, "g_x%d" % i, [128, 1024], F32) for i in range(4)])
            junk = JunkRot([sb(ph, "g_junk%d" % i, [128, 256], BF16) for i in range(3)])
            sts = Rot([sb(ph, "g_st%d" % i, [128, 8], F32) for i in range(4)])
            hbs = Rot([sb(ph, "g_hb%d" % i, [128, 1024], BF16) for i in range(4)])
            hTs = Rot([sb(ph, "g_hT%d" % i, [128, 8, 128], BF16) for i in range(4)])
            kfs = Rot([sb(ph, "g_kf%d" % i, [128, 512], F32) for i in range(4)])
            qf = sb(ph, "g_qf", [128, 512], F32)
            vbs = Rot([sb(ph, "g_vb%d" % i, [128, 1024], BF16) for i in range(4)])
            sg = sb(ph, "g_sg", [128, 1024], F32)
            aTs = Rot([sb(ph, "g_aT%d" % i, [17, 128], BF16) for i in range(4)])
            spbs = Rot([sb(ph, "g_sp%d" % i, [128, 512], F32) for i in range(4)])
            E1s = Rot([sb(ph, "g_E1%d" % i, [128, 512], F32) for i in range(4)])
            E2 = sb(ph, "g_E2", [128, 512], F32)
            decs = Rot([sb(ph, "g_dec%d" % i, [128, 4], F32) for i in range(4)])
            kends = Rot([sb(ph, "g_kend%d" % i, [128, 512], BF16) for i in range(4)])
            qdec = sb(ph, "g_qdec", [128, 512], BF16)
            qkT = sb(ph, "g_qkT", [128, 8, 128], BF16)
            attT = sb(ph, "g_attT", [128, 512], BF16)
            ogl = sb(ph, "g_ogl", [128, 1024], BF16)
            ogTs = Rot([sb(ph, "g_ogT%d" % i, [128, 8, 128], BF16) for i in range(2)])
            PJs = Rot([ps(ph, "g_PJ%d" % i, [128, 512]) for i in range(2)])
            PA = ps(ph, "g_PA", [128, 512])
            PB = ps(ph, "g_PB", [128, 512])
            PU = [ps(ph, "g_PU%d" % i, [128, 512]) for i in range(2)]
            PT = ps(ph, "g_PT", [128, 8, 128], BF16)
            PS = ps(ph, "g_PS", [128, 512])

            for i in range(7):
                c0 = i * 512
                n = min(512, 3088 - c0)
                kb.dma('pool', Wg[:, :, c0:c0 + n], wchunks(w_in, c0, n), Wg)
            kb.dma('pool', wa2[0:16, :], w_a2, wa2)
            kb.dma('pool', wa2[16:17, :], vecs["b_gla_a2"].ap(), wa2)
            kb.dma('sp', lmt[:], c_lmt, lmt)
            kb.dma('sp', umt[:], c_umt, umt)
            kb.op('dve', lambda: V_.memset(negc[:], -1.0 / 16.0), w=[negc])
            kb.op('dve', lambda: V_.memset(S[:], 0.0), w=[S])
            for aT_ in aTs.t:
                kb.op('pool', lambda aT_=aT_: G_.memset(aT_[:], 1.0), w=[aT_])

            def g_pre(xsrc):
                def f():
                    xt = xts.next(); st = sts.next(); hb = hbs.next()
                    kb.dma('sp', xt[:], xsrc, xt)
                    norm_only(xt, g_mix, hb, st, hb)
                    return (st, hb)
                return f

            def gla_block(pre, own, p, slot):
                st, hb = pre
                hT = hTs.next()
                kf = kfs.next(); vb = vbs.next(); aT = aTs.next(); spb = spbs.next(); E1 = E1s.next(); E3 = E1
                dec = decs.next(); kend = kends.next(); kinv = kend
                transp_T(hb, PT, hT[:], hT)
                yield
                hf = lambda c: hT[:, c, :]
                for c in range(8):
                    kb.op('pe', lambda c=c: P_.matmul(PS[0:16, 0:128], lhsT=Wg[:, c, OFF['ga']:OFF['ga'] + 16],
                                                      rhs=hT[:, c, :], start=(c == 0), stop=(c == 7)),
                          r=[hT, Wg], w=[PS])
                kb.op('act', lambda: A_.copy(out=aT[0:16, :], in_=PS[0:16, 0:128]), r=[PS], w=[aT])
                pj = PJs.next()
                proj(hf, hT, Wg, OFF['gk'], 512, pj)
                kb.op('dve', lambda: V_.tensor_copy(out=kf[:], in_=pj[:]), r=[pj], w=[kf])
                yield
                kb.op('pe', lambda: P_.matmul(PA[:], lhsT=aT[:], rhs=wa2[:], start=True, stop=True),
                      r=[aT, wa2], w=[PA])
                pj = PJs.next()
                proj(hf, hT, Wg, OFF['gv'], 512, pj)
                kb.op('dve', lambda pj=pj: V_.tensor_copy(out=vb[:, 0:512], in_=pj[:]), r=[pj], w=[vb])
                yield
                kb.op('act', lambda: A_.activation(out=spb[:], in_=PA[:], func=AF.Exp, scale=-1.0), r=[PA], w=[spb])
                kb.op('act', lambda: A_.activation(out=spb[:], in_=spb[:], func=AF.Ln, bias=1.0), r=[spb], w=[spb])
                pj = PJs.next()
                proj(hf, hT, Wg, OFF['gv'] + 512, 512, pj)
                kb.op('dve', lambda pj=pj: V_.tensor_copy(out=vb[:, 512:1024], in_=pj[:]), r=[pj], w=[vb])
                yield
                if own:
                    pj = PJs.next()
                    proj(hf, hT, Wg, OFF['gq'], 512, pj)
                    kb.op('act', lambda: A_.copy(out=qf[:], in_=pj[:]), r=[pj], w=[qf])

                    def g_proj(nt):
                        pj = PJs.next()
                        proj(hf, hT, Wg, OFF['gg'] + nt * 512, 512, pj)
                        kb.op('act', lambda: A_.activation(out=sg[:, nt * 512:(nt + 1) * 512], in_=pj[:], func=AF.Silu),
                              r=[pj], w=[sg])
                yield
                if not own:
                    kb.op('pe', lambda: P_.matmul(PB[:], lhsT=umt[:], rhs=spb[:], start=True, stop=True),
                          r=[umt, spb], w=[PB])
                    for h in range(4):
                        kb.op('pe', lambda h=h: P_.matmul(PS[:, 128 + 2 * h:130 + 2 * h], lhsT=spb[:, h * 128:(h + 1) * 128],
                                                          rhs=negc[:], start=True, stop=True), r=[spb, negc], w=[PS])
                    yield
                    kb.op('act', lambda: A_.activation(out=E3[:], in_=PB[:], func=AF.Exp), r=[PB], w=[E3])
                    kb.op('act', lambda: A_.activation(out=dec[:], in_=PS[:, 128:136:2], func=AF.Exp), r=[PS], w=[dec])
                    kb.op('dve', lambda: V_.tensor_tensor(out=kend[:], in0=kf[:], in1=E3[:], op=ALU.mult),
                          r=[kf, E3], w=[kend])
                    for h in range(4):
                        kb.op('pe', lambda h=h: P_.matmul(PU[h // 2][:, (h % 2) * 256:(h % 2 + 1) * 256],
                                                          lhsT=kend[:, h * 128:(h + 1) * 128],
                                                          rhs=vb[:, h * 256:(h + 1) * 256], start=True, stop=True),
                              r=[kend, vb], w=[PU[h // 2]])
                    yield
                    if p == 0:
                        kb.op('dve', lambda: V_.tensor_scalar(out=Ssel[:], in0=S[:], scalar1=wsel[:, 0:1], scalar2=None,
                                                              op0=ALU.mult), r=[S, wsel], w=[Ssel])
                    else:
                        kb.op('dve', lambda: V_.scalar_tensor_tensor(out=Ssel[:], in0=S[:], scalar=wsel[:, p:p + 1],
                                                                     in1=Ssel[:], op0=ALU.mult, op1=ALU.add),
                              r=[S, wsel, Ssel], w=[Ssel])
                    for h in range(4):
                        kb.op('dve', lambda h=h: V_.scalar_tensor_tensor(
                            out=S[:, h * 256:(h + 1) * 256], in0=S[:, h * 256:(h + 1) * 256], scalar=dec[:, h:h + 1],
                            in1=PU[h // 2][:, (h % 2) * 256:(h % 2 + 1) * 256], op0=ALU.mult, op1=ALU.add),
                            r=[S, dec, PU[h // 2]], w=[S])
                else:
                    kb.op('pe', lambda: P_.matmul(PB[:], lhsT=lmt[:], rhs=spb[:], start=True, stop=True),
                          r=[lmt, spb], w=[PB])
                    yield
                    kb.op('act', lambda: A_.activation(out=E1[:], in_=PB[:], func=AF.Exp), r=[PB], w=[E1])
                    kb.op('act', lambda: A_.activation(out=E2[:], in_=PB[:], func=AF.Exp, scale=-1.0), r=[PB], w=[E2])
                    g_proj(0)
                    kb.op('dve', lambda: V_.scalar_tensor_tensor(out=qdec[:], in0=qf[:], scalar=128.0 ** -0.5, in1=E1[:],
                                                                 op0=ALU.mult, op1=ALU.mult), r=[qf, E1], w=[qdec])
                    kb.op('dve', lambda: V_.tensor_tensor(out=kinv[:], in0=kf[:], in1=E2[:], op=ALU.mult),
                          r=[kf, E2], w=[kinv])
                    g_proj(1)
                    for h in range(4):
                        kb.op('pe', lambda h=h: P_.transpose(out=PT[:, h, :], in_=qdec[:, h * 128:(h + 1) * 128],
                                                             identity=ident[:]), r=[qdec, ident], w=[PT])
                        kb.op('pe', lambda h=h: P_.transpose(out=PT[:, 4 + h, :], in_=kinv[:, h * 128:(h + 1) * 128],
                                                             identity=ident[:]), r=[kinv, ident], w=[PT])
                    yield
                    kb.op('act', lambda: A_.copy(out=qkT[:], in_=PT[:]), r=[PT], w=[qkT])
                    for h in range(4):
                        kb.op('pe', lambda h=h: P_.matmul(PB[:, h * 128:(h + 1) * 128], lhsT=qkT[:, 4 + h, :],
                                                          rhs=qkT[:, h, :], start=True, stop=True), r=[qkT], w=[PB])
                    kb.op('dve', lambda: V_.tensor_tensor(out=attT[:], in0=PB[:], in1=caus4[:], op=ALU.mult),
                          r=[PB, caus4], w=[attT])
                    yield
                    kb.op('pool', lambda: G_.tensor_copy(out=Sbf[:], in_=Ssel[:]), r=[Ssel], w=[Sbf])
                    for h in range(4):
                        o_ap = PU[h // 2][:, (h % 2) * 256:(h % 2 + 1) * 256]
                        kb.op('pe', lambda h=h, o_ap=o_ap: P_.matmul(o_ap, lhsT=attT[:, h * 128:(h + 1) * 128],
                                                                     rhs=vb[:, h * 256:(h + 1) * 256], start=True, stop=False),
                              r=[attT, vb], w=[PU[h // 2]])
                        kb.op('pe', lambda h=h, o_ap=o_ap: P_.matmul(o_ap, lhsT=qkT[:, h, :],
                                                                     rhs=Sbf[:, h * 256:(h + 1) * 256], start=False, stop=True),
                              r=[qkT, Sbf], w=[PU[h // 2]])
                    for h in range(4):
                        kb.op('act', lambda h=h: A_.activation(out=junk[:, 0:256], in_=PU[h // 2][:, (h % 2) * 256:(h % 2 + 1) * 256],
                                                               func=AF.Square, accum_out=st[:, 4 + h:5 + h]),
                              r=[PU[h // 2]], w=[junk, st])
                    yield
                    rstd_from_ss(st[:, 4:8], 4, 256.0, st)
                    for h in range(4):
                        kb.op('pool', lambda h=h: G_.tensor_tensor(out=sg[:, h * 256:(h + 1) * 256], in0=sg[:, h * 256:(h + 1) * 256],
                                                                   in1=gnorm[:], op=ALU.mult), r=[sg, gnorm], w=[sg])
                    for h in range(4):
                        kb.op('dve', lambda h=h: V_.scalar_tensor_tensor(
                            out=ogl[:, h * 256:(h + 1) * 256], in0=PU[h // 2][:, (h % 2) * 256:(h % 2 + 1) * 256],
                            scalar=st[:, 4 + h:5 + h], in1=sg[:, h * 256:(h + 1) * 256], op0=ALU.mult, op1=ALU.mult),
                            r=[PU[h // 2], st, sg], w=[ogl])
                    for c in range(8):
                        kb.op('pe', lambda c=c: P_.transpose(out=PT[:, c, :], in_=ogl[:, c * 128:(c + 1) * 128],
                                                             identity=ident[:]), r=[ogl, ident], w=[PT])
                    ogT = ogTs.next()
                    kb.op('act', lambda: A_.copy(out=ogT[:], in_=PT[:]), r=[PT], w=[ogT])
                    kb.dma('sp', ogla_scr[slot], ogT[:].rearrange("p a b -> p (a b)"), ogT, load=False)

            items = []
            for m in range(NOWN):
                for p in range(4):
                    blk = 4 * m + p
                    items.append((g_pre(x_full[blk * 128:(blk + 1) * 128, :]),
                                  lambda pre, p=p, m=m: gla_block(pre, False, p, m)))
                items.append((g_pre(x_own[m * 128:(m + 1) * 128, :]), lambda pre, m=m: gla_block(pre, True, 0, m)))
            pipeline2(items, depth=2, lag=4, ahead=2)
            kb.barrier()

        KT = sb(top, "KT", [128, 2, TT], BF16)
        VV = sb(top, "VV", [128, NB, 256], BF16)
        kidxT = sb(top, "kidxT", [128, TT // 2], BF16)
        with ExitStack() as ph:
            Wk = sb(ph, "k_W", [128, 8, 576], BF16)
            g_mix = sb(ph, "g_mixt", [128, 1024], F32)
            kg2 = sb(ph, "kg2", [128, 256], F32)
            ikg = sb(ph, "ikg", [128, 128], F32)
            load_gain(g_mix, "g_mix", 1024)
            load_gain(kg2, "dsa_k_norm", 128, rep=2)
            load_gain(ikg, "idx_k_norm", 64, rep=2)
            xts = Rot([sb(ph, "k_x%d" % i, [128, 1024], F32) for i in range(4)])
            junk = JunkRot([sb(ph, "k_junk%d" % i, [128, 1024], BF16) for i in range(3)])
            sts = Rot([sb(ph, "k_st%d" % i, [128, 8], F32) for i in range(4)])
            hbs = Rot([sb(ph, "k_hb%d" % i, [128, 1024], BF16) for i in range(4)])
            hTs = Rot([sb(ph, "k_hT%d" % i, [128, 8, 128], BF16) for i in range(3)])
            knbs = Rot([sb(ph, "k_knb%d" % i, [128, 256], BF16) for i in range(3)])
            ikbs = Rot([sb(ph, "k_ikb%d" % i, [128, 128], BF16) for i in range(3)])
            PJs = Rot([ps(ph, "k_PJ%d" % i, [128, 512]) for i in range(2)])
            PI = ps(ph, "k_PI", [128, 512])
            PT = ps(ph, "k_PT", [128, 8, 128], BF16)
            PT2 = ps(ph, "k_PT2", [128, 4, 128], BF16)
            kb.dma('pool', Wk[:, :, 0:512], wchunks(w_in, OFF['dk'], 512), Wk)
            kb.dma('pool', Wk[:, :, 512:576], wchunks(w_in, OFF['ik'], 64), Wk)
            def k_pre(blk):
                def f():
                    xt = xts.next(); st = sts.next(); hb = hbs.next()
                    kb.dma('sp', xt[:], x_full[blk * 128:(blk + 1) * 128, :], xt)
                    norm_only(xt, g_mix, junk, st, hb)
                    return (st, hb)
                return f

            def k_block(blk, pre):
                st, hb = pre
                hT = hTs.next(); knb = knbs.next(); ikb = ikbs.next()
                transp_T(hb, PT, hT[:], hT)
                yield
                hf = lambda c, hT=hT: hT[:, c, :]
                pj = PJs.next()
                proj(hf, hT, Wk, 0, 512, pj)
                proj(hf, hT, Wk, 512, 64, PI)
                yield
                kb.op('act', lambda pj=pj, blk=blk: A_.copy(out=VV[:, blk, :], in_=pj[:, 256:512]), r=[pj], w=[])
                for g in range(2):
                    kb.op('act', lambda g=g, pj=pj, st=st: A_.activation(out=junk[:, 0:128], in_=pj[:, g * 128:(g + 1) * 128],
                                                                         func=AF.Square, accum_out=st[:, 4 + g:5 + g]),
                          r=[pj], w=[junk, st])
                kb.op('act', lambda st=st: A_.activation(out=junk[:, 0:64], in_=PI[:, 0:64], func=AF.Square,
                                                         accum_out=st[:, 6:7]), r=[PI], w=[junk, st])
                yield
                rstd_from_ss(st[:, 4:6], 2, 128.0, st)
                rstd_from_ss(st[:, 6:7], 1, 64.0, st)
                for g in range(2):
                    kb.op('dve', lambda g=g, pj=pj, st=st: V_.scalar_tensor_tensor(
                        out=knb[:, g * 128:(g + 1) * 128], in0=pj[:, g * 128:(g + 1) * 128], scalar=st[:, 4 + g:5 + g],
                        in1=kg2[:, g * 128:(g + 1) * 128], op0=ALU.mult, op1=ALU.mult), r=[pj, st, kg2], w=[knb])
                for hh in range(2):
                    kb.op('dve', lambda st=st, hh=hh: V_.scalar_tensor_tensor(
                        out=ikb[:, hh * 64:(hh + 1) * 64], in0=PI[:, 0:64], scalar=st[:, 6:7], in1=ikg[:, 0:64],
                        op0=ALU.mult, op1=ALU.mult), r=[PI, st, ikg], w=[ikb])
                yield
                for g in range(2):
                    kb.op('pe', lambda g=g: P_.transpose(out=PT2[:, g, :], in_=knb[:, g * 128:(g + 1) * 128],
                                                         identity=ident[:]), r=[knb, ident], w=[PT2])
                kb.op('pe', lambda: P_.transpose(out=PT2[:, 2, :], in_=ikb[:], identity=ident[:]),
                      r=[ikb, ident], w=[PT2])
                kb.op('act', lambda blk=blk: A_.copy(out=KT[:, :, blk * 128:(blk + 1) * 128], in_=PT2[:, 0:2, :]),
                      r=[PT2], w=[])
                hp = 0 if blk < NB // 2 else 64
                bl = blk if blk < NB // 2 else blk - NB // 2
                kb.op('act', lambda bl=bl, hp=hp: A_.copy(out=kidxT[hp:hp + 64, bl * 128:(bl + 1) * 128],
                                                          in_=PT2[hp:hp + 64, 2, :]), r=[PT2], w=[])
            pipeline2([(k_pre(blk), lambda pre, blk=blk: k_block(blk, pre)) for blk in range(NB)], depth=2, lag=2, ahead=2)
            if dbg:
                dbgs['KT'] = nc.dram_tensor("dbg_KT", [128, 2 * TT], BF16, kind="ExternalOutput")
                kb.dma('sp', dbgs['KT'].ap(), KT[:].rearrange("p a b -> p (a b)"), KT, load=False)
                dbgs['kidxT'] = nc.dram_tensor("dbg_kidxT", [128, TT // 2], BF16, kind="ExternalOutput")
                kb.dma('sp', dbgs['kidxT'].ap(), kidxT[:], kidxT, load=False)
            kb.barrier()

        U8 = mybir.dt.uint8
        for j in range(NTILE):
            with ExitStack() as tl:
                hTo = sb(tl, "c_hTo", [128, 8, 512], BF16)
                OdT = sb(tl, "c_OdT", [128, 8, 512], BF16)
                with ExitStack() as ph:
                    g_mix = sb(ph, "g_mixt", [128, 1024], F32)
                    xt = sb(ph, "c0_x", [128, 1024], F32)
                    junk = JunkRot([sb(ph, "c0_junk%d" % i, [128, 1024], BF16) for i in range(3)])
                    st = sb(ph, "c0_st", [128, 8], F32)
                    hbs4 = [sb(ph, "c0_hb%d" % i, [128, 1024], BF16) for i in range(4)]
                    xts2 = Rot([xt, sb(ph, "c0_x2", [128, 1024], F32)])
                    sts4 = [sb(ph, "c0_st%d" % i, [128, 8], F32) for i in range(4)]
                    PTs = Rot([ps(ph, "c0_PT%d" % i, [128, 8, 128], BF16) for i in range(2)])
                    load_gain(g_mix, "g_mix", 1024)
                    for s in range(4):
                        m = 4 * j + s
                        xt_ = xts2.next()
                        kb.dma('sp', xt_[:], x_own[m * 128:(m + 1) * 128, :], xt_)
                        norm_only(xt_, g_mix, junk, sts4[s], hbs4[s])
                    for s in range(4):
                        transp_T(hbs4[s], PTs.next(), hTo[:, :, s * 128:(s + 1) * 128], hTo)
                    kb.barrier()
                with ExitStack() as c1:
                    QT = sb(c1, "c1_QT", [128, 8, 512], BF16)
                    IQT = sb(c1, "c1_IQT", [128, 8, 512], BF16)
                    wt = sb(c1, "c1_wt", [128, 4, 8], F32)
                    wabs = sb(c1, "c1_wabs", [128, 4, 8], F32)
                    wsgn = sb(c1, "c1_wsgn", [128, 4, 8], F32)
                    with ExitStack() as ph:
                        qg8 = sb(ph, "qg8", [128, 1024], F32)
                        wbufs = Rot([sb(ph, "c1a_w%d" % i, [128, 8, 512], BF16) for i in range(2)])
                        wiw = sb(ph, "c1a_wiw", [128, 8, 8], BF16)
                        qn = sb(ph, "c1a_qn", [128, 512], BF16)
                        iqd = sb(ph, "c1a_iqd", [128, 8, 2, 64], BF16)
                        junk = JunkRot([sb(ph, "c1a_junk%d" % i, [128, 128], BF16) for i in range(3)])
                        st = sb(ph, "c1a_st", [128, 8], F32)
                        PJs = Rot([ps(ph, "c1a_PJ%d" % i, [128, 512]) for i in range(2)])
                        PJw = ps(ph, "c1a_PJw", [128, 512])
                        PT = ps(ph, "c1a_PT", [128, 8, 128], BF16)
                        load_gain(qg8, "dsa_q_norm", 128, rep=8, scale=128.0 ** -0.5)
                        kb.dma('pool', wiw[:], wchunks(w_in, OFF['iw'], 8), wiw)
                        pend = None
                        for wi, col0 in enumerate((OFF['dq'], OFF['dq'] + 512, OFF['iq'])):
                            wb = wbufs.next()
                            kb.dma('pool', wb[:], wchunks(w_in, col0, 512), wb)
                            for s in range(4):
                                hf = lambda c, s=s: hTo[:, c, s * 128:(s + 1) * 128]
                                pj = PJs.next()
                                proj(hf, hTo, wb, 0, 512, pj)
                                if pend is not None:
                                    pend()
                                def post(wi=wi, s=s, pj=pj, hf=hf):
                                    if wi < 2:
                                        for hh in range(4):
                                            kb.op('act', lambda hh=hh, pj=pj: A_.activation(
                                                out=junk[:], in_=pj[:, hh * 128:(hh + 1) * 128], func=AF.Square,
                                                accum_out=st[:, hh:hh + 1]), r=[pj], w=[junk, st])
                                        rstd_from_ss(st[:, 0:4], 4, 128.0, st)
                                        for hh in range(4):
                                            kb.op('dve', lambda hh=hh, pj=pj, wi=wi: V_.scalar_tensor_tensor(
                                                out=qn[:, hh * 128:(hh + 1) * 128], in0=pj[:, hh * 128:(hh + 1) * 128],
                                                scalar=st[:, hh:hh + 1], in1=qg8[:, (wi * 4 + hh) * 128:(wi * 4 + hh + 1) * 128],
                                                op0=ALU.mult, op1=ALU.mult), r=[pj, st, qg8], w=[qn])
                                        for hh in range(4):
                                            kb.op('pe', lambda hh=hh: P_.transpose(out=PT[:, hh, :], in_=qn[:, hh * 128:(hh + 1) * 128],
                                                                                   identity=ident[:]), r=[qn, ident], w=[PT])
                                        kb.op('act', lambda wi=wi, s=s: A_.copy(out=QT[:, wi * 4:wi * 4 + 4, s * 128:(s + 1) * 128],
                                                                                in_=PT[:, 0:4, :]), r=[PT], w=[QT])
                                    else:
                                        pv = pj[:, :].rearrange("p (h d) -> p h d", h=8)
                                        for dd in range(2):
                                            kb.op('act', lambda dd=dd, pv=pv: A_.copy(out=iqd[:, :, dd, :], in_=pv), r=[pj], w=[iqd])
                                        for h in range(8):
                                            kb.op('pe', lambda h=h: P_.transpose(
                                                out=PT[:, h, :], in_=iqd[:, h, :, :].rearrange("p a b -> p (a b)"),
                                                identity=ident[:]), r=[iqd, ident], w=[PT])
                                        kb.op('act', lambda s=s: A_.copy(out=IQT[:, :, s * 128:(s + 1) * 128], in_=PT[:]),
                                              r=[PT], w=[IQT])
                                        pj2 = PJw
                                        proj(hf, hTo, wiw, 0, 8, pj2)
                                        kb.op('dve', lambda s=s, pj2=pj2: V_.tensor_scalar(
                                            out=wt[:, s, :], in0=pj2[:, 0:8], scalar1=(8.0 ** -0.5) * (64.0 ** -0.5), scalar2=None,
                                            op0=ALU.mult), r=[pj2], w=[wt])
                                        kb.op('dve', lambda s=s: V_.tensor_scalar(out=wsgn[:, s, :], in0=wt[:, s, :], scalar1=0.0, scalar2=2.0,
                                                                                   op0=ALU.is_ge, op1=ALU.mult), r=[wt], w=[wsgn])
                                        kb.op('dve', lambda s=s: V_.tensor_scalar(out=wsgn[:, s, :], in0=wsgn[:, s, :], scalar1=-1.0, scalar2=None,
                                                                                   op0=ALU.add), r=[wsgn], w=[wsgn])
                                        kb.op('dve', lambda s=s: V_.tensor_tensor(out=wabs[:, s, :], in0=wt[:, s, :], in1=wsgn[:, s, :], op=ALU.mult),
                                              r=[wt, wsgn], w=[wabs])
                                pend = post
                        pend()
                        kb.barrier()
                    with ExitStack() as ph:
                        NKT = 4 * (j + 1)
                        scs = [sb(ph, "c1b_sc%d" % i, [128, NKT * 512], F32) for i in range(2)]
                        jk = sb(ph, "c1b_jk", [128, 2048], U8)
                        cmask = sb(ph, "cmaskt", [128, 512], F32)
                        cbis = sb(ph, "cbis", [128, NBIS], F32)
                        Rs = Rot([sb(ph, "c1b_R%d" % i, [128, 512], BF16) for i in range(2)])
                        Ps = Rot([sb(ph, "c1b_P%d" % i, [128, 512], BF16) for i in range(3)])
                        nmt = [sb(ph, "c1b_nm%d" % i, [128, 512], BF16) for i in range(NKT)]
                        Dm = sb(ph, "c1b_Dm", [128, 8, 128], BF16)
                        rec = sb(ph, "c1b_rec", [128, 512], F32)
                        bss = [sb(ph, "c1b_bs%d" % i, [128, 8], F32) for i in range(2)]
                        dl = sb(ph, "c1b_dl", [128, NBIS], F32)
                        PSIs = Rot([ps(ph, "c1b_PI%d" % i, [128, 512]) for i in range(2)])
                        PSC = ps(ph, "c1b_PC", [128, 512])
                        PSSs = Rot([ps(ph, "c1b_PS%d" % i, [128, 512]) for i in range(3)])
                        PSO = ps(ph, "c1b_PO", [128, 512])
                        PSM = ps(ph, "c1b_PM", [128, 512])
                        kb.dma('sp', cmask[:], cmask_d, cmask)
                        kb.dma('sp', cbis[:], c_bis, cbis)

                        def c1_index(s):
                            m = 4 * j + s
                            scores = scs[s % 2]
                            tok = slice(s * 128, (s + 1) * 128)
                            pend = None
                            for kt in range(m + 1):
                                hp = 0 if kt * 512 < TT // 2 else 64
                                kc = kt * 512 - (0 if hp == 0 else TT // 2)
                                for h in range(8):
                                    psi = PSIs.next()
                                    kb.op('pe', lambda: P_.matmul(
                                        psi[:], lhsT=IQT[hp:hp + 64, h, tok], rhs=kidxT[hp:hp + 64, kc:kc + 512],
                                        start=True, stop=True), r=[IQT, kidxT], w=[psi])
                                    R = Rs.next()
                                    kb.op('act', lambda: A_.activation(out=R[:], in_=psi[:], func=AF.Relu, scale=wabs[:, s, h:h + 1]),
                                          r=[psi, wabs], w=[R])
                                    if pend is not None:
                                        pend()
                                    def acc(h=h, R=R, kt=kt):
                                        kb.op('pe', lambda: P_.matmul(PSC[:], lhsT=Dm[:, h, :], rhs=R[:], start=(h == 0), stop=(h == 7)),
                                              r=[Dm, R], w=[PSC])
                                        if h == 7:
                                            kb.op('act', lambda: A_.copy(out=scores[:, kt * 512:(kt + 1) * 512], in_=PSC[:]),
                                                  r=[PSC], w=[scores])
                                    pend = acc
                                    if h % 4 == 3:
                                        yield 2.7
                            pend()
                            yield 0.5

                        def c1_dm(s):
                            for h in range(8):
                                kb.op('dve', lambda: V_.tensor_scalar(out=Dm[:, h, :], in0=ident[:], scalar1=wsgn[:, s, h:h + 1],
                                                                      scalar2=None, op0=ALU.mult), r=[ident, wsgn], w=[Dm])

                        def c1_bisect(s):
                            m = 4 * j + s
                            nk = 512 * (m + 1)
                            scores = scs[s % 2]
                            bs = bss[s % 2]
                            kb.op('dve', lambda: V_.tensor_reduce(out=bs[:, 0:1], in_=scores[:, 0:nk], axis=AX.X, op=ALU.max,
                                                                  apply_absolute_value=True), r=[scores], w=[bs])
                            kb.op('dve', lambda: V_.tensor_tensor(out=scores[:, m * 512:(m + 1) * 512],
                                                                  in0=scores[:, m * 512:(m + 1) * 512], in1=cmask[:], op=ALU.add),
                                  r=[scores, cmask], w=[scores])
                            kb.op('dve', lambda: V_.tensor_scalar(out=dl[:], in0=cbis[:], scalar1=bs[:, 0:1], scalar2=None,
                                                                  op0=ALU.mult), r=[cbis, bs], w=[dl])
                            kb.op('dve', lambda: V_.tensor_scalar(out=bs[:, 1:2], in0=bs[:, 0:1], scalar1=-1.0, scalar2=None,
                                                                  op0=ALU.mult), r=[bs], w=[bs])
                            yield nk / 850.0 + 1.5
                            for k in range(NBIS):
                                kb.op('dve', lambda: V_.tensor_tensor(out=bs[:, 2:3], in0=bs[:, 1:2], in1=dl[:, k:k + 1],
                                                                      op=ALU.add), r=[bs, dl], w=[bs])
                                for ci, c0 in enumerate(range(0, nk, 2048)):
                                    cw = min(2048, nk - c0)
                                    kb.op('dve', lambda: V_.tensor_scalar(
                                        out=jk[:, 0:cw], in0=scores[:, c0:c0 + cw], scalar1=bs[:, 2:3],
                                        scalar2=(None if ci == 0 else bs[:, 3:4]), op0=ALU.is_ge, op1=ALU.add,
                                        accum_out=bs[:, 3:4]), r=[scores, bs], w=[jk, bs])
                                kb.op('dve', lambda: V_.scalar_tensor_tensor(out=bs[:, 4:5], in0=bs[:, 3:4], scalar=255.5,
                                                                             in1=dl[:, k:k + 1], op0=ALU.is_ge, op1=ALU.mult),
                                      r=[bs, dl], w=[bs])
                                kb.op('dve', lambda: V_.tensor_tensor(out=bs[:, 1:2], in0=bs[:, 1:2], in1=bs[:, 4:5], op=ALU.add),
                                      r=[bs], w=[bs])
                                yield nk / 850.0 + 1.3

                        def c1_nm(s):
                            m = 4 * j + s
                            scores = scs[s % 2]
                            bs = bss[s % 2]
                            for kt in range(m + 1):
                                kb.op('dve', lambda: V_.tensor_scalar(
                                    out=nmt[kt][:], in0=scores[:, kt * 512:(kt + 1) * 512], scalar1=bs[:, 1:2], scalar2=NEG,
                                    op0=ALU.is_lt, op1=ALU.mult), r=[scores, bs], w=[nmt[kt]])

                        def c1_attn(s):
                            m = 4 * j + s
                            tok = slice(s * 128, (s + 1) * 128)
                            nkb = 4 * (m + 1)
                            for g in range(2):
                                pend = None
                                for kb_ in range(nkb):
                                    nm = nmt[kb_ // 4]
                                    pss = PSSs.next()
                                    kb.op('pe', lambda: P_.matmul(
                                        pss[:], lhsT=nm[:, (kb_ % 4) * 128:(kb_ % 4 + 1) * 128], rhs=i4[:], start=True, stop=False),
                                        r=[nm, i4], w=[pss])
                                    kb.op('pe', lambda: P_.matmul(
                                        pss[:], lhsT=KT[:, g, kb_ * 128:(kb_ + 1) * 128], rhs=QT[:, 4 * g:4 * g + 4, tok],
                                        start=False, stop=True), r=[KT, QT], w=[pss])
                                    Pb = Ps.next()
                                    kb.op('act', lambda: A_.activation(out=Pb[:], in_=pss[:], func=AF.Exp), r=[pss], w=[Pb])
                                    if pend is not None:
                                        pend()
                                    def pv(kb_=kb_, Pb=Pb, g=g):
                                        kb.op('pe', lambda: P_.matmul(
                                            PSO[:], lhsT=VV[:, kb_, g * 128:(g + 1) * 128], rhs=Pb[:],
                                            start=(kb_ == 0), stop=(kb_ == nkb - 1)), r=[VV, Pb], w=[PSO])
                                        kb.op('pe', lambda: P_.matmul(
                                            PSM[:], lhsT=ones[:], rhs=Pb[:], start=(kb_ == 0), stop=(kb_ == nkb - 1)),
                                            r=[ones, Pb], w=[PSM])
                                    pend = pv
                                    yield 1.1
                                pend()
                                kb.op('dve', lambda: V_.reciprocal(out=rec[:], in_=PSM[:]), r=[PSM], w=[rec])
                                kb.op('dve', lambda: V_.tensor_tensor(
                                    out=OdT[:, 4 * g:4 * g + 4, tok], in0=PSO[:, :].rearrange("p (h q) -> p h q", h=4),
                                    in1=rec[:, :].rearrange("p (h q) -> p h q", h=4), op=ALU.mult), r=[PSO, rec], w=[OdT])
                                yield 1.0

                        def weave3(gens):
                            acc_t = [0.0 for _ in gens]
                            live = [g is not None for g in gens]
                            while any(live):
                                i = min((k for k in range(len(gens)) if live[k]), key=lambda k: acc_t[k])
                                try:
                                    acc_t[i] += next(gens[i])
                                except StopIteration:
                                    live[i] = False

                        c1_dm(0)
                        for t in range(-2, 4):
                            gl = []
                            if 0 <= t + 2 < 4:
                                gl.append(c1_index(t + 2))
                            if 0 <= t + 1 < 4:
                                gl.append(c1_bisect(t + 1))
                            if 0 <= t < 4:
                                gl.append(c1_attn(t))
                            weave3(gl)
                            if 0 <= t + 3 < 4:
                                c1_dm(t + 3)
                            if 0 <= t + 1 < 4:
                                c1_nm(t + 1)
                        if dbg and j == 0:
                            dbgs['OdT'] = nc.dram_tensor("dbg_OdT", [128, 8 * 512], BF16, kind="ExternalOutput")
                            kb.dma('sp', dbgs['OdT'].ap(), OdT[:].rearrange("p a b -> p (a b)"), OdT, load=False)
                        kb.barrier()
                OmT = sb(tl, "c_OmT", [128, 8, 512], BF16)
                x1 = sb(tl, "c_x1", [128, 4, 1024], F32)
                with ExitStack() as ph:
                    mqg4 = sb(ph, "mqg4", [128, 1024], F32)
                    wbufs = Rot([sb(ph, "c2_w%d" % i, [128, 8, 512], BF16) for i in range(2)])
                    mqf = sb(ph, "c2_mqf", [128, 4, 1024], F32)
                    mqb = sb(ph, "c2_mqb", [128, 1024], BF16)
                    MQT = sb(ph, "c2_MQT", [128, 8, 512], BF16)
                    junk = JunkRot([sb(ph, "c2_junk%d" % i, [128, 256], BF16) for i in range(3)])
                    st = sb(ph, "c2_st", [128, 8], F32)
                    Pms = Rot([sb(ph, "c2_P%d" % i, [128, 512], BF16) for i in range(2)])
                    rec = sb(ph, "c2_rec", [128, 512], F32)
                    PJs = Rot([ps(ph, "c2_PJ%d" % i, [128, 512]) for i in range(2)])
                    PT = ps(ph, "c2_PT", [128, 8, 128], BF16)
                    PSs = Rot([ps(ph, "c2_PS%d" % i, [128, 512]) for i in range(2)])
                    PO2 = [ps(ph, "c2_PO%d" % i, [128, 512]) for i in range(2)]
                    PM2 = ps(ph, "c2_PM", [128, 512])
                    load_gain(mqg4, "mem_q_norm", 256, rep=4, scale=256.0 ** -0.5)
                    for nt in range(2):
                        wb = wbufs.next()
                        kb.dma('pool', wb[:], wchunks(w_in, OFF['mq'] + nt * 512, 512), wb)
                        for s in range(4):
                            pj = PJs.next()
                            proj(lambda c, s=s: hTo[:, c, s * 128:(s + 1) * 128], hTo, wb, 0, 512, pj)
                            kb.op('act', lambda pj=pj, s=s, nt=nt: A_.copy(out=mqf[:, s, nt * 512:(nt + 1) * 512], in_=pj[:]),
                                  r=[pj], w=[mqf])
                    for s in range(4):
                        for h in range(4):
                            kb.op('act', lambda h=h, s=s: A_.activation(out=junk[:], in_=mqf[:, s, h * 256:(h + 1) * 256],
                                                                        func=AF.Square, accum_out=st[:, h:h + 1]),
                                  r=[mqf], w=[junk, st])
                        rstd_from_ss(st[:, 0:4], 4, 256.0, st)
                        for h in range(4):
                            kb.op('dve', lambda h=h, s=s: V_.scalar_tensor_tensor(
                                out=mqb[:, h * 256:(h + 1) * 256], in0=mqf[:, s, h * 256:(h + 1) * 256], scalar=st[:, h:h + 1],
                                in1=mqg4[:, h * 256:(h + 1) * 256], op0=ALU.mult, op1=ALU.mult), r=[mqf, st, mqg4], w=[mqb])
                        for c8 in range(8):
                            kb.op('pe', lambda c8=c8: P_.transpose(out=PT[:, c8, :], in_=mqb[:, c8 * 128:(c8 + 1) * 128],
                                                                   identity=ident[:]), r=[mqb, ident], w=[PT])
                        kb.op('act', lambda s=s: A_.copy(out=MQT[:, :, s * 128:(s + 1) * 128], in_=PT[:]), r=[PT], w=[MQT])
                    for h in range(4):
                        pend = None
                        for mc in range(2):
                            pss = PSs.next()
                            for c in range(2):
                                kb.op('pe', lambda pss=pss, h=h, mc=mc, c=c: P_.matmul(
                                    pss[:], lhsT=memKT[:, h * 2 + c, mc * 128:(mc + 1) * 128], rhs=MQT[:, h * 2 + c, :],
                                    start=(c == 0), stop=(c == 1)), r=[memKT, MQT], w=[pss])
                            Pm = Pms.next()
                            kb.op('act', lambda pss=pss, Pm=Pm: A_.activation(out=Pm[:], in_=pss[:], func=AF.Exp),
                                  r=[pss], w=[Pm])
                            if pend is not None:
                                pend()
                            def pvm(Pm=Pm, h=h, mc=mc):
                                for vc in range(2):
                                    kb.op('pe', lambda vc=vc: P_.matmul(
                                        PO2[vc][:], lhsT=memV[:, mc, h * 256 + vc * 128:h * 256 + (vc + 1) * 128], rhs=Pm[:],
                                        start=(mc == 0), stop=(mc == 1)), r=[memV, Pm], w=[PO2[vc]])
                                kb.op('pe', lambda: P_.matmul(PM2[:], lhsT=ones[:], rhs=Pm[:], start=(mc == 0),
                                                              stop=(mc == 1)), r=[ones, Pm], w=[PM2])
                            pend = pvm
                        pend()
                        kb.op('dve', lambda: V_.reciprocal(out=rec[:], in_=PM2[:]), r=[PM2], w=[rec])
                        for vc in range(2):
                            kb.op('dve', lambda h=h, vc=vc: V_.tensor_tensor(out=OmT[:, h * 2 + vc, :], in0=PO2[vc][:], in1=rec[:],
                                                                             op=ALU.mult), r=[PO2[vc], rec], w=[OmT])
                    kb.barrier()
                with ExitStack() as ph:
                    wbufs = Rot([sb(ph, "c3_w%d" % i, [128, 8, 1024], BF16) for i in range(2)])
                    bgs = Rot([sb(ph, "c3_bg%d" % i, [1, 512], BF16) for i in range(2)])
                    gt = sb(ph, "c3_gt", [128, 512], F32)
                    tmp = sb(ph, "c3_tmp", [128, 512], F32)
                    mbf = sb(ph, "c3_mbf", [128, 1024], BF16)
                    mT = sb(ph, "c3_mT", [128, 8, 128], BF16)
                    xo = sb(ph, "c3_xo", [128, 1024], F32)
                    ogs = [sb(ph, "c3_og%d" % i, [128, 8, 128], BF16) for i in range(4)]
                    for s in range(4):
                        kb.dma('sp', ogs[s][:].rearrange("p a b -> p (a b)"), ogla_scr[4 * j + s], ogs[s])
                    PGs = Rot([ps(ph, "c3_PG%d" % i, [128, 512]) for i in range(2)])
                    PPs = Rot([ps(ph, "c3_PP%d" % i, [128, 512]) for i in range(2)])
                    PT = ps(ph, "c3_PT", [128, 8, 128], BF16)
                    def c3_load(br, nt):
                        wbuf = wbufs.next(); bg = bgs.next()
                        kb.dma('pool', wbuf[:, :, 0:512], wchunks(w_in, OFF['gates'] + br * 1024 + nt * 512, 512), wbuf)
                        kb.dma('pool', wbuf[:, :, 512:1024], wchunks(w_o[br], nt * 512, 512), wbuf)
                        kb.dma('pool', bg[:], vecs["b_gate"].ap()[:, br * 1024 + nt * 512:br * 1024 + (nt + 1) * 512], bg)
                        return wbuf, bg

                    def c3_load_out():
                        wbuf = wbufs.next()
                        kb.dma('pool', wbuf[:, :, 0:512], wchunks(w_out, 0, 512), wbuf)
                        kb.dma('pool', wbuf[:, :, 512:1024], wchunks(w_out, 512, 512), wbuf)
                        return wbuf

                    chunks = [(br, nt) for br in range(3) for nt in range(2)]
                    nxt = c3_load(*chunks[0])
                    for ci, (br, nt) in enumerate(chunks):
                        if True:
                            wbuf, bg = nxt
                            nxt = c3_load(*chunks[ci + 1]) if ci + 1 < len(chunks) else (c3_load_out(), None)
                            for s in range(4):
                                tok = slice(s * 128, (s + 1) * 128)
                                pg = PGs.next()
                                for c in range(8):
                                    kb.op('pe', lambda pg=pg, c=c, tok=tok: P_.matmul(pg[:], lhsT=hTo[:, c, tok], rhs=wbuf[:, c, 0:512],
                                                                                      start=(c == 0), stop=False), r=[hTo, wbuf], w=[pg])
                                kb.op('pe', lambda pg=pg: P_.matmul(pg[:], lhsT=ones[0:1, :], rhs=bg[:], start=False, stop=True),
                                      r=[ones, bg], w=[pg])
                                kb.op('act', lambda pg=pg: A_.activation(out=gt[:], in_=pg[:], func=AF.Sigmoid), r=[pg], w=[gt])
                                pp = PPs.next()
                                for c in range(8):
                                    if br == 0:
                                        lh = ogs[s][:, c, :]
                                        lt = ogs[s]
                                    elif br == 1:
                                        lh = OdT[:, c, tok]
                                        lt = OdT
                                    else:
                                        lh = OmT[:, c, tok]
                                        lt = OmT
                                    kb.op('pe', lambda pp=pp, c=c, lh=lh: P_.matmul(pp[:], lhsT=lh, rhs=wbuf[:, c, 512:1024],
                                                                                    start=(c == 0), stop=(c == 7)), r=[lt, wbuf], w=[pp])
                                dst = x1[:, s, nt * 512:(nt + 1) * 512]
                                if br == 0:
                                    kb.op('dve', lambda pp=pp, dst=dst: V_.tensor_tensor(out=dst, in0=gt[:], in1=pp[:], op=ALU.mult),
                                          r=[gt, pp], w=[x1])
                                else:
                                    kb.op('dve', lambda pp=pp: V_.tensor_tensor(out=tmp[:], in0=gt[:], in1=pp[:], op=ALU.mult),
                                          r=[gt, pp], w=[tmp])
                                    kb.op('dve', lambda dst=dst: V_.tensor_tensor(out=dst, in0=dst, in1=tmp[:], op=ALU.add),
                                          r=[tmp, x1], w=[x1])
                    wbuf = nxt[0]
                    mbfs = [mbf, sb(ph, "c3_mbf2", [128, 1024], BF16)]
                    mTs = [mT, sb(ph, "c3_mT2", [128, 8, 128], BF16)]

                    def c3_prep(s):
                        mb_, mt_ = mbfs[s % 2], mTs[s % 2]
                        kb.op('act', lambda: A_.copy(out=mb_[:], in_=x1[:, s, :]), r=[x1], w=[mb_])
                        for c in range(8):
                            kb.op('pe', lambda c=c: P_.transpose(out=PT[:, c, :], in_=mb_[:, c * 128:(c + 1) * 128],
                                                                 identity=ident[:]), r=[mb_, ident], w=[PT])
                        kb.op('act', lambda: A_.copy(out=mt_[:], in_=PT[:]), r=[PT], w=[mt_])

                    c3_prep(0)
                    for s in range(4):
                        m = 4 * j + s
                        if s + 1 < 4:
                            c3_prep(s + 1)
                        mt_ = mTs[s % 2]
                        kb.dma('sp', xo[:], x_own[m * 128:(m + 1) * 128, :], xo)
                        for nt in range(2):
                            pp = PPs.next()
                            for c in range(8):
                                kb.op('pe', lambda pp=pp, c=c, nt=nt: P_.matmul(pp[:], lhsT=mt_[:, c, :], rhs=wbuf[:, c, nt * 512:(nt + 1) * 512],
                                                                                start=(c == 0), stop=(c == 7)), r=[mt_, wbuf], w=[pp])
                            kb.op('dve', lambda pp=pp, s=s, nt=nt: V_.tensor_tensor(
                                out=x1[:, s, nt * 512:(nt + 1) * 512], in0=xo[:, nt * 512:(nt + 1) * 512], in1=pp[:], op=ALU.add),
                                r=[xo, pp], w=[x1])
                    if dbg and j == 0:
                        dbgs['x1'] = nc.dram_tensor("dbg_x1", [128, 4096], F32, kind="ExternalOutput")
                        kb.dma('sp', dbgs['x1'].ap(), x1[:].rearrange("p a b -> p (a b)"), x1, load=False)
                    kb.barrier()
                with ExitStack() as ph:
                    g_ffn = sb(ph, "g_ffnt", [128, 1024], F32)
                    xnT = sb(ph, "c4_xnT", [128, 8, 512], BF16)
                    junk = JunkRot([sb(ph, "c4_junk%d" % i, [128, 1024], BF16) for i in range(3)])
                    hbs4 = [sb(ph, "c4_hb%d" % i, [128, 1024], BF16) for i in range(4)]
                    sts4 = [sb(ph, "c4_st%d" % i, [128, 8], F32) for i in range(4)]
                    wr = sb(ph, "c4_wr", [128, 8, 20], BF16)
                    brow = sb(ph, "c4_brow", [1, 20], BF16)
                    lgs4 = [sb(ph, "c4_lg%d" % i, [128, 20], F32) for i in range(4)]
                    rts4 = [sb(ph, "c4_rt%d" % i, [128, 64], F32) for i in range(4)]
                    comb = sb(ph, "c4_comb", [128, 4, 16], F32)
                    wgs = Rot([sb(ph, "c4_wg%d" % i, [128, 8, 256], BF16) for i in range(3)])
                    wus = Rot([sb(ph, "c4_wu%d" % i, [128, 8, 256], BF16) for i in range(3)])
                    wds = Rot([sb(ph, "c4_wd%d" % i, [128, 2, 1024], BF16) for i in range(3)])
                    sgs = Rot([sb(ph, "c4_sg%d" % i, [128, 512], F32) for i in range(2)])
                    hid = [sb(ph, "c4_hid%d" % i, [128, 512], BF16) for i in range(2)]
                    PGs = Rot([ps(ph, "c4_PG%d" % i, [128, 512]) for i in range(2)])
                    PUs = Rot([ps(ph, "c4_PU%d" % i, [128, 512]) for i in range(2)])
                    PDs = Rot([ps(ph, "c4_PD%d" % i, [128, 512]) for i in range(2)])
                    PT = ps(ph, "c4_PT", [128, 8, 128], BF16)
                    PR = ps(ph, "c4_PR", [128, 512])
                    load_gain(g_ffn, "g_ffn", 1024)
                    kb.dma('pool', wr[:, :, 0:4], wchunks(w_r1, 0, 4), wr)
                    kb.dma('pool', wr[:, :, 4:20], wchunks(w_r2, 0, 16), wr)
                    kb.dma('pool', brow[:, 0:4], vecs["b_r1"].ap(), brow)
                    kb.dma('pool', brow[:, 4:20], vecs["b_r2"].ap(), brow)
                    BIG = 1.0e4

                    def c4_head(s):
                        hb = hbs4[s]; st = sts4[s]; lg = lgs4[s]; rt = rts4[s]
                        PRs = PR[:, 32 * s:32 * s + 20]
                        tok = slice(s * 128, (s + 1) * 128)
                        kb.op('act', lambda s=s: A_.activation(out=junk[:], in_=x1[:, s, :], func=AF.Square, accum_out=st[:, 0:1]),
                              r=[x1], w=[junk, st])
                        rstd_from_ss(st[:, 0:1], 1, 1024.0, st)
                        yield
                        kb.op('dve', lambda s=s: V_.scalar_tensor_tensor(out=hb[:], in0=x1[:, s, :], scalar=st[:, 0:1], in1=g_ffn[:],
                                                                         op0=ALU.mult, op1=ALU.mult), r=[x1, st, g_ffn], w=[hb])
                        yield
                        for c in range(8):
                            kb.op('pe', lambda c=c: P_.transpose(out=PT[:, c, :], in_=hb[:, c * 128:(c + 1) * 128],
                                                                 identity=ident[:]), r=[hb, ident], w=[PT])
                        kb.op('act', lambda tok=tok: A_.copy(out=xnT[:, :, tok], in_=PT[:]), r=[PT], w=[xnT])
                        yield
                        for c in range(8):
                            kb.op('pe', lambda c=c, tok=tok: P_.matmul(PRs, lhsT=xnT[:, c, tok], rhs=wr[:, c, :],
                                                                       start=(c == 0), stop=False), r=[xnT, wr], w=[PR])
                        kb.op('pe', lambda: P_.matmul(PRs, lhsT=ones[0:1, :], rhs=brow[:], start=False, stop=True),
                              r=[ones, brow], w=[PR])
                        kb.op('dve', lambda: V_.tensor_copy(out=lg[:], in_=PRs), r=[PR], w=[lg])
                        yield
                        dv = lambda fn, r=(), w=(): kb.op('dve', fn, r=[lg, rt] + list(r), w=[rt] + list(w))
                        dv(lambda: V_.tensor_reduce(out=rt[:, 0:1], in_=lg[:, 0:4], axis=AX.X, op=ALU.max))
                        yield
                        dv(lambda: V_.tensor_scalar(out=rt[:, 1:2], in0=rt[:, 0:1], scalar1=-1.0, scalar2=None, op0=ALU.mult))
                        yield
                        kb.op('act', lambda: A_.activation(out=rt[:, 48:52], in_=lg[:, 0:4], func=AF.Exp, bias=rt[:, 1:2],
                                                           accum_out=rt[:, 2:3]), r=[lg, rt], w=[rt])
                        yield
                        dv(lambda: V_.reciprocal(out=rt[:, 3:4], in_=rt[:, 2:3]))
                        yield
                        dv(lambda: V_.tensor_scalar(out=rt[:, 4:8], in0=lg[:, 0:4], scalar1=rt[:, 0:1], scalar2=None, op0=ALU.is_ge))
                        yield
                        dv(lambda: V_.tensor_scalar(out=rt[:, 8:12], in0=rt[:, 4:8], scalar1=BIG, scalar2=-BIG, op0=ALU.mult,
                                                    op1=ALU.add))
                        yield
                        for g in range(4):
                            dv(lambda g=g: V_.tensor_scalar(out=rt[:, 12 + 4 * g:16 + 4 * g], in0=lg[:, 4 + 4 * g:8 + 4 * g],
                                                            scalar1=rt[:, 8 + g:9 + g], scalar2=None, op0=ALU.add))
                            yield
                        dv(lambda: V_.tensor_reduce(out=rt[:, 28:29], in_=rt[:, 12:28], axis=AX.X, op=ALU.max))
                        yield
                        dv(lambda: V_.tensor_scalar(out=rt[:, 29:30], in0=rt[:, 28:29], scalar1=-1.0, scalar2=None, op0=ALU.mult))
                        yield
                        dv(lambda: V_.tensor_scalar(out=rt[:, 30:46], in0=rt[:, 12:28], scalar1=rt[:, 28:29], scalar2=-BIG,
                                                    op0=ALU.is_ge, op1=ALU.mult))
                        yield
                        dv(lambda: V_.tensor_tensor(out=rt[:, 30:46], in0=rt[:, 30:46], in1=rt[:, 12:28], op=ALU.add))
                        yield
                        dv(lambda: V_.tensor_reduce(out=rt[:, 46:47], in_=rt[:, 30:46], axis=AX.X, op=ALU.max))
                        yield
                        kb.op('act', lambda: A_.activation(out=rt[:, 48:64], in_=rt[:, 12:28], func=AF.Exp, bias=rt[:, 29:30]),
                              r=[rt], w=[rt])
                        yield
                        dv(lambda: V_.scalar_tensor_tensor(out=rt[:, 48:64], in0=rt[:, 12:28], scalar=rt[:, 46:47], in1=rt[:, 48:64],
                                                           op0=ALU.is_ge, op1=ALU.mult))
                        yield
                        dv(lambda: V_.tensor_reduce(out=rt[:, 47:48], in_=rt[:, 48:64], axis=AX.X, op=ALU.add))
                        yield
                        dv(lambda: V_.reciprocal(out=rt[:, 47:48], in_=rt[:, 47:48]))
                        yield
                        dv(lambda: V_.tensor_tensor(out=rt[:, 47:48], in0=rt[:, 47:48], in1=rt[:, 3:4], op=ALU.mult))
                        yield
                        dv(lambda s=s: V_.tensor_scalar(out=comb[:, s, :], in0=rt[:, 48:64], scalar1=rt[:, 47:48], scalar2=None,
                                                        op0=ALU.mult), w=[comb])
                        yield
                    pipeline([c4_head(s) for s in range(4)], depth=4, lag=0)
                    hidA = [hid, [sb(ph, "c4_hidB%d" % i, [128, 512], BF16) for i in range(2)]]
                    wsets = {}

                    def c4_load(e):
                        wg_ = wgs.next(); wu_ = wus.next(); wd_ = wds.next()
                        kb.dma('pool', wg_[:], w_gate[e].rearrange("(c p) n -> p c n", p=128), wg_)
                        kb.dma('pool', wu_[:], w_up[e].rearrange("(c p) n -> p c n", p=128), wu_)
                        kb.dma('pool', wd_[:], w_down[e].rearrange("(c p) n -> p c n", p=128), wd_)
                        wsets[e] = (wg_, wu_, wd_)

                    def c4_gu(e, fc):
                        wg_, wu_, _ = wsets[e]
                        hd = hidA[e % 2][fc]
                        pg = PGs.next(); pu = PUs.next()
                        for c in range(8):
                            kb.op('pe', lambda c=c: P_.matmul(
                                pg[:], lhsT=wg_[:, c, fc * 128:(fc + 1) * 128], rhs=xnT[:, c, :], start=(c == 0), stop=(c == 7)),
                                r=[wg_, xnT], w=[pg])
                        for c in range(8):
                            kb.op('pe', lambda c=c: P_.matmul(
                                pu[:], lhsT=wu_[:, c, fc * 128:(fc + 1) * 128], rhs=xnT[:, c, :], start=(c == 0), stop=(c == 7)),
                                r=[wu_, xnT], w=[pu])
                        sg_ = sgs.next()
                        kb.op('act', lambda: A_.activation(out=sg_[:], in_=pg[:], func=AF.Silu), r=[pg], w=[sg_])
                        kb.op('dve', lambda: V_.tensor_tensor(out=hd[:], in0=sg_[:], in1=pu[:], op=ALU.mult),
                              r=[sg_, pu], w=[hd])

                    def c4_down(e, slots):
                        _, _, wd_ = wsets[e]
                        for s in slots:
                            tok = slice(s * 128, (s + 1) * 128)
                            for nt in range(2):
                                pd = PDs.next()
                                for fc in range(2):
                                    hd = hidA[e % 2][fc]
                                    kb.op('pe', lambda fc=fc, hd=hd: P_.matmul(
                                        pd[:], lhsT=hd[:, tok], rhs=wd_[:, fc, nt * 512:(nt + 1) * 512], start=(fc == 0), stop=(fc == 1)),
                                        r=[hd, wd_], w=[pd])
                                kb.op('dve', lambda s=s, nt=nt: V_.scalar_tensor_tensor(
                                    out=x1[:, s, nt * 512:(nt + 1) * 512], in0=pd[:], scalar=comb[:, s, e:e + 1],
                                    in1=x1[:, s, nt * 512:(nt + 1) * 512], op0=ALU.mult, op1=ALU.add), r=[pd, comb, x1], w=[x1])

                    c4_load(0)
                    c4_load(1)
                    c4_gu(0, 0)
                    c4_gu(0, 1)
                    for e in range(16):
                        if e + 2 < 16:
                            c4_load(e + 2)
                        if e + 1 < 16:
                            c4_gu(e + 1, 0)
                        c4_down(e, [0, 1])
                        if e + 1 < 16:
                            c4_gu(e + 1, 1)
                        c4_down(e, [2, 3])
                    for s in range(4):
                        m = 4 * j + s
                        kb.dma('sp', out_own[m * 128:(m + 1) * 128, :], x1[:, s, :], x1, load=False)
                    kb.barrier()
    return nc, kb, dbgs


def _consts():
    bf = ml_dtypes.bfloat16
    c = {}
    c["c_ident"] = np.eye(128, dtype=np.float32).astype(bf)
    c["c_i4"] = np.tile(np.eye(128, dtype=np.float32), (1, 4)).astype(bf)
    c["c_ones"] = np.ones((128, 128), np.float32).astype(bf)
    j = np.arange(128)[:, None]
    i = np.arange(128)[None, :]
    c["c_lmt"] = np.where(j <= i, -1.0 / 16.0, 0.0).astype(np.float32)
    c["c_umt"] = np.where(j > i, -1.0 / 16.0, 0.0).astype(np.float32)
    c["c_caus4"] = np.tile(np.where(j <= i, 1.0, 0.0), (1, 4)).astype(np.float32)
    sel = np.zeros((16, 16, 128), np.float32)
    for e in range(16):
        sel[e, e, :] = 1.0
    c["c_sel16"] = sel.reshape(16, 2048).astype(bf)
    c["c_bis"] = np.tile((2.0 ** (1.0 - np.arange(1, NBIS + 1)))[None, :], (128, 1)).astype(np.float32)
    return c


_CACHE = {}


def kernel(x, mem, g_mix, g_mem, w_in, w_gla_a2, b_gla_a2, gla_norm, w_mem_kv,
           dsa_q_norm, dsa_k_norm, idx_k_norm, mem_q_norm, mem_k_norm, b_gate,
           w_o_gla, w_o_dsa, w_o_mem, w_out, g_ffn, w_r1, b_r1, w_r2, b_r2,
           w_gate, w_up, w_down, _dbg=False):
    f = lambda a: np.ascontiguousarray(np.asarray(a, dtype=np.float32))
    x = f(x)
    B, TT, _ = x.shape
    key = (TT, _dbg)
    if key not in _CACHE:
        _CACHE[key] = build_nc(TT, dbg=_dbg)
    nc, kb, dbgs = _CACHE[key]
    NB = TT // 128
    consts = _consts()
    shared = {
        "w_in": f(w_in)[0], "w_gla_a2": f(w_gla_a2)[0], "w_mem_kv": f(w_mem_kv)[0],
        "w_o_gla": f(w_o_gla)[0], "w_o_dsa": f(w_o_dsa)[0], "w_o_mem": f(w_o_mem)[0], "w_out": f(w_out)[0],
        "w_r1": f(w_r1)[0], "w_r2": f(w_r2)[0], "w_gate": f(w_gate)[0], "w_up": f(w_up)[0], "w_down": f(w_down)[0],
        "g_mix": f(g_mix), "g_mem": f(g_mem), "g_ffn": f(g_ffn), "gla_norm": f(gla_norm), "dsa_q_norm": f(dsa_q_norm),
        "dsa_k_norm": f(dsa_k_norm), "idx_k_norm": f(idx_k_norm), "mem_q_norm": f(mem_q_norm), "mem_k_norm": f(mem_k_norm),
        "b_gla_a2": f(b_gla_a2), "b_gate": f(b_gate), "b_r1": f(b_r1), "b_r2": f(b_r2),
    }
    shared.update(consts)
    memf = f(mem)
    in_maps = []
    tpos = np.arange(128)[:, None]
    spos = np.arange(128)[None, :]
    for c in range(8):
        b, r = c // 4, c % 4
        xb = x[b].reshape(NB, 128, D)
        own = np.ascontiguousarray(xb[r::4].reshape(-1, D))
        cm = np.zeros((128, 512), np.float32)
        for p in range(4):
            if p == r:
                cm[:, p * 128:(p + 1) * 128] = np.where(spos <= tpos, 0.0, -1e30)
            elif p > r:
                cm[:, p * 128:(p + 1) * 128] = -1e30
        ws = np.zeros((128, 4), np.float32)
        ws[:, r] = 1.0
        d = dict(shared)
        d.update({"x_full": x[b], "x_own": own, "mem": memf[b], "cmask": cm, "wsel": ws})
        in_maps.append(d)
    res = run_bass_kernel_spmd(nc, in_maps, core_ids=list(range(8)))
    out = np.zeros((B, NB, 128, D), np.float32)
    for c in range(8):
        b, r = c // 4, c % 4
        out[b, r::4] = res.results[c]["out_own"].reshape(NB // 4, 128, D)
    if _dbg:
        kernel.last = res
    return out.reshape(B, TT, D)
```

```python
import numpy as np
import ml_dtypes
from contextlib import ExitStack
import concourse.bass as bass
import concourse.mybir as mybir
from concourse.bass_utils import run_bass_kernel_spmd

F32 = mybir.dt.float32
BF16 = mybir.dt.bfloat16
AF = mybir.ActivationFunctionType
ALU = mybir.AluOpType
AX = mybir.AxisListType

D = 1024
D_IN = 9304
EPS = 1e-6
OFF = dict(gq=0, gk=512, gv=1024, gg=2048, ga=3072, dq=3088, dk=4112, dv=4368, iq=4624,
           ik=5136, iw=5200, mq=5208, gates=6232)
NBIS = 14
NEG = -30000.0


class T:
    def __init__(self, h):
        self.h = h
        self.w = None
        self.r = {}
        self.dsem = None
        self.dcnt = 0
        self.wx = []

    def __getitem__(self, k):
        return self.h[k]


class Rot:
    def __init__(self, tiles):
        self.t = tiles
        self.i = 0

    def next(self):
        t = self.t[self.i % len(self.t)]
        self.i += 1
        return t


class JunkRot:
    def __init__(self, tiles):
        self.t = tiles
        self.i = 0

    def advance(self):
        self.i += 1
        return self.t[self.i % len(self.t)]

    def __getitem__(self, k):
        return self.t[self.i % len(self.t)].h[k]


class KB:
    def __init__(self, nc):
        self.nc = nc
        self.eng = {'pe': nc.tensor, 'act': nc.scalar, 'dve': nc.vector, 'pool': nc.gpsimd, 'sp': nc.sync}
        self.sem = {k: nc.alloc_semaphore("s_" + k) for k in self.eng}
        self.cnt = {k: 0 for k in self.eng}
        self.waited = {k: {} for k in self.eng}
        self.dma_evs = {}
        self.nsem = 5
        self.free_sems = []
        self.free_sems_sw = []
        self.dma_tiles = []
        self.temp_recs = []
        self.nops = 0

    def _deps(self, r, w):
        deps = []
        for t in r:
            if t.w is not None:
                deps.append(t.w)
            deps.extend(t.wx)
        for t in w:
            if t.w is not None:
                deps.append(t.w)
            deps.extend(t.wx)
            deps.extend(t.r.values())
        return deps

    def _wait(self, e, deps):
        wd = self.waited[e]
        best = {}
        for (sem, sid, val, prod) in deps:
            if prod == e and e == 'pe':
                continue
            if wd.get(sid, 0) >= val:
                continue
            if sid not in best or best[sid][1] < val:
                best[sid] = (sem, val)
        for sid, (sem, val) in best.items():
            self.eng[e].wait_ge(sem, val)
            wd[sid] = val

    def op(self, e, fn, r=(), w=()):
        w = [x.advance() if isinstance(x, JunkRot) else x for x in w]
        self._wait(e, self._deps(r, w))
        ins = fn()
        self.cnt[e] += 1
        ins.then_inc(self.sem[e], 1)
        ev = (self.sem[e], e, self.cnt[e], e)
        for t in r:
            t.r[e] = ev
        for t in w:
            t.w = ev
            t.wx = []
            t.r = {}
        self.nops += 1
        return ev

    def dma(self, q, out_ap, in_ap, sb_t, load=True):
        def new_rec():
            pool_ = self.free_sems_sw if q == 'pool' else self.free_sems
            if pool_:
                return pool_.pop()
            r_ = [self.nc.alloc_semaphore("d%d" % self.nsem), "d%d" % self.nsem, 0, q]
            self.nsem += 1
            return r_

        fresh = (q == 'pool' and load and sb_t.dsem is not None and sb_t.w is not None
                 and sb_t.w[3] == 'dma' and not sb_t.r)
        if fresh:
            rec = new_rec()
            self.temp_recs.append(rec)
        else:
            deps = self._deps((), (sb_t,)) if load else self._deps((sb_t,), ())
            self._wait(q, deps)
            if sb_t.dsem is not None and sb_t.dsem[3] != q:
                self.temp_recs.append(sb_t.dsem)
                sb_t.dsem = new_rec()
            if sb_t.dsem is None:
                sb_t.dsem = new_rec()
                self.dma_tiles.append(sb_t)
            rec = sb_t.dsem
        ins = self.eng[q].dma_start(out=out_ap, in_=in_ap)
        rec[2] += 16
        ins.then_inc(rec[0], 16)
        ev = (rec[0], rec[1], rec[2], 'dma')
        if load:
            if fresh:
                sb_t.wx = sb_t.wx + [sb_t.w]
            else:
                sb_t.wx = []
            sb_t.w = ev
            sb_t.r = {}
        else:
            sb_t.r['dma%d' % rec[2]] = ev
        self.dma_evs[rec[1]] = ev
        return ev

    def barrier(self):
        self._wait('sp', list(self.dma_evs.values()))
        evs = [(self.sem[k], k, self.cnt[k], k) for k in self.eng if k != 'sp' and self.cnt[k] > 0]
        self._wait('sp', evs)
        self.eng['sp'].sem_inc(self.sem['sp'], 1)
        self.cnt['sp'] += 1
        ev = (self.sem['sp'], 'sp', self.cnt['sp'], 'sp')
        for k in self.eng:
            if k != 'sp':
                self._wait(k, [ev])
        for t in self.dma_tiles:
            self.temp_recs.append(t.dsem)
            t.dsem = None
        self.dma_tiles = []
        for rec in self.temp_recs:
            (self.free_sems_sw if rec[3] == 'pool' else self.free_sems).append(rec)
        self.temp_recs = []
        self.dma_evs = {}


def pipeline(gens, depth=2, lag=4):
    active = []
    idx = 0
    n = len(gens)
    while idx < n or active:
        if idx < n and len(active) < depth and (not active or active[-1][1] >= lag):
            active.append([gens[idx], 0])
            idx += 1
        for a in list(active):
            try:
                next(a[0])
                a[1] += 1
            except StopIteration:
                active.remove(a)


def pipeline2(items, depth, lag, ahead):
    n = len(items)
    pre_res = {}

    def ensure(k):
        if k < n and k not in pre_res:
            pre_res[k] = items[k][0]()

    active = []
    idx = 0
    while idx < n or active:
        if idx < n and len(active) < depth and (not active or active[-1][1] >= lag):
            for k in range(idx, idx + ahead + 1):
                ensure(k)
            active.append([items[idx][1](pre_res.pop(idx)), 0])
            idx += 1
        for a in list(active):
            try:
                next(a[0])
                a[1] += 1
            except StopIteration:
                active.remove(a)


def build_nc(TT, dbg=False):
    NB = TT // 128
    NOWN = NB // 4
    NTILE = NOWN // 4
    nc = bass.Bass("TRN2", target_bir_lowering=False)
    kb = KB(nc)
    V_, A_, P_, G_ = nc.vector, nc.scalar, nc.tensor, nc.gpsimd

    def din(name, shape, dt=F32):
        return nc.dram_tensor(name, list(shape), dt, kind="ExternalInput")

    x_full = din("x_full", [TT, D]).ap()
    x_own = din("x_own", [NOWN * 128, D]).ap()
    mem = din("mem", [256, D]).ap()
    cmask_d = din("cmask", [128, 512]).ap()
    wsel_d = din("wsel", [128, 4]).ap()
    w_in = din("w_in", [D, D_IN]).ap()
    w_a2 = din("w_gla_a2", [16, 512]).ap()
    w_mem_kv = din("w_mem_kv", [D, 2048]).ap()
    w_o = [din(n, [D, D]).ap() for n in ("w_o_gla", "w_o_dsa", "w_o_mem")]
    w_out = din("w_out", [D, D]).ap()
    w_r1 = din("w_r1", [D, 4]).ap()
    w_r2 = din("w_r2", [D, 16]).ap()
    w_gate = din("w_gate", [16, D, 256]).ap()
    w_up = din("w_up", [16, D, 256]).ap()
    w_down = din("w_down", [16, 256, D]).ap()
    vecs = {}
    for n, L in (("g_mix", 1024), ("g_mem", 1024), ("g_ffn", 1024), ("gla_norm", 256), ("dsa_q_norm", 128),
                 ("dsa_k_norm", 128), ("idx_k_norm", 64), ("mem_q_norm", 256), ("mem_k_norm", 256),
                 ("b_gla_a2", 512), ("b_gate", 3072), ("b_r1", 4), ("b_r2", 16)):
        vecs[n] = din(n, [1, L])
    c_ident = din("c_ident", [128, 128], BF16).ap()
    c_i4 = din("c_i4", [128, 512], BF16).ap()
    c_ones = din("c_ones", [128, 128], BF16).ap()
    c_lmt = din("c_lmt", [128, 128]).ap()
    c_umt = din("c_umt", [128, 128]).ap()
    c_caus4 = din("c_caus4", [128, 512]).ap()
    c_sel16 = din("c_sel16", [16, 2048], BF16).ap()
    c_bis = din("c_bis", [128, NBIS]).ap()
    out_own = nc.dram_tensor("out_own", [NOWN * 128, D], F32, kind="ExternalOutput").ap()
    dbgs = {}
    ogla_scr = nc.dram_tensor("ogla_scr", [NOWN, 128, 1024], BF16, kind="Internal").ap()

    def bcast(name, L):
        return bass.AP(tensor=vecs[name], offset=0, ap=[[0, 128], [1, L]])

    top = ExitStack()

    uid = [0]

    def sb(stack, name, shape, dt):
        uid[0] += 1
        return T(stack.enter_context(nc.sbuf_tensor("%s_%d" % (name, uid[0]), list(shape), dt)))

    def ps(stack, name, shape, dt=F32):
        uid[0] += 1
        return T(stack.enter_context(nc.psum_tensor("%s_%d" % (name, uid[0]), list(shape), dt)))

    def wchunks(src2d, col0, ncol):
        return src2d.rearrange("(c p) n -> p c n", p=128)[:, :, col0:col0 + ncol]

    with top:
        ident = sb(top, "ident", [128, 128], BF16)
        i4 = sb(top, "i4", [128, 512], BF16)
        ones = sb(top, "ones", [128, 128], BF16)
        wsel = sb(top, "wselt", [128, 4], F32)
        rstd_all = sb(top, "rstd_all", [128, NB], F32)
        memKT = sb(top, "memKT", [128, 8, 256], BF16)
        memV = sb(top, "memV", [128, 2, 1024], BF16)

        for t_, src_ in ((ident, c_ident), (i4, c_i4), (ones, c_ones), (wsel, wsel_d)):
            kb.dma('sp', t_[:], src_, t_)

        def load_gain(t_, name, L, rep=1, scale=None):
            for i in range(rep):
                kb.dma('sp', t_[:, i * L:(i + 1) * L], bcast(name, L), t_)
            if scale is not None:
                kb.op('dve', lambda: V_.tensor_scalar(out=t_[:], in0=t_[:], scalar1=scale, scalar2=None,
                                                      op0=ALU.mult), r=[t_], w=[t_])

        def rstd_from_ss(ss, n, width, tmp):
            kb.op('act', lambda: A_.activation(out=ss, in_=ss, func=AF.Ln, bias=EPS, scale=1.0 / width), r=[tmp], w=[tmp])
            kb.op('act', lambda: A_.activation(out=ss, in_=ss, func=AF.Exp, scale=-0.5), r=[tmp], w=[tmp])

        def norm_only(xt, gain, junk, st, hb, ss_ap=None, ss_t=None):
            if ss_ap is None:
                ss_ap, ss_t = st[:, 0:1], st
            kb.op('act', lambda: A_.activation(out=junk[:], in_=xt[:], func=AF.Square, accum_out=ss_ap),
                  r=[xt], w=[junk, ss_t])
            rstd_from_ss(ss_ap, 1, 1024.0, ss_t)
            kb.op('dve', lambda: V_.scalar_tensor_tensor(out=hb[:], in0=xt[:], scalar=ss_ap, in1=gain[:],
                                                         op0=ALU.mult, op1=ALU.mult), r=[xt, ss_t, gain], w=[hb])

        def transp_T(hb, PT, hT_ap, hT_t):
            for c in range(8):
                kb.op('pe', lambda c=c: P_.transpose(out=PT[:, c, :], in_=hb[:, c * 128:(c + 1) * 128],
                                                     identity=ident[:]), r=[hb, ident], w=[PT])
            kb.op('act', lambda: A_.copy(out=hT_ap, in_=PT[:]), r=[PT], w=[hT_t])

        def norm_T(xt, gain, junk, st, hb, PT, hT_ap, hT_t):
            norm_only(xt, gain, junk, st, hb)
            transp_T(hb, PT, hT_ap, hT_t)

        def proj(hT_ap_fn, hT_t, W, col0, ncol, PJ_t, PJ_ap=None):
            o = PJ_t[:, 0:ncol] if PJ_ap is None else PJ_ap
            for c in range(8):
                kb.op('pe', lambda c=c: P_.matmul(o, lhsT=hT_ap_fn(c), rhs=W[:, c, col0:col0 + ncol],
                                                  start=(c == 0), stop=(c == 7)), r=[hT_t, W], w=[PJ_t])

        with ExitStack() as ph:
            g_mem = sb(ph, "g_memt", [128, 1024], F32)
            mkg4 = sb(ph, "mkg4", [128, 1024], F32)
            xt = sb(ph, "p0_x", [128, 1024], F32)
            junk = JunkRot([sb(ph, "p0_junk%d" % i, [128, 1024], BF16) for i in range(3)])
            st = sb(ph, "p0_st", [128, 8], F32)
            hb = sb(ph, "p0_hb", [128, 1024], BF16)
            mhT = sb(ph, "p0_mhT", [128, 8, 256], BF16)
            wkv = [sb(ph, "p0_wkv%d" % i, [128, 8, 512], BF16) for i in range(2)]
            kfs = [sb(ph, "p0_kf%d" % i, [128, 1024], F32) for i in range(2)]
            kbf = sb(ph, "p0_kbf", [128, 1024], BF16)
            PT = ps(ph, "p0_PT", [128, 8, 128], BF16)
            PJ = [ps(ph, "p0_PJ%d" % i, [128, 512]) for i in range(2)]
            kb.dma('sp', g_mem[:], bcast("g_mem", 1024), g_mem)
            for i in range(4):
                kb.dma('sp', mkg4[:, i * 256:(i + 1) * 256], bcast("mem_k_norm", 256), mkg4)
            for mb in range(2):
                kb.dma('sp', xt[:], mem[mb * 128:(mb + 1) * 128, :], xt)
                norm_T(xt, g_mem, junk, st, hb, PT, mhT[:, :, mb * 128:(mb + 1) * 128], mhT)
            wrot = Rot(wkv)
            pjrot = Rot(PJ)
            for nt in range(4):
                wt = wrot.next()
                kb.dma('pool', wt[:], wchunks(w_mem_kv, nt * 512, 512), wt)
                for mb in range(2):
                    pj = pjrot.next()
                    kf = kfs[mb]
                    proj(lambda c, mb=mb: mhT[:, c, mb * 128:(mb + 1) * 128], mhT, wt, 0, 512, pj)
                    if nt < 2:
                        kb.op('act', lambda pj=pj, nt=nt, kf=kf: A_.copy(out=kf[:, nt * 512:(nt + 1) * 512], in_=pj[:]),
                              r=[pj], w=[kf])
                        if nt == 1:
                            for h in range(4):
                                kb.op('act', lambda h=h, kf=kf: A_.activation(out=junk[:, 0:256], in_=kf[:, h * 256:(h + 1) * 256],
                                                                       func=AF.Square, accum_out=st[:, h:h + 1]),
                                      r=[kf], w=[junk, st])
                            rstd_from_ss(st[:, 0:4], 4, 256.0, st)
                            for h in range(4):
                                kb.op('dve', lambda h=h, kf=kf: V_.scalar_tensor_tensor(
                                    out=kbf[:, h * 256:(h + 1) * 256], in0=kf[:, h * 256:(h + 1) * 256],
                                    scalar=st[:, h:h + 1], in1=mkg4[:, h * 256:(h + 1) * 256],
                                    op0=ALU.mult, op1=ALU.mult), r=[kf, st, mkg4], w=[kbf])
                            for c8 in range(8):
                                kb.op('pe', lambda c8=c8: P_.transpose(out=PT[:, c8, :], in_=kbf[:, c8 * 128:(c8 + 1) * 128],
                                                                       identity=ident[:]), r=[kbf, ident], w=[PT])
                            kb.op('act', lambda mb=mb: A_.copy(out=memKT[:, :, mb * 128:(mb + 1) * 128], in_=PT[:]),
                                  r=[PT], w=[memKT])
                    else:
                        kb.op('act', lambda pj=pj, nt=nt, mb=mb: A_.copy(
                            out=memV[:, mb, (nt - 2) * 512:(nt - 1) * 512], in_=pj[:]), r=[pj], w=[memV])
            kb.barrier()


        with ExitStack() as ph:
            Wg = sb(ph, "g_W", [128, 8, 3088], BF16)
            caus4 = sb(ph, "caus4", [128, 512], F32)
            g_mix = sb(ph, "g_mixt", [128, 1024], F32)
            gnorm = sb(ph, "gnormt", [128, 256], F32)
            kb.dma('sp', caus4[:], c_caus4, caus4)
            load_gain(g_mix, "g_mix", 1024)
            load_gain(gnorm, "gla_norm", 256)
            wa2 = sb(ph, "g_wa2", [17, 512], BF16)
            lmt = sb(ph, "g_lmt", [128, 128], F32)
            umt = sb(ph, "g_umt", [128, 128], F32)
            negc = sb(ph, "g_negc", [128, 2], F32)
            S = sb(ph, "g_S", [128, 1024], F32)
            Ssel = sb(ph, "g_Ssel", [128, 1024], F32)
            Sbf = sb(ph, "g_Sbf", [128, 1024], BF16)
            xts = Rot([sb(ph, "g_x%d" % i, [128, 1024], F32) for i in range(4)])
            junk = JunkRot([sb(ph, "g_junk%d" % i, [128, 256], BF16) for i in range(3)])
            sts = Rot([sb(ph, "g_st%d" % i, [128, 8], F32) for i in range(4)])
            hbs = Rot([sb(ph, "g_hb%d" % i, [128, 1024], BF16) for i in range(4)])
            hTs = Rot([sb(ph, "g_hT%d" % i, [128, 8, 128], BF16) for i in range(4)])
            kfs = Rot([sb(ph, "g_kf%d" % i, [128, 512], F32) for i in range(4)])
            qf = sb(ph, "g_qf", [128, 512], F32)
            vbs = Rot([sb(ph, "g_vb%d" % i, [128, 1024], BF16) for i in range(4)])
            sg = sb(ph, "g_sg", [128, 1024], F32)
            aTs = Rot([sb(ph, "g_aT%d" % i, [17, 128], BF16) for i in range(4)])
            spbs = Rot([sb(ph, "g_sp%d" % i, [128, 512], F32) for i in range(4)])
            E1s = Rot([sb(ph, "g_E1%d" % i, [128, 512], F32) for i in range(4)])
            E2 = sb(ph, "g_E2", [128, 512], F32)
            decs = Rot([sb(ph, "g_dec%d" % i, [128, 4], F32) for i in range(4)])
            kends = Rot([sb(ph, "g_kend%d" % i, [128, 512], BF16) for i in range(4)])
            qdec = sb(ph, "g_qdec", [128, 512], BF16)
            qkT = sb(ph, "g_qkT", [128, 8, 128], BF16)
            attT = sb(ph, "g_attT", [128, 512], BF16)
            ogl = sb(ph, "g_ogl", [128, 1024], BF16)
            ogTs = Rot([sb(ph, "g_ogT%d" % i, [128, 8, 128], BF16) for i in range(2)])
            PJs = Rot([ps(ph, "g_PJ%d" % i, [128, 512]) for i in range(2)])
            PA = ps(ph, "g_PA", [128, 512])
            PB = ps(ph, "g_PB", [128, 512])
            PU = [ps(ph, "g_PU%d" % i, [128, 512]) for i in range(2)]
            PT = ps(ph, "g_PT", [128, 8, 128], BF16)
            PS = ps(ph, "g_PS", [128, 512])

            for i in range(7):
                c0 = i * 512
                n = min(512, 3088 - c0)
                kb.dma('pool', Wg[:, :, c0:c0 + n], wchunks(w_in, c0, n), Wg)
            kb.dma('pool', wa2[0:16, :], w_a2, wa2)
            kb.dma('pool', wa2[16:17, :], vecs["b_gla_a2"].ap(), wa2)
            kb.dma('sp', lmt[:], c_lmt, lmt)
            kb.dma('sp', umt[:], c_umt, umt)
            kb.op('dve', lambda: V_.memset(negc[:], -1.0 / 16.0), w=[negc])
            kb.op('dve', lambda: V_.memset(S[:], 0.0), w=[S])
            for aT_ in aTs.t:
                kb.op('pool', lambda aT_=aT_: G_.memset(aT_[:], 1.0), w=[aT_])

            def g_pre(xsrc, blk=None):
                def f():
                    xt = xts.next(); st = sts.next(); hb = hbs.next()
                    kb.dma('sp', xt[:], xsrc, xt)
                    if blk is None:
                        norm_only(xt, g_mix, hb, st, hb)
                    else:
                        norm_only(xt, g_mix, hb, st, hb, ss_ap=rstd_all[:, blk:blk + 1], ss_t=rstd_all)
                    return (st, hb)
                return f

            def gla_block(pre, own, p, slot):
                st, hb = pre
                hT = hTs.next()
                kf = kfs.next(); vb = vbs.next(); aT = aTs.next(); spb = spbs.next(); E1 = E1s.next(); E3 = E1
                dec = decs.next(); kend = kends.next(); kinv = kend
                transp_T(hb, PT, hT[:], hT)
                yield
                hf = lambda c: hT[:, c, :]
                for c in range(8):
                    kb.op('pe', lambda c=c: P_.matmul(PS[0:16, 0:128], lhsT=Wg[:, c, OFF['ga']:OFF['ga'] + 16],
                                                      rhs=hT[:, c, :], start=(c == 0), stop=(c == 7)),
                          r=[hT, Wg], w=[PS])
                kb.op('act', lambda: A_.copy(out=aT[0:16, :], in_=PS[0:16, 0:128]), r=[PS], w=[aT])
                pj = PJs.next()
                proj(hf, hT, Wg, OFF['gk'], 512, pj)
                kb.op('dve', lambda: V_.tensor_copy(out=kf[:], in_=pj[:]), r=[pj], w=[kf])
                yield
                kb.op('pe', lambda: P_.matmul(PA[:], lhsT=aT[:], rhs=wa2[:], start=True, stop=True),
                      r=[aT, wa2], w=[PA])
                pj = PJs.next()
                proj(hf, hT, Wg, OFF['gv'], 512, pj)
                kb.op('dve', lambda pj=pj: V_.tensor_copy(out=vb[:, 0:512], in_=pj[:]), r=[pj], w=[vb])
                yield
                kb.op('act', lambda: A_.activation(out=spb[:], in_=PA[:], func=AF.Exp, scale=-1.0), r=[PA], w=[spb])
                kb.op('act', lambda: A_.activation(out=spb[:], in_=spb[:], func=AF.Ln, bias=1.0), r=[spb], w=[spb])
                pj = PJs.next()
                proj(hf, hT, Wg, OFF['gv'] + 512, 512, pj)
                kb.op('dve', lambda pj=pj: V_.tensor_copy(out=vb[:, 512:1024], in_=pj[:]), r=[pj], w=[vb])
                yield
                if own:
                    pj = PJs.next()
                    proj(hf, hT, Wg, OFF['gq'], 512, pj)
                    kb.op('act', lambda: A_.copy(out=qf[:], in_=pj[:]), r=[pj], w=[qf])

                    def g_proj(nt):
                        pj = PJs.next()
                        proj(hf, hT, Wg, OFF['gg'] + nt * 512, 512, pj)
                        kb.op('act', lambda: A_.activation(out=sg[:, nt * 512:(nt + 1) * 512], in_=pj[:], func=AF.Silu),
                              r=[pj], w=[sg])
                yield
                if not own:
                    kb.op('pe', lambda: P_.matmul(PB[:], lhsT=umt[:], rhs=spb[:], start=True, stop=True),
                          r=[umt, spb], w=[PB])
                    for h in range(4):
                        kb.op('pe', lambda h=h: P_.matmul(PS[:, 128 + 2 * h:130 + 2 * h], lhsT=spb[:, h * 128:(h + 1) * 128],
                                                          rhs=negc[:], start=True, stop=True), r=[spb, negc], w=[PS])
                    yield
                    kb.op('act', lambda: A_.activation(out=E3[:], in_=PB[:], func=AF.Exp), r=[PB], w=[E3])
                    kb.op('act', lambda: A_.activation(out=dec[:], in_=PS[:, 128:136:2], func=AF.Exp), r=[PS], w=[dec])
                    kb.op('dve', lambda: V_.tensor_tensor(out=kend[:], in0=kf[:], in1=E3[:], op=ALU.mult),
                          r=[kf, E3], w=[kend])
                    for h in range(4):
                        kb.op('pe', lambda h=h: P_.matmul(PU[h // 2][:, (h % 2) * 256:(h % 2 + 1) * 256],
                                                          lhsT=kend[:, h * 128:(h + 1) * 128],
                                                          rhs=vb[:, h * 256:(h + 1) * 256], start=True, stop=True),
                              r=[kend, vb], w=[PU[h // 2]])
                    yield
                    if p == 0:
                        kb.op('dve', lambda: V_.tensor_scalar(out=Ssel[:], in0=S[:], scalar1=wsel[:, 0:1], scalar2=None,
                                                              op0=ALU.mult), r=[S, wsel], w=[Ssel])
                    else:
                        kb.op('dve', lambda: V_.scalar_tensor_tensor(out=Ssel[:], in0=S[:], scalar=wsel[:, p:p + 1],
                                                                     in1=Ssel[:], op0=ALU.mult, op1=ALU.add),
                              r=[S, wsel, Ssel], w=[Ssel])
                    for h in range(4):
                        kb.op('dve', lambda h=h: V_.scalar_tensor_tensor(
                            out=S[:, h * 256:(h + 1) * 256], in0=S[:, h * 256:(h + 1) * 256], scalar=dec[:, h:h + 1],
                            in1=PU[h // 2][:, (h % 2) * 256:(h % 2 + 1) * 256], op0=ALU.mult, op1=ALU.add),
                            r=[S, dec, PU[h // 2]], w=[S])
                else:
                    kb.op('pe', lambda: P_.matmul(PB[:], lhsT=lmt[:], rhs=spb[:], start=True, stop=True),
                          r=[lmt, spb], w=[PB])
                    yield
                    kb.op('act', lambda: A_.activation(out=E1[:], in_=PB[:], func=AF.Exp), r=[PB], w=[E1])
                    kb.op('act', lambda: A_.activation(out=E2[:], in_=PB[:], func=AF.Exp, scale=-1.0), r=[PB], w=[E2])
                    g_proj(0)
                    kb.op('dve', lambda: V_.scalar_tensor_tensor(out=qdec[:], in0=qf[:], scalar=128.0 ** -0.5, in1=E1[:],
                                                                 op0=ALU.mult, op1=ALU.mult), r=[qf, E1], w=[qdec])
                    kb.op('dve', lambda: V_.tensor_tensor(out=kinv[:], in0=kf[:], in1=E2[:], op=ALU.mult),
                          r=[kf, E2], w=[kinv])
                    g_proj(1)
                    for h in range(4):
                        kb.op('pe', lambda h=h: P_.transpose(out=PT[:, h, :], in_=qdec[:, h * 128:(h + 1) * 128],
                                                             identity=ident[:]), r=[qdec, ident], w=[PT])
                        kb.op('pe', lambda h=h: P_.transpose(out=PT[:, 4 + h, :], in_=kinv[:, h * 128:(h + 1) * 128],
                                                             identity=ident[:]), r=[kinv, ident], w=[PT])
                    yield
                    kb.op('act', lambda: A_.copy(out=qkT[:], in_=PT[:]), r=[PT], w=[qkT])
                    for h in range(4):
                        kb.op('pe', lambda h=h: P_.matmul(PB[:, h * 128:(h + 1) * 128], lhsT=qkT[:, 4 + h, :],
                                                          rhs=qkT[:, h, :], start=True, stop=True), r=[qkT], w=[PB])
                    kb.op('dve', lambda: V_.tensor_tensor(out=attT[:], in0=PB[:], in1=caus4[:], op=ALU.mult),
                          r=[PB, caus4], w=[attT])
                    yield
                    kb.op('pool', lambda: G_.tensor_copy(out=Sbf[:], in_=Ssel[:]), r=[Ssel], w=[Sbf])
                    for h in range(4):
                        o_ap = PU[h // 2][:, (h % 2) * 256:(h % 2 + 1) * 256]
                        kb.op('pe', lambda h=h, o_ap=o_ap: P_.matmul(o_ap, lhsT=attT[:, h * 128:(h + 1) * 128],
                                                                     rhs=vb[:, h * 256:(h + 1) * 256], start=True, stop=False),
                              r=[attT, vb], w=[PU[h // 2]])
                        kb.op('pe', lambda h=h, o_ap=o_ap: P_.matmul(o_ap, lhsT=qkT[:, h, :],
                                                                     rhs=Sbf[:, h * 256:(h + 1) * 256], start=False, stop=True),
                              r=[qkT, Sbf], w=[PU[h // 2]])
                    for h in range(4):
                        kb.op('act', lambda h=h: A_.activation(out=junk[:, 0:256], in_=PU[h // 2][:, (h % 2) * 256:(h % 2 + 1) * 256],
                                                               func=AF.Square, accum_out=st[:, 4 + h:5 + h]),
                              r=[PU[h // 2]], w=[junk, st])
                    yield
                    rstd_from_ss(st[:, 4:8], 4, 256.0, st)
                    for h in range(4):
                        kb.op('pool', lambda h=h: G_.tensor_tensor(out=sg[:, h * 256:(h + 1) * 256], in0=sg[:, h * 256:(h + 1) * 256],
                                                                   in1=gnorm[:], op=ALU.mult), r=[sg, gnorm], w=[sg])
                    for h in range(4):
                        kb.op('dve', lambda h=h: V_.scalar_tensor_tensor(
                            out=ogl[:, h * 256:(h + 1) * 256], in0=PU[h // 2][:, (h % 2) * 256:(h % 2 + 1) * 256],
                            scalar=st[:, 4 + h:5 + h], in1=sg[:, h * 256:(h + 1) * 256], op0=ALU.mult, op1=ALU.mult),
                            r=[PU[h // 2], st, sg], w=[ogl])
                    for c in range(8):
                        kb.op('pe', lambda c=c: P_.transpose(out=PT[:, c, :], in_=ogl[:, c * 128:(c + 1) * 128],
                                                             identity=ident[:]), r=[ogl, ident], w=[PT])
                    ogT = ogTs.next()
                    kb.op('act', lambda: A_.copy(out=ogT[:], in_=PT[:]), r=[PT], w=[ogT])
                    kb.dma('sp', ogla_scr[slot], ogT[:].rearrange("p a b -> p (a b)"), ogT, load=False)

            items = []
            for m in range(NOWN):
                for p in range(4):
                    blk = 4 * m + p
                    items.append((g_pre(x_full[blk * 128:(blk + 1) * 128, :], blk),
                                  lambda pre, p=p, m=m: gla_block(pre, False, p, m)))
                items.append((g_pre(x_own[m * 128:(m + 1) * 128, :]), lambda pre, m=m: gla_block(pre, True, 0, m)))
            pipeline2(items, depth=2, lag=4, ahead=2)
            kb.barrier()

        KT = sb(top, "KT", [128, 2, TT], BF16)
        VV = sb(top, "VV", [128, NB, 256], BF16)
        kidxT = sb(top, "kidxT", [128, TT // 2], BF16)
        with ExitStack() as ph:
            Wk = sb(ph, "k_W", [128, 8, 576], BF16)
            g_mix = sb(ph, "g_mixt", [128, 1024], F32)
            kg2 = sb(ph, "kg2", [128, 256], F32)
            ikg = sb(ph, "ikg", [128, 128], F32)
            load_gain(g_mix, "g_mix", 1024)
            load_gain(kg2, "dsa_k_norm", 128, rep=2)
            load_gain(ikg, "idx_k_norm", 64, rep=2)
            xts = Rot([sb(ph, "k_x%d" % i, [128, 1024], F32) for i in range(4)])
            junk = JunkRot([sb(ph, "k_junk%d" % i, [128, 1024], BF16) for i in range(3)])
            sts = Rot([sb(ph, "k_st%d" % i, [128, 8], F32) for i in range(4)])
            hbs = Rot([sb(ph, "k_hb%d" % i, [128, 1024], BF16) for i in range(4)])
            hTs = Rot([sb(ph, "k_hT%d" % i, [128, 8, 128], BF16) for i in range(3)])
            knbs = Rot([sb(ph, "k_knb%d" % i, [128, 256], BF16) for i in range(3)])
            ikbs = Rot([sb(ph, "k_ikb%d" % i, [128, 128], BF16) for i in range(3)])
            PJs = Rot([ps(ph, "k_PJ%d" % i, [128, 512]) for i in range(2)])
            PI = ps(ph, "k_PI", [128, 512])
            PT = ps(ph, "k_PT", [128, 8, 128], BF16)
            PT2 = ps(ph, "k_PT2", [128, 4, 128], BF16)
            kb.dma('pool', Wk[:, :, 0:512], wchunks(w_in, OFF['dk'], 512), Wk)
            kb.dma('pool', Wk[:, :, 512:576], wchunks(w_in, OFF['ik'], 64), Wk)
            def k_pre(blk):
                def f():
                    xt = xts.next(); st = sts.next(); hb = hbs.next()
                    kb.dma('sp', xt[:], x_full[blk * 128:(blk + 1) * 128, :], xt)
                    kb.op('dve', lambda: V_.scalar_tensor_tensor(out=hb[:], in0=xt[:], scalar=rstd_all[:, blk:blk + 1], in1=g_mix[:],
                                                                 op0=ALU.mult, op1=ALU.mult), r=[xt, rstd_all, g_mix], w=[hb])
                    return (st, hb)
                return f

            def k_block(blk, pre):
                st, hb = pre
                hT = hTs.next(); knb = knbs.next(); ikb = ikbs.next()
                transp_T(hb, PT, hT[:], hT)
                yield
                hf = lambda c, hT=hT: hT[:, c, :]
                pj = PJs.next()
                proj(hf, hT, Wk, 0, 512, pj)
                proj(hf, hT, Wk, 512, 64, PI)
                yield
                kb.op('act', lambda pj=pj, blk=blk: A_.copy(out=VV[:, blk, :], in_=pj[:, 256:512]), r=[pj], w=[])
                for g in range(2):
                    kb.op('act', lambda g=g, pj=pj, st=st: A_.activation(out=junk[:, 0:128], in_=pj[:, g * 128:(g + 1) * 128],
                                                                         func=AF.Square, accum_out=st[:, 4 + g:5 + g]),
                          r=[pj], w=[junk, st])
                kb.op('act', lambda st=st: A_.activation(out=junk[:, 0:64], in_=PI[:, 0:64], func=AF.Square,
                                                         accum_out=st[:, 6:7]), r=[PI], w=[junk, st])
                yield
                rstd_from_ss(st[:, 4:6], 2, 128.0, st)
                rstd_from_ss(st[:, 6:7], 1, 64.0, st)
                for g in range(2):
                    kb.op('dve', lambda g=g, pj=pj, st=st: V_.scalar_tensor_tensor(
                        out=knb[:, g * 128:(g + 1) * 128], in0=pj[:, g * 128:(g + 1) * 128], scalar=st[:, 4 + g:5 + g],
                        in1=kg2[:, g * 128:(g + 1) * 128], op0=ALU.mult, op1=ALU.mult), r=[pj, st, kg2], w=[knb])
                for hh in range(2):
                    kb.op('dve', lambda st=st, hh=hh: V_.scalar_tensor_tensor(
                        out=ikb[:, hh * 64:(hh + 1) * 64], in0=PI[:, 0:64], scalar=st[:, 6:7], in1=ikg[:, 0:64],
                        op0=ALU.mult, op1=ALU.mult), r=[PI, st, ikg], w=[ikb])
                yield
                for g in range(2):
                    kb.op('pe', lambda g=g: P_.transpose(out=PT2[:, g, :], in_=knb[:, g * 128:(g + 1) * 128],
                                                         identity=ident[:]), r=[knb, ident], w=[PT2])
                kb.op('pe', lambda: P_.transpose(out=PT2[:, 2, :], in_=ikb[:], identity=ident[:]),
                      r=[ikb, ident], w=[PT2])
                kb.op('act', lambda blk=blk: A_.copy(out=KT[:, :, blk * 128:(blk + 1) * 128], in_=PT2[:, 0:2, :]),
                      r=[PT2], w=[])
                hp = 0 if blk < NB // 2 else 64
                bl = blk if blk < NB // 2 else blk - NB // 2
                kb.op('act', lambda bl=bl, hp=hp: A_.copy(out=kidxT[hp:hp + 64, bl * 128:(bl + 1) * 128],
                                                          in_=PT2[hp:hp + 64, 2, :]), r=[PT2], w=[])
            pipeline2([(k_pre(blk), lambda pre, blk=blk: k_block(blk, pre)) for blk in range(NB)], depth=2, lag=2, ahead=2)
            if dbg:
                dbgs['KT'] = nc.dram_tensor("dbg_KT", [128, 2 * TT], BF16, kind="ExternalOutput")
                kb.dma('sp', dbgs['KT'].ap(), KT[:].rearrange("p a b -> p (a b)"), KT, load=False)
                dbgs['kidxT'] = nc.dram_tensor("dbg_kidxT", [128, TT // 2], BF16, kind="ExternalOutput")
                kb.dma('sp', dbgs['kidxT'].ap(), kidxT[:], kidxT, load=False)
            kb.barrier()

        U8 = mybir.dt.uint8
        for j in range(NTILE):
            with ExitStack() as tl:
                hTo = sb(tl, "c_hTo", [128, 8, 512], BF16)
                OdT = sb(tl, "c_OdT", [128, 8, 512], BF16)
                with ExitStack() as ph:
                    g_mix = sb(ph, "g_mixt", [128, 1024], F32)
                    xt = sb(ph, "c0_x", [128, 1024], F32)
                    junk = JunkRot([sb(ph, "c0_junk%d" % i, [128, 1024], BF16) for i in range(3)])
                    st = sb(ph, "c0_st", [128, 8], F32)
                    hbs4 = [sb(ph, "c0_hb%d" % i, [128, 1024], BF16) for i in range(4)]
                    xts2 = Rot([xt, sb(ph, "c0_x2", [128, 1024], F32)])
                    sts4 = [sb(ph, "c0_st%d" % i, [128, 8], F32) for i in range(4)]
                    PTs = Rot([ps(ph, "c0_PT%d" % i, [128, 8, 128], BF16) for i in range(2)])
                    load_gain(g_mix, "g_mix", 1024)
                    for s in range(4):
                        m = 4 * j + s
                        xt_ = xts2.next()
                        kb.dma('sp', xt_[:], x_own[m * 128:(m + 1) * 128, :], xt_)
                        norm_only(xt_, g_mix, junk, sts4[s], hbs4[s])
                    for s in range(4):
                        transp_T(hbs4[s], PTs.next(), hTo[:, :, s * 128:(s + 1) * 128], hTo)
                    kb.barrier()
                with ExitStack() as c1:
                    QT = sb(c1, "c1_QT", [128, 8, 512], BF16)
                    IQT = sb(c1, "c1_IQT", [128, 8, 512], BF16)
                    wt = sb(c1, "c1_wt", [128, 4, 8], F32)
                    wabs = sb(c1, "c1_wabs", [128, 4, 8], F32)
                    wsgn = sb(c1, "c1_wsgn", [128, 4, 8], F32)
                    with ExitStack() as ph:
                        qg8 = sb(ph, "qg8", [128, 1024], F32)
                        wbufs = Rot([sb(ph, "c1a_w%d" % i, [128, 8, 512], BF16) for i in range(2)])
                        wiw = sb(ph, "c1a_wiw", [128, 8, 8], BF16)
                        qn = sb(ph, "c1a_qn", [128, 512], BF16)
                        iqd = sb(ph, "c1a_iqd", [128, 8, 2, 64], BF16)
                        junk = JunkRot([sb(ph, "c1a_junk%d" % i, [128, 128], BF16) for i in range(3)])
                        st = sb(ph, "c1a_st", [128, 8], F32)
                        PJs = Rot([ps(ph, "c1a_PJ%d" % i, [128, 512]) for i in range(2)])
                        PJw = ps(ph, "c1a_PJw", [128, 512])
                        PT = ps(ph, "c1a_PT", [128, 8, 128], BF16)
                        load_gain(qg8, "dsa_q_norm", 128, rep=8, scale=128.0 ** -0.5)
                        kb.dma('pool', wiw[:], wchunks(w_in, OFF['iw'], 8), wiw)
                        pend = None
                        for wi, col0 in enumerate((OFF['dq'], OFF['dq'] + 512, OFF['iq'])):
                            wb = wbufs.next()
                            kb.dma('pool', wb[:], wchunks(w_in, col0, 512), wb)
                            for s in range(4):
                                hf = lambda c, s=s: hTo[:, c, s * 128:(s + 1) * 128]
                                pj = PJs.next()
                                proj(hf, hTo, wb, 0, 512, pj)
                                if pend is not None:
                                    pend()
                                def post(wi=wi, s=s, pj=pj, hf=hf):
                                    if wi < 2:
                                        for hh in range(4):
                                            kb.op('act', lambda hh=hh, pj=pj: A_.activation(
                                                out=junk[:], in_=pj[:, hh * 128:(hh + 1) * 128], func=AF.Square,
                                                accum_out=st[:, hh:hh + 1]), r=[pj], w=[junk, st])
                                        rstd_from_ss(st[:, 0:4], 4, 128.0, st)
                                        for hh in range(4):
                                            kb.op('dve', lambda hh=hh, pj=pj, wi=wi: V_.scalar_tensor_tensor(
                                                out=qn[:, hh * 128:(hh + 1) * 128], in0=pj[:, hh * 128:(hh + 1) * 128],
                                                scalar=st[:, hh:hh + 1], in1=qg8[:, (wi * 4 + hh) * 128:(wi * 4 + hh + 1) * 128],
                                                op0=ALU.mult, op1=ALU.mult), r=[pj, st, qg8], w=[qn])
                                        for hh in range(4):
                                            kb.op('pe', lambda hh=hh: P_.transpose(out=PT[:, hh, :], in_=qn[:, hh * 128:(hh + 1) * 128],
                                                                                   identity=ident[:]), r=[qn, ident], w=[PT])
                                        kb.op('act', lambda wi=wi, s=s: A_.copy(out=QT[:, wi * 4:wi * 4 + 4, s * 128:(s + 1) * 128],
                                                                                in_=PT[:, 0:4, :]), r=[PT], w=[QT])
                                    else:
                                        pv = pj[:, :].rearrange("p (h d) -> p h d", h=8)
                                        for dd in range(2):
                                            kb.op('act', lambda dd=dd, pv=pv: A_.copy(out=iqd[:, :, dd, :], in_=pv), r=[pj], w=[iqd])
                                        for h in range(8):
                                            kb.op('pe', lambda h=h: P_.transpose(
                                                out=PT[:, h, :], in_=iqd[:, h, :, :].rearrange("p a b -> p (a b)"),
                                                identity=ident[:]), r=[iqd, ident], w=[PT])
                                        kb.op('act', lambda s=s: A_.copy(out=IQT[:, :, s * 128:(s + 1) * 128], in_=PT[:]),
                                              r=[PT], w=[IQT])
                                        pj2 = PJw
                                        proj(hf, hTo, wiw, 0, 8, pj2)
                                        kb.op('dve', lambda s=s, pj2=pj2: V_.tensor_scalar(
                                            out=wt[:, s, :], in0=pj2[:, 0:8], scalar1=(8.0 ** -0.5) * (64.0 ** -0.5), scalar2=None,
                                            op0=ALU.mult), r=[pj2], w=[wt])
                                        kb.op('dve', lambda s=s: V_.tensor_scalar(out=wsgn[:, s, :], in0=wt[:, s, :], scalar1=0.0, scalar2=2.0,
                                                                                   op0=ALU.is_ge, op1=ALU.mult), r=[wt], w=[wsgn])
                                        kb.op('dve', lambda s=s: V_.tensor_scalar(out=wsgn[:, s, :], in0=wsgn[:, s, :], scalar1=-1.0, scalar2=None,
                                                                                   op0=ALU.add), r=[wsgn], w=[wsgn])
                                        kb.op('dve', lambda s=s: V_.tensor_tensor(out=wabs[:, s, :], in0=wt[:, s, :], in1=wsgn[:, s, :], op=ALU.mult),
                                              r=[wt, wsgn], w=[wabs])
                                pend = post
                        pend()
                        kb.barrier()
                    with ExitStack() as ph:
                        NKT = 4 * (j + 1)
                        scs = [sb(ph, "c1b_sc%d" % i, [128, NKT * 512], F32) for i in range(2)]
                        jk = sb(ph, "c1b_jk", [128, 2048], U8)
                        cmask = sb(ph, "cmaskt", [128, 512], F32)
                        cbis = sb(ph, "cbis", [128, NBIS], F32)
                        Rs = Rot([sb(ph, "c1b_R%d" % i, [128, 512], BF16) for i in range(2)])
                        Ps = Rot([sb(ph, "c1b_P%d" % i, [128, 512], BF16) for i in range(3)])
                        nmt = [sb(ph, "c1b_nm%d" % i, [128, 512], BF16) for i in range(NKT)]
                        Dm = sb(ph, "c1b_Dm", [128, 8, 128], BF16)
                        rec = sb(ph, "c1b_rec", [128, 512], F32)
                        bss = [sb(ph, "c1b_bs%d" % i, [128, 8], F32) for i in range(2)]
                        dl = sb(ph, "c1b_dl", [128, NBIS], F32)
                        PSIs = Rot([ps(ph, "c1b_PI%d" % i, [128, 512]) for i in range(2)])
                        PSC = ps(ph, "c1b_PC", [128, 512])
                        PSSs = Rot([ps(ph, "c1b_PS%d" % i, [128, 512]) for i in range(3)])
                        PSO = ps(ph, "c1b_PO", [128, 512])
                        PSM = ps(ph, "c1b_PM", [128, 512])
                        kb.dma('sp', cmask[:], cmask_d, cmask)
                        kb.dma('sp', cbis[:], c_bis, cbis)

                        def c1_index(s):
                            m = 4 * j + s
                            scores = scs[s % 2]
                            tok = slice(s * 128, (s + 1) * 128)
                            pend = None
                            for kt in range(m + 1):
                                hp = 0 if kt * 512 < TT // 2 else 64
                                kc = kt * 512 - (0 if hp == 0 else TT // 2)
                                for h in range(8):
                                    psi = PSIs.next()
                                    kb.op('pe', lambda: P_.matmul(
                                        psi[:], lhsT=IQT[hp:hp + 64, h, tok], rhs=kidxT[hp:hp + 64, kc:kc + 512],
                                        start=True, stop=True), r=[IQT, kidxT], w=[psi])
                                    R = Rs.next()
                                    kb.op('act', lambda: A_.activation(out=R[:], in_=psi[:], func=AF.Relu, scale=wabs[:, s, h:h + 1]),
                                          r=[psi, wabs], w=[R])
                                    if pend is not None:
                                        pend()
                                    def acc(h=h, R=R, kt=kt):
                                        kb.op('pe', lambda: P_.matmul(PSC[:], lhsT=Dm[:, h, :], rhs=R[:], start=(h == 0), stop=(h == 7)),
                                              r=[Dm, R], w=[PSC])
                                        if h == 7:
                                            kb.op('act', lambda: A_.copy(out=scores[:, kt * 512:(kt + 1) * 512], in_=PSC[:]),
                                                  r=[PSC], w=[scores])
                                    pend = acc
                                    if h % 4 == 3:
                                        yield 2.7
                            pend()
                            yield 0.5

                        def c1_dm(s):
                            for h in range(8):
                                kb.op('dve', lambda: V_.tensor_scalar(out=Dm[:, h, :], in0=ident[:], scalar1=wsgn[:, s, h:h + 1],
                                                                      scalar2=None, op0=ALU.mult), r=[ident, wsgn], w=[Dm])

                        def c1_bisect(s):
                            m = 4 * j + s
                            nk = 512 * (m + 1)
                            scores = scs[s % 2]
                            bs = bss[s % 2]
                            kb.op('dve', lambda: V_.tensor_reduce(out=bs[:, 0:1], in_=scores[:, 0:nk], axis=AX.X, op=ALU.max,
                                                                  apply_absolute_value=True), r=[scores], w=[bs])
                            kb.op('dve', lambda: V_.tensor_tensor(out=scores[:, m * 512:(m + 1) * 512],
                                                                  in0=scores[:, m * 512:(m + 1) * 512], in1=cmask[:], op=ALU.add),
                                  r=[scores, cmask], w=[scores])
                            kb.op('dve', lambda: V_.tensor_scalar(out=dl[:], in0=cbis[:], scalar1=bs[:, 0:1], scalar2=None,
                                                                  op0=ALU.mult), r=[cbis, bs], w=[dl])
                            kb.op('dve', lambda: V_.tensor_scalar(out=bs[:, 1:2], in0=bs[:, 0:1], scalar1=-1.0, scalar2=None,
                                                                  op0=ALU.mult), r=[bs], w=[bs])
                            yield nk / 850.0 + 1.5
                            for k in range(NBIS):
                                kb.op('dve', lambda: V_.tensor_tensor(out=bs[:, 2:3], in0=bs[:, 1:2], in1=dl[:, k:k + 1],
                                                                      op=ALU.add), r=[bs, dl], w=[bs])
                                for ci, c0 in enumerate(range(0, nk, 2048)):
                                    cw = min(2048, nk - c0)
                                    kb.op('dve', lambda: V_.tensor_scalar(
                                        out=jk[:, 0:cw], in0=scores[:, c0:c0 + cw], scalar1=bs[:, 2:3],
                                        scalar2=(None if ci == 0 else bs[:, 3:4]), op0=ALU.is_ge, op1=ALU.add,
                                        accum_out=bs[:, 3:4]), r=[scores, bs], w=[jk, bs])
                                kb.op('dve', lambda: V_.scalar_tensor_tensor(out=bs[:, 4:5], in0=bs[:, 3:4], scalar=255.5,
                                                                             in1=dl[:, k:k + 1], op0=ALU.is_ge, op1=ALU.mult),
                                      r=[bs, dl], w=[bs])
                                kb.op('dve', lambda: V_.tensor_tensor(out=bs[:, 1:2], in0=bs[:, 1:2], in1=bs[:, 4:5], op=ALU.add),
                                      r=[bs], w=[bs])
                                yield nk / 850.0 + 1.3

                        def c1_nm(s):
                            m = 4 * j + s
                            scores = scs[s % 2]
                            bs = bss[s % 2]
                            for kt in range(m + 1):
                                kb.op('dve', lambda: V_.tensor_scalar(
                                    out=nmt[kt][:], in0=scores[:, kt * 512:(kt + 1) * 512], scalar1=bs[:, 1:2], scalar2=NEG,
                                    op0=ALU.is_lt, op1=ALU.mult), r=[scores, bs], w=[nmt[kt]])

                        def c1_attn(s):
                            m = 4 * j + s
                            tok = slice(s * 128, (s + 1) * 128)
                            nkb = 4 * (m + 1)
                            for g in range(2):
                                pend = None
                                for kb_ in range(nkb):
                                    nm = nmt[kb_ // 4]
                                    pss = PSSs.next()
                                    kb.op('pe', lambda: P_.matmul(
                                        pss[:], lhsT=nm[:, (kb_ % 4) * 128:(kb_ % 4 + 1) * 128], rhs=i4[:], start=True, stop=False),
                                        r=[nm, i4], w=[pss])
                                    kb.op('pe', lambda: P_.matmul(
                                        pss[:], lhsT=KT[:, g, kb_ * 128:(kb_ + 1) * 128], rhs=QT[:, 4 * g:4 * g + 4, tok],
                                        start=False, stop=True), r=[KT, QT], w=[pss])
                                    Pb = Ps.next()
                                    kb.op('act', lambda: A_.activation(out=Pb[:], in_=pss[:], func=AF.Exp), r=[pss], w=[Pb])
                                    if pend is not None:
                                        pend()
                                    def pv(kb_=kb_, Pb=Pb, g=g):
                                        kb.op('pe', lambda: P_.matmul(
                                            PSO[:], lhsT=VV[:, kb_, g * 128:(g + 1) * 128], rhs=Pb[:],
                                            start=(kb_ == 0), stop=(kb_ == nkb - 1)), r=[VV, Pb], w=[PSO])
                                        kb.op('pe', lambda: P_.matmul(
                                            PSM[:], lhsT=ones[:], rhs=Pb[:], start=(kb_ == 0), stop=(kb_ == nkb - 1)),
                                            r=[ones, Pb], w=[PSM])
                                    pend = pv
                                    yield 1.1
                                pend()
                                kb.op('dve', lambda: V_.reciprocal(out=rec[:], in_=PSM[:]), r=[PSM], w=[rec])
                                kb.op('dve', lambda: V_.tensor_tensor(
                                    out=OdT[:, 4 * g:4 * g + 4, tok], in0=PSO[:, :].rearrange("p (h q) -> p h q", h=4),
                                    in1=rec[:, :].rearrange("p (h q) -> p h q", h=4), op=ALU.mult), r=[PSO, rec], w=[OdT])
                                yield 1.0

                        def weave3(gens):
                            acc_t = [0.0 for _ in gens]
                            live = [g is not None for g in gens]
                            while any(live):
                                i = min((k for k in range(len(gens)) if live[k]), key=lambda k: acc_t[k])
                                try:
                                    acc_t[i] += next(gens[i])
                                except StopIteration:
                                    live[i] = False

                        c1_dm(0)
                        for t in range(-2, 4):
                            gl = []
                            if 0 <= t + 2 < 4:
                                gl.append(c1_index(t + 2))
                            if 0 <= t + 1 < 4:
                                gl.append(c1_bisect(t + 1))
                            if 0 <= t < 4:
                                gl.append(c1_attn(t))
                            weave3(gl)
                            if 0 <= t + 3 < 4:
                                c1_dm(t + 3)
                            if 0 <= t + 1 < 4:
                                c1_nm(t + 1)
                        if dbg and j == 0:
                            dbgs['OdT'] = nc.dram_tensor("dbg_OdT", [128, 8 * 512], BF16, kind="ExternalOutput")
                            kb.dma('sp', dbgs['OdT'].ap(), OdT[:].rearrange("p a b -> p (a b)"), OdT, load=False)
                        kb.barrier()
                OmT = sb(tl, "c_OmT", [128, 8, 512], BF16)
                x1 = sb(tl, "c_x1", [128, 4, 1024], F32)
                s23 = ExitStack()
                c3wbufs = Rot([sb(s23, "c3_w%d" % i, [128, 8, 1024], BF16) for i in range(2)])
                c3bgs = Rot([sb(s23, "c3_bg%d" % i, [1, 512], BF16) for i in range(2)])
                def c3_load(br, nt):
                    wbuf = c3wbufs.next(); bg = c3bgs.next()
                    kb.dma('pool', wbuf[:, :, 0:512], wchunks(w_in, OFF['gates'] + br * 1024 + nt * 512, 512), wbuf)
                    kb.dma('pool', wbuf[:, :, 512:1024], wchunks(w_o[br], nt * 512, 512), wbuf)
                    kb.dma('pool', bg[:], vecs["b_gate"].ap()[:, br * 1024 + nt * 512:br * 1024 + (nt + 1) * 512], bg)
                    return wbuf, bg

                def c3_load_out():
                    wbuf = c3wbufs.next()
                    kb.dma('pool', wbuf[:, :, 0:512], wchunks(w_out, 0, 512), wbuf)
                    kb.dma('pool', wbuf[:, :, 512:1024], wchunks(w_out, 512, 512), wbuf)
                    return wbuf

                c3_first = c3_load(0, 0)
                with ExitStack() as ph:
                    mqg4 = sb(ph, "mqg4", [128, 1024], F32)
                    wbufs = Rot([sb(ph, "c2_w%d" % i, [128, 8, 512], BF16) for i in range(2)])
                    mqf = sb(ph, "c2_mqf", [128, 4, 1024], F32)
                    mqb = sb(ph, "c2_mqb", [128, 1024], BF16)
                    MQT = sb(ph, "c2_MQT", [128, 8, 512], BF16)
                    junk = JunkRot([sb(ph, "c2_junk%d" % i, [128, 256], BF16) for i in range(3)])
                    st = sb(ph, "c2_st", [128, 8], F32)
                    Pms = Rot([sb(ph, "c2_P%d" % i, [128, 512], BF16) for i in range(2)])
                    rec = sb(ph, "c2_rec", [128, 512], F32)
                    PJs = Rot([ps(ph, "c2_PJ%d" % i, [128, 512]) for i in range(2)])
                    PT = ps(ph, "c2_PT", [128, 8, 128], BF16)
                    PSs = Rot([ps(ph, "c2_PS%d" % i, [128, 512]) for i in range(2)])
                    PO2 = [ps(ph, "c2_PO%d" % i, [128, 512]) for i in range(2)]
                    PM2 = ps(ph, "c2_PM", [128, 512])
                    load_gain(mqg4, "mem_q_norm", 256, rep=4, scale=256.0 ** -0.5)
                    for nt in range(2):
                        wb = wbufs.next()
                        kb.dma('pool', wb[:], wchunks(w_in, OFF['mq'] + nt * 512, 512), wb)
                        for s in range(4):
                            pj = PJs.next()
                            proj(lambda c, s=s: hTo[:, c, s * 128:(s + 1) * 128], hTo, wb, 0, 512, pj)
                            kb.op('act', lambda pj=pj, s=s, nt=nt: A_.copy(out=mqf[:, s, nt * 512:(nt + 1) * 512], in_=pj[:]),
                                  r=[pj], w=[mqf])
                    mqbs = [mqb, mqb, mqb, mqb]
                    sts4 = [st] + [sb(ph, "c2_st%d" % i, [128, 8], F32) for i in range(1, 4)]

                    def c2_qnorm(s):
                        st_ = sts4[s]; mqb_ = mqbs[s]
                        for h in range(4):
                            kb.op('act', lambda h=h: A_.activation(out=junk[:], in_=mqf[:, s, h * 256:(h + 1) * 256],
                                                                   func=AF.Square, accum_out=st_[:, h:h + 1]),
                                  r=[mqf], w=[junk, st_])
                        yield
                        rstd_from_ss(st_[:, 0:4], 4, 256.0, st_)
                        yield
                        for h in range(4):
                            kb.op('dve', lambda h=h: V_.scalar_tensor_tensor(
                                out=mqb_[:, h * 256:(h + 1) * 256], in0=mqf[:, s, h * 256:(h + 1) * 256], scalar=st_[:, h:h + 1],
                                in1=mqg4[:, h * 256:(h + 1) * 256], op0=ALU.mult, op1=ALU.mult), r=[mqf, st_, mqg4], w=[mqb_])
                        yield
                        for c8 in range(8):
                            kb.op('pe', lambda c8=c8: P_.transpose(out=PT[:, c8, :], in_=mqb_[:, c8 * 128:(c8 + 1) * 128],
                                                                   identity=ident[:]), r=[mqb_, ident], w=[PT])
                        kb.op('act', lambda: A_.copy(out=MQT[:, :, s * 128:(s + 1) * 128], in_=PT[:]), r=[PT], w=[MQT])

                    pipeline([c2_qnorm(s) for s in range(4)], depth=4, lag=1)
                    for h in range(4):
                        pend = None
                        for mc in range(2):
                            pss = PSs.next()
                            for c in range(2):
                                kb.op('pe', lambda pss=pss, h=h, mc=mc, c=c: P_.matmul(
                                    pss[:], lhsT=memKT[:, h * 2 + c, mc * 128:(mc + 1) * 128], rhs=MQT[:, h * 2 + c, :],
                                    start=(c == 0), stop=(c == 1)), r=[memKT, MQT], w=[pss])
                            Pm = Pms.next()
                            kb.op('act', lambda pss=pss, Pm=Pm: A_.activation(out=Pm[:], in_=pss[:], func=AF.Exp),
                                  r=[pss], w=[Pm])
                            if pend is not None:
                                pend()
                            def pvm(Pm=Pm, h=h, mc=mc):
                                for vc in range(2):
                                    kb.op('pe', lambda vc=vc: P_.matmul(
                                        PO2[vc][:], lhsT=memV[:, mc, h * 256 + vc * 128:h * 256 + (vc + 1) * 128], rhs=Pm[:],
                                        start=(mc == 0), stop=(mc == 1)), r=[memV, Pm], w=[PO2[vc]])
                                kb.op('pe', lambda: P_.matmul(PM2[:], lhsT=ones[:], rhs=Pm[:], start=(mc == 0),
                                                              stop=(mc == 1)), r=[ones, Pm], w=[PM2])
                            pend = pvm
                        pend()
                        kb.op('dve', lambda: V_.reciprocal(out=rec[:], in_=PM2[:]), r=[PM2], w=[rec])
                        for vc in range(2):
                            kb.op('dve', lambda h=h, vc=vc: V_.tensor_tensor(out=OmT[:, h * 2 + vc, :], in0=PO2[vc][:], in1=rec[:],
                                                                             op=ALU.mult), r=[PO2[vc], rec], w=[OmT])
                    kb.barrier()
                with ExitStack() as ph:
                    gt = sb(ph, "c3_gt", [128, 512], F32)
                    tmp = sb(ph, "c3_tmp", [128, 512], F32)
                    mbf = sb(ph, "c3_mbf", [128, 1024], BF16)
                    mT = sb(ph, "c3_mT", [128, 8, 128], BF16)
                    xo = sb(ph, "c3_xo", [128, 1024], F32)
                    ogs = [sb(ph, "c3_og%d" % i, [128, 8, 128], BF16) for i in range(4)]
                    for s in range(4):
                        kb.dma('sp', ogs[s][:].rearrange("p a b -> p (a b)"), ogla_scr[4 * j + s], ogs[s])
                    PGs = Rot([ps(ph, "c3_PG%d" % i, [128, 512]) for i in range(2)])
                    PPs = Rot([ps(ph, "c3_PP%d" % i, [128, 512]) for i in range(2)])
                    PT = ps(ph, "c3_PT", [128, 8, 128], BF16)
                    chunks = [(br, nt) for br in range(3) for nt in range(2)]
                    nxt = c3_first
                    for ci, (br, nt) in enumerate(chunks):
                        if True:
                            wbuf, bg = nxt
                            nxt = c3_load(*chunks[ci + 1]) if ci + 1 < len(chunks) else (c3_load_out(), None)
                            for s in range(4):
                                tok = slice(s * 128, (s + 1) * 128)
                                pg = PGs.next()
                                for c in range(8):
                                    kb.op('pe', lambda pg=pg, c=c, tok=tok: P_.matmul(pg[:], lhsT=hTo[:, c, tok], rhs=wbuf[:, c, 0:512],
                                                                                      start=(c == 0), stop=False), r=[hTo, wbuf], w=[pg])
                                kb.op('pe', lambda pg=pg: P_.matmul(pg[:], lhsT=ones[0:1, :], rhs=bg[:], start=False, stop=True),
                                      r=[ones, bg], w=[pg])
                                kb.op('act', lambda pg=pg: A_.activation(out=gt[:], in_=pg[:], func=AF.Sigmoid), r=[pg], w=[gt])
                                pp = PPs.next()
                                for c in range(8):
                                    if br == 0:
                                        lh = ogs[s][:, c, :]
                                        lt = ogs[s]
                                    elif br == 1:
                                        lh = OdT[:, c, tok]
                                        lt = OdT
                                    else:
                                        lh = OmT[:, c, tok]
                                        lt = OmT
                                    kb.op('pe', lambda pp=pp, c=c, lh=lh: P_.matmul(pp[:], lhsT=lh, rhs=wbuf[:, c, 512:1024],
                                                                                    start=(c == 0), stop=(c == 7)), r=[lt, wbuf], w=[pp])
                                dst = x1[:, s, nt * 512:(nt + 1) * 512]
                                if br == 0:
                                    kb.op('dve', lambda pp=pp, dst=dst: V_.tensor_tensor(out=dst, in0=gt[:], in1=pp[:], op=ALU.mult),
                                          r=[gt, pp], w=[x1])
                                else:
                                    kb.op('dve', lambda pp=pp: V_.tensor_tensor(out=tmp[:], in0=gt[:], in1=pp[:], op=ALU.mult),
                                          r=[gt, pp], w=[tmp])
                                    kb.op('dve', lambda dst=dst: V_.tensor_tensor(out=dst, in0=dst, in1=tmp[:], op=ALU.add),
                                          r=[tmp, x1], w=[x1])
                    wbuf = nxt[0]
                    mbfs = [mbf, sb(ph, "c3_mbf2", [128, 1024], BF16)]
                    mTs = [mT, sb(ph, "c3_mT2", [128, 8, 128], BF16)]

                    def c3_prep(s):
                        mb_, mt_ = mbfs[s % 2], mTs[s % 2]
                        kb.op('act', lambda: A_.copy(out=mb_[:], in_=x1[:, s, :]), r=[x1], w=[mb_])
                        for c in range(8):
                            kb.op('pe', lambda c=c: P_.transpose(out=PT[:, c, :], in_=mb_[:, c * 128:(c + 1) * 128],
                                                                 identity=ident[:]), r=[mb_, ident], w=[PT])
                        kb.op('act', lambda: A_.copy(out=mt_[:], in_=PT[:]), r=[PT], w=[mt_])

                    c3_prep(0)
                    for s in range(4):
                        m = 4 * j + s
                        if s + 1 < 4:
                            c3_prep(s + 1)
                        mt_ = mTs[s % 2]
                        kb.dma('sp', xo[:], x_own[m * 128:(m + 1) * 128, :], xo)
                        for nt in range(2):
                            pp = PPs.next()
                            for c in range(8):
                                kb.op('pe', lambda pp=pp, c=c, nt=nt: P_.matmul(pp[:], lhsT=mt_[:, c, :], rhs=wbuf[:, c, nt * 512:(nt + 1) * 512],
                                                                                start=(c == 0), stop=(c == 7)), r=[mt_, wbuf], w=[pp])
                            kb.op('dve', lambda pp=pp, s=s, nt=nt: V_.tensor_tensor(
                                out=x1[:, s, nt * 512:(nt + 1) * 512], in0=xo[:, nt * 512:(nt + 1) * 512], in1=pp[:], op=ALU.add),
                                r=[xo, pp], w=[x1])
                    if dbg and j == 0:
                        dbgs['x1'] = nc.dram_tensor("dbg_x1", [128, 4096], F32, kind="ExternalOutput")
                        kb.dma('sp', dbgs['x1'].ap(), x1[:].rearrange("p a b -> p (a b)"), x1, load=False)
                    kb.barrier()
                s23.close()
                with ExitStack() as ph:
                    g_ffn = sb(ph, "g_ffnt", [128, 1024], F32)
                    xnT = sb(ph, "c4_xnT", [128, 8, 512], BF16)
                    junk = JunkRot([sb(ph, "c4_junk%d" % i, [128, 1024], BF16) for i in range(3)])
                    hbs4 = [sb(ph, "c4_hb%d" % i, [128, 1024], BF16) for i in range(4)]
                    sts4 = [sb(ph, "c4_st%d" % i, [128, 8], F32) for i in range(4)]
                    wr = sb(ph, "c4_wr", [128, 8, 20], BF16)
                    brow = sb(ph, "c4_brow", [1, 20], BF16)
                    lgs4 = [sb(ph, "c4_lg%d" % i, [128, 20], F32) for i in range(4)]
                    rts4 = [sb(ph, "c4_rt%d" % i, [128, 64], F32) for i in range(4)]
                    comb = sb(ph, "c4_comb", [128, 4, 16], F32)
                    wgs = Rot([sb(ph, "c4_wg%d" % i, [128, 8, 256], BF16) for i in range(3)])
                    wus = Rot([sb(ph, "c4_wu%d" % i, [128, 8, 256], BF16) for i in range(3)])
                    wds = Rot([sb(ph, "c4_wd%d" % i, [128, 2, 1024], BF16) for i in range(3)])
                    sgs = Rot([sb(ph, "c4_sg%d" % i, [128, 512], F32) for i in range(2)])
                    hid = [sb(ph, "c4_hid%d" % i, [128, 512], BF16) for i in range(2)]
                    PGs = Rot([ps(ph, "c4_PG%d" % i, [128, 512]) for i in range(2)])
                    PUs = Rot([ps(ph, "c4_PU%d" % i, [128, 512]) for i in range(2)])
                    PDs = Rot([ps(ph, "c4_PD%d" % i, [128, 512]) for i in range(2)])
                    PT = ps(ph, "c4_PT", [128, 8, 128], BF16)
                    PR = ps(ph, "c4_PR", [128, 512])
                    load_gain(g_ffn, "g_ffn", 1024)
                    kb.dma('pool', wr[:, :, 0:4], wchunks(w_r1, 0, 4), wr)
                    kb.dma('pool', wr[:, :, 4:20], wchunks(w_r2, 0, 16), wr)
                    kb.dma('pool', brow[:, 0:4], vecs["b_r1"].ap(), brow)
                    kb.dma('pool', brow[:, 4:20], vecs["b_r2"].ap(), brow)
                    BIG = 1.0e4

                    def c4_head(s):
                        hb = hbs4[s]; st = sts4[s]; lg = lgs4[s]; rt = rts4[s]
                        PRs = PR[:, 32 * s:32 * s + 20]
                        tok = slice(s * 128, (s + 1) * 128)
                        kb.op('act', lambda s=s: A_.activation(out=junk[:], in_=x1[:, s, :], func=AF.Square, accum_out=st[:, 0:1]),
                              r=[x1], w=[junk, st])
                        rstd_from_ss(st[:, 0:1], 1, 1024.0, st)
                        yield
                        kb.op('dve', lambda s=s: V_.scalar_tensor_tensor(out=hb[:], in0=x1[:, s, :], scalar=st[:, 0:1], in1=g_ffn[:],
                                                                         op0=ALU.mult, op1=ALU.mult), r=[x1, st, g_ffn], w=[hb])
                        yield
                        for c in range(8):
                            kb.op('pe', lambda c=c: P_.transpose(out=PT[:, c, :], in_=hb[:, c * 128:(c + 1) * 128],
                                                                 identity=ident[:]), r=[hb, ident], w=[PT])
                        kb.op('act', lambda tok=tok: A_.copy(out=xnT[:, :, tok], in_=PT[:]), r=[PT], w=[xnT])
                        yield
                        for c in range(8):
                            kb.op('pe', lambda c=c, tok=tok: P_.matmul(PRs, lhsT=xnT[:, c, tok], rhs=wr[:, c, :],
                                                                       start=(c == 0), stop=False), r=[xnT, wr], w=[PR])
                        kb.op('pe', lambda: P_.matmul(PRs, lhsT=ones[0:1, :], rhs=brow[:], start=False, stop=True),
                              r=[ones, brow], w=[PR])
                        kb.op('dve', lambda: V_.tensor_copy(out=lg[:], in_=PRs), r=[PR], w=[lg])
                        yield
                        dv = lambda fn, r=(), w=(): kb.op('dve', fn, r=[lg, rt] + list(r), w=[rt] + list(w))
                        dv(lambda: V_.tensor_reduce(out=rt[:, 0:1], in_=lg[:, 0:4], axis=AX.X, op=ALU.max))
                        yield
                        dv(lambda: V_.tensor_scalar(out=rt[:, 1:2], in0=rt[:, 0:1], scalar1=-1.0, scalar2=None, op0=ALU.mult))
                        yield
                        kb.op('act', lambda: A_.activation(out=rt[:, 48:52], in_=lg[:, 0:4], func=AF.Exp, bias=rt[:, 1:2],
                                                           accum_out=rt[:, 2:3]), r=[lg, rt], w=[rt])
                        yield
                        dv(lambda: V_.reciprocal(out=rt[:, 3:4], in_=rt[:, 2:3]))
                        yield
                        dv(lambda: V_.tensor_scalar(out=rt[:, 4:8], in0=lg[:, 0:4], scalar1=rt[:, 0:1], scalar2=None, op0=ALU.is_ge))
                        yield
                        dv(lambda: V_.tensor_scalar(out=rt[:, 8:12], in0=rt[:, 4:8], scalar1=BIG, scalar2=-BIG, op0=ALU.mult,
                                                    op1=ALU.add))
                        yield
                        for g in range(4):
                            dv(lambda g=g: V_.tensor_scalar(out=rt[:, 12 + 4 * g:16 + 4 * g], in0=lg[:, 4 + 4 * g:8 + 4 * g],
                                                            scalar1=rt[:, 8 + g:9 + g], scalar2=None, op0=ALU.add))
                            yield
                        dv(lambda: V_.tensor_reduce(out=rt[:, 28:29], in_=rt[:, 12:28], axis=AX.X, op=ALU.max))
                        yield
                        dv(lambda: V_.tensor_scalar(out=rt[:, 29:30], in0=rt[:, 28:29], scalar1=-1.0, scalar2=None, op0=ALU.mult))
                        yield
                        dv(lambda: V_.tensor_scalar(out=rt[:, 30:46], in0=rt[:, 12:28], scalar1=rt[:, 28:29], scalar2=-BIG,
                                                    op0=ALU.is_ge, op1=ALU.mult))
                        yield
                        dv(lambda: V_.tensor_tensor(out=rt[:, 30:46], in0=rt[:, 30:46], in1=rt[:, 12:28], op=ALU.add))
                        yield
                        dv(lambda: V_.tensor_reduce(out=rt[:, 46:47], in_=rt[:, 30:46], axis=AX.X, op=ALU.max))
                        yield
                        kb.op('act', lambda: A_.activation(out=rt[:, 48:64], in_=rt[:, 12:28], func=AF.Exp, bias=rt[:, 29:30]),
                              r=[rt], w=[rt])
                        yield
                        dv(lambda: V_.scalar_tensor_tensor(out=rt[:, 48:64], in0=rt[:, 12:28], scalar=rt[:, 46:47], in1=rt[:, 48:64],
                                                           op0=ALU.is_ge, op1=ALU.mult))
                        yield
                        dv(lambda: V_.tensor_reduce(out=rt[:, 47:48], in_=rt[:, 48:64], axis=AX.X, op=ALU.add))
                        yield
                        dv(lambda: V_.reciprocal(out=rt[:, 47:48], in_=rt[:, 47:48]))
                        yield
                        dv(lambda: V_.tensor_tensor(out=rt[:, 47:48], in0=rt[:, 47:48], in1=rt[:, 3:4], op=ALU.mult))
                        yield
                        dv(lambda s=s: V_.tensor_scalar(out=comb[:, s, :], in0=rt[:, 48:64], scalar1=rt[:, 47:48], scalar2=None,
                                                        op0=ALU.mult), w=[comb])
                        yield
                    pipeline([c4_head(s) for s in range(4)], depth=4, lag=0)
                    hidA = [hid, [sb(ph, "c4_hidB%d" % i, [128, 512], BF16) for i in range(2)]]
                    wsets = {}

                    def c4_load(e):
                        wg_ = wgs.next(); wu_ = wus.next(); wd_ = wds.next()
                        kb.dma('pool', wg_[:], w_gate[e].rearrange("(c p) n -> p c n", p=128), wg_)
                        kb.dma('pool', wu_[:], w_up[e].rearrange("(c p) n -> p c n", p=128), wu_)
                        kb.dma('pool', wd_[:], w_down[e].rearrange("(c p) n -> p c n", p=128), wd_)
                        wsets[e] = (wg_, wu_, wd_)

                    def c4_gu(e, fc):
                        wg_, wu_, _ = wsets[e]
                        hd = hidA[e % 2][fc]
                        pg = PGs.next(); pu = PUs.next()
                        for c in range(8):
                            kb.op('pe', lambda c=c: P_.matmul(
                                pg[:], lhsT=wg_[:, c, fc * 128:(fc + 1) * 128], rhs=xnT[:, c, :], start=(c == 0), stop=(c == 7)),
                                r=[wg_, xnT], w=[pg])
                        for c in range(8):
                            kb.op('pe', lambda c=c: P_.matmul(
                                pu[:], lhsT=wu_[:, c, fc * 128:(fc + 1) * 128], rhs=xnT[:, c, :], start=(c == 0), stop=(c == 7)),
                                r=[wu_, xnT], w=[pu])
                        sg_ = sgs.next()
                        kb.op('act', lambda: A_.activation(out=sg_[:], in_=pg[:], func=AF.Silu), r=[pg], w=[sg_])
                        kb.op('dve', lambda: V_.tensor_tensor(out=hd[:], in0=sg_[:], in1=pu[:], op=ALU.mult),
                              r=[sg_, pu], w=[hd])

                    def c4_down(e, slots):
                        _, _, wd_ = wsets[e]
                        for s in slots:
                            tok = slice(s * 128, (s + 1) * 128)
                            for nt in range(2):
                                pd = PDs.next()
                                for fc in range(2):
                                    hd = hidA[e % 2][fc]
                                    kb.op('pe', lambda fc=fc, hd=hd: P_.matmul(
                                        pd[:], lhsT=hd[:, tok], rhs=wd_[:, fc, nt * 512:(nt + 1) * 512], start=(fc == 0), stop=(fc == 1)),
                                        r=[hd, wd_], w=[pd])
                                kb.op('dve', lambda s=s, nt=nt: V_.scalar_tensor_tensor(
                                    out=x1[:, s, nt * 512:(nt + 1) * 512], in0=pd[:], scalar=comb[:, s, e:e + 1],
                                    in1=x1[:, s, nt * 512:(nt + 1) * 512], op0=ALU.mult, op1=ALU.add), r=[pd, comb, x1], w=[x1])

                    c4_load(0)
                    c4_load(1)
                    c4_gu(0, 0)
                    c4_gu(0, 1)
                    for e in range(16):
                        if e + 2 < 16:
                            c4_load(e + 2)
                        if e + 1 < 16:
                            c4_gu(e + 1, 0)
                        c4_down(e, [0, 1])
                        if e + 1 < 16:
                            c4_gu(e + 1, 1)
                        c4_down(e, [2, 3])
                    for s in range(4):
                        m = 4 * j + s
                        kb.dma('sp', out_own[m * 128:(m + 1) * 128, :], x1[:, s, :], x1, load=False)
                    kb.barrier()
    return nc, kb, dbgs


def _consts():
    bf = ml_dtypes.bfloat16
    c = {}
    c["c_ident"] = np.eye(128, dtype=np.float32).astype(bf)
    c["c_i4"] = np.tile(np.eye(128, dtype=np.float32), (1, 4)).astype(bf)
    c["c_ones"] = np.ones((128, 128), np.float32).astype(bf)
    j = np.arange(128)[:, None]
    i = np.arange(128)[None, :]
    c["c_lmt"] = np.where(j <= i, -1.0 / 16.0, 0.0).astype(np.float32)
    c["c_umt"] = np.where(j > i, -1.0 / 16.0, 0.0).astype(np.float32)
    c["c_caus4"] = np.tile(np.where(j <= i, 1.0, 0.0), (1, 4)).astype(np.float32)
    sel = np.zeros((16, 16, 128), np.float32)
    for e in range(16):
        sel[e, e, :] = 1.0
    c["c_sel16"] = sel.reshape(16, 2048).astype(bf)
    c["c_bis"] = np.tile((2.0 ** (1.0 - np.arange(1, NBIS + 1)))[None, :], (128, 1)).astype(np.float32)
    return c


_CACHE = {}


def kernel(x, mem, g_mix, g_mem, w_in, w_gla_a2, b_gla_a2, gla_norm, w_mem_kv,
           dsa_q_norm, dsa_k_norm, idx_k_norm, mem_q_norm, mem_k_norm, b_gate,
           w_o_gla, w_o_dsa, w_o_mem, w_out, g_ffn, w_r1, b_r1, w_r2, b_r2,
           w_gate, w_up, w_down, _dbg=False):
    f = lambda a: np.ascontiguousarray(np.asarray(a, dtype=np.float32))
    x = f(x)
    B, TT, _ = x.shape
    key = (TT, _dbg)
    if key not in _CACHE:
        _CACHE[key] = build_nc(TT, dbg=_dbg)
    nc, kb, dbgs = _CACHE[key]
    NB = TT // 128
    consts = _consts()
    shared = {
        "w_in": f(w_in)[0], "w_gla_a2": f(w_gla_a2)[0], "w_mem_kv": f(w_mem_kv)[0],
        "w_o_gla": f(w_o_gla)[0], "w_o_dsa": f(w_o_dsa)[0], "w_o_mem": f(w_o_mem)[0], "w_out": f(w_out)[0],
        "w_r1": f(w_r1)[0], "w_r2": f(w_r2)[0], "w_gate": f(w_gate)[0], "w_up": f(w_up)[0], "w_down": f(w_down)[0],
        "g_mix": f(g_mix), "g_mem": f(g_mem), "g_ffn": f(g_ffn), "gla_norm": f(gla_norm), "dsa_q_norm": f(dsa_q_norm),
        "dsa_k_norm": f(dsa_k_norm), "idx_k_norm": f(idx_k_norm), "mem_q_norm": f(mem_q_norm), "mem_k_norm": f(mem_k_norm),
        "b_gla_a2": f(b_gla_a2), "b_gate": f(b_gate), "b_r1": f(b_r1), "b_r2": f(b_r2),
    }
    shared.update(consts)
    memf = f(mem)
    in_maps = []
    tpos = np.arange(128)[:, None]
    spos = np.arange(128)[None, :]
    for c in range(8):
        b, r = c // 4, c % 4
        xb = x[b].reshape(NB, 128, D)
        own = np.ascontiguousarray(xb[r::4].reshape(-1, D))
        cm = np.zeros((128, 512), np.float32)
        for p in range(4):
            if p == r:
                cm[:, p * 128:(p + 1) * 128] = np.where(spos <= tpos, 0.0, -1e30)
            elif p > r:
                cm[:, p * 128:(p + 1) * 128] = -1e30
        ws = np.zeros((128, 4), np.float32)
        ws[:, r] = 1.0
        d = dict(shared)
        d.update({"x_full": x[b], "x_own": own, "mem": memf[b], "cmask": cm, "wsel": ws})
        in_maps.append(d)
    res = run_bass_kernel_spmd(nc, in_maps, core_ids=list(range(8)))
    out = np.zeros((B, NB, 128, D), np.float32)
    for c in range(8):
        b, r = c // 4, c % 4
        out[b, r::4] = res.results[c]["out_own"].reshape(NB // 4, 128, D)
    if _dbg:
        kernel.last = res
    return out.reshape(B, TT, D)
```

```python
import numpy as np
import ml_dtypes
from contextlib import ExitStack
import concourse.bass as bass
import concourse.mybir as mybir
from concourse.bass_utils import run_bass_kernel_spmd

F32 = mybir.dt.float32
BF16 = mybir.dt.bfloat16
AF = mybir.ActivationFunctionType
ALU = mybir.AluOpType
AX = mybir.AxisListType

D = 1024
D_IN = 9304
EPS = 1e-6
OFF = dict(gq=0, gk=512, gv=1024, gg=2048, ga=3072, dq=3088, dk=4112, dv=4368, iq=4624,
           ik=5136, iw=5200, mq=5208, gates=6232)
NBIS = 14
NEG = -30000.0


class T:
    def __init__(self, h):
        self.h = h
        self.w = None
        self.r = {}
        self.dsem = None
        self.dcnt = 0
        self.wx = []

    def __getitem__(self, k):
        return self.h[k]


class Rot:
    def __init__(self, tiles):
        self.t = tiles
        self.i = 0

    def next(self):
        t = self.t[self.i % len(self.t)]
        self.i += 1
        return t


class JunkRot:
    def __init__(self, tiles):
        self.t = tiles
        self.i = 0

    def advance(self):
        self.i += 1
        return self.t[self.i % len(self.t)]

    def __getitem__(self, k):
        return self.t[self.i % len(self.t)].h[k]


class KB:
    def __init__(self, nc):
        self.nc = nc
        self.eng = {'pe': nc.tensor, 'act': nc.scalar, 'dve': nc.vector, 'pool': nc.gpsimd, 'sp': nc.sync}
        self.sem = {k: nc.alloc_semaphore("s_" + k) for k in self.eng}
        self.cnt = {k: 0 for k in self.eng}
        self.waited = {k: {} for k in self.eng}
        self.dma_evs = {}
        self.nsem = 5
        self.free_sems = []
        self.free_sems_sw = []
        self.dma_tiles = []
        self.temp_recs = []
        self.nops = 0

    def _deps(self, r, w):
        deps = []
        for t in r:
            if t.w is not None:
                deps.append(t.w)
            deps.extend(t.wx)
        for t in w:
            if t.w is not None:
                deps.append(t.w)
            deps.extend(t.wx)
            deps.extend(t.r.values())
        return deps

    def _wait(self, e, deps):
        wd = self.waited[e]
        best = {}
        for (sem, sid, val, prod) in deps:
            if prod == e and e == 'pe':
                continue
            if wd.get(sid, 0) >= val:
                continue
            if sid not in best or best[sid][1] < val:
                best[sid] = (sem, val)
        for sid, (sem, val) in best.items():
            self.eng[e].wait_ge(sem, val)
            wd[sid] = val

    def op(self, e, fn, r=(), w=()):
        w = [x.advance() if isinstance(x, JunkRot) else x for x in w]
        self._wait(e, self._deps(r, w))
        ins = fn()
        self.cnt[e] += 1
        ins.then_inc(self.sem[e], 1)
        ev = (self.sem[e], e, self.cnt[e], e)
        for t in r:
            t.r[e] = ev
        for t in w:
            t.w = ev
            t.wx = []
            t.r = {}
        self.nops += 1
        return ev

    def dma(self, q, out_ap, in_ap, sb_t, load=True):
        def new_rec():
            pool_ = self.free_sems_sw if q == 'pool' else self.free_sems
            if pool_:
                return pool_.pop()
            r_ = [self.nc.alloc_semaphore("d%d" % self.nsem), "d%d" % self.nsem, 0, q]
            self.nsem += 1
            return r_

        fresh = (q == 'pool' and load and sb_t.dsem is not None and sb_t.w is not None
                 and sb_t.w[3] == 'dma' and not sb_t.r)
        if fresh:
            rec = new_rec()
            self.temp_recs.append(rec)
        else:
            deps = self._deps((), (sb_t,)) if load else self._deps((sb_t,), ())
            self._wait(q, deps)
            if sb_t.dsem is not None and sb_t.dsem[3] != q:
                self.temp_recs.append(sb_t.dsem)
                sb_t.dsem = new_rec()
            if sb_t.dsem is None:
                sb_t.dsem = new_rec()
                self.dma_tiles.append(sb_t)
            rec = sb_t.dsem
        ins = self.eng[q].dma_start(out=out_ap, in_=in_ap)
        rec[2] += 16
        ins.then_inc(rec[0], 16)
        ev = (rec[0], rec[1], rec[2], 'dma')
        if load:
            if fresh:
                sb_t.wx = sb_t.wx + [sb_t.w]
            else:
                sb_t.wx = []
            sb_t.w = ev
            sb_t.r = {}
        else:
            sb_t.r['dma%d' % rec[2]] = ev
        self.dma_evs[rec[1]] = ev
        return ev

    def barrier(self):
        self._wait('sp', list(self.dma_evs.values()))
        evs = [(self.sem[k], k, self.cnt[k], k) for k in self.eng if k != 'sp' and self.cnt[k] > 0]
        self._wait('sp', evs)
        self.eng['sp'].sem_inc(self.sem['sp'], 1)
        self.cnt['sp'] += 1
        ev = (self.sem['sp'], 'sp', self.cnt['sp'], 'sp')
        for k in self.eng:
            if k != 'sp':
                self._wait(k, [ev])
        for t in self.dma_tiles:
            self.temp_recs.append(t.dsem)
            t.dsem = None
        self.dma_tiles = []
        for rec in self.temp_recs:
            (self.free_sems_sw if rec[3] == 'pool' else self.free_sems).append(rec)
        self.temp_recs = []
        self.dma_evs = {}


def pipeline(gens, depth=2, lag=4):
    active = []
    idx = 0
    n = len(gens)
    while idx < n or active:
        if idx < n and len(active) < depth and (not active or active[-1][1] >= lag):
            active.append([gens[idx], 0])
            idx += 1
        for a in list(active):
            try:
                next(a[0])
                a[1] += 1
            except StopIteration:
                active.remove(a)


def pipeline2(items, depth, lag, ahead, newest_first=False):
    n = len(items)
    pre_res = {}

    def ensure(k):
        if k < n and k not in pre_res:
            pre_res[k] = items[k][0]()

    active = []
    idx = 0
    while idx < n or active:
        if idx < n and len(active) < depth and (not active or active[-1][1] >= lag):
            for k in range(idx, idx + ahead + 1):
                ensure(k)
            active.append([items[idx][1](pre_res.pop(idx)), 0])
            idx += 1
        for a in (list(reversed(active)) if newest_first else list(active)):
            try:
                next(a[0])
                a[1] += 1
            except StopIteration:
                active.remove(a)


def build_nc(TT, dbg=False):
    NB = TT // 128
    NOWN = NB // 4
    NTILE = NOWN // 4
    nc = bass.Bass("TRN2", target_bir_lowering=False)
    kb = KB(nc)
    V_, A_, P_, G_ = nc.vector, nc.scalar, nc.tensor, nc.gpsimd

    def din(name, shape, dt=F32):
        return nc.dram_tensor(name, list(shape), dt, kind="ExternalInput")

    x_full = din("x_full", [TT, D]).ap()
    x_own = din("x_own", [NOWN * 128, D]).ap()
    mem = din("mem", [256, D]).ap()
    cmask_d = din("cmask", [128, 512]).ap()
    wsel_d = din("wsel", [128, 4]).ap()
    w_in = din("w_in", [D, D_IN]).ap()
    w_a2 = din("w_gla_a2", [16, 512]).ap()
    w_mem_kv = din("w_mem_kv", [D, 2048]).ap()
    w_o = [din(n, [D, D]).ap() for n in ("w_o_gla", "w_o_dsa", "w_o_mem")]
    w_out = din("w_out", [D, D]).ap()
    w_r1 = din("w_r1", [D, 4]).ap()
    w_r2 = din("w_r2", [D, 16]).ap()
    w_gate = din("w_gate", [16, D, 256]).ap()
    w_up = din("w_up", [16, D, 256]).ap()
    w_down = din("w_down", [16, 256, D]).ap()
    vecs = {}
    for n, L in (("g_mix", 1024), ("g_mem", 1024), ("g_ffn", 1024), ("gla_norm", 256), ("dsa_q_norm", 128),
                 ("dsa_k_norm", 128), ("idx_k_norm", 64), ("mem_q_norm", 256), ("mem_k_norm", 256),
                 ("b_gla_a2", 512), ("b_gate", 3072), ("b_r1", 4), ("b_r2", 16)):
        vecs[n] = din(n, [1, L])
    c_ident = din("c_ident", [128, 128], BF16).ap()
    c_i4 = din("c_i4", [128, 512], BF16).ap()
    c_ones = din("c_ones", [128, 128], BF16).ap()
    c_lmt = din("c_lmt", [128, 128]).ap()
    c_umt = din("c_umt", [128, 128]).ap()
    c_caus4 = din("c_caus4", [128, 512]).ap()
    c_sel16 = din("c_sel16", [16, 2048], BF16).ap()
    c_bis = din("c_bis", [128, NBIS]).ap()
    out_own = nc.dram_tensor("out_own", [NOWN * 128, D], F32, kind="ExternalOutput").ap()
    dbgs = {}
    ogla_scr = nc.dram_tensor("ogla_scr", [NOWN, 128, 1024], BF16, kind="Internal").ap()

    def bcast(name, L):
        return bass.AP(tensor=vecs[name], offset=0, ap=[[0, 128], [1, L]])

    top = ExitStack()

    uid = [0]

    def sb(stack, name, shape, dt):
        uid[0] += 1
        return T(stack.enter_context(nc.sbuf_tensor("%s_%d" % (name, uid[0]), list(shape), dt)))

    def ps(stack, name, shape, dt=F32):
        uid[0] += 1
        return T(stack.enter_context(nc.psum_tensor("%s_%d" % (name, uid[0]), list(shape), dt)))

    def wchunks(src2d, col0, ncol):
        return src2d.rearrange("(c p) n -> p c n", p=128)[:, :, col0:col0 + ncol]

    with top:
        ident = sb(top, "ident", [128, 128], BF16)
        i4 = sb(top, "i4", [128, 512], BF16)
        ones = sb(top, "ones", [128, 128], BF16)
        wsel = sb(top, "wselt", [128, 4], F32)
        rstd_all = sb(top, "rstd_all", [128, NB], F32)
        memKT = sb(top, "memKT", [128, 8, 256], BF16)
        memV = sb(top, "memV", [128, 2, 1024], BF16)

        for t_, src_ in ((ident, c_ident), (i4, c_i4), (ones, c_ones), (wsel, wsel_d)):
            kb.dma('sp', t_[:], src_, t_)

        def load_gain(t_, name, L, rep=1, scale=None):
            for i in range(rep):
                kb.dma('sp', t_[:, i * L:(i + 1) * L], bcast(name, L), t_)
            if scale is not None:
                kb.op('dve', lambda: V_.tensor_scalar(out=t_[:], in0=t_[:], scalar1=scale, scalar2=None,
                                                      op0=ALU.mult), r=[t_], w=[t_])

        def rstd_from_ss(ss, n, width, tmp):
            kb.op('act', lambda: A_.activation(out=ss, in_=ss, func=AF.Ln, bias=EPS, scale=1.0 / width), r=[tmp], w=[tmp])
            kb.op('act', lambda: A_.activation(out=ss, in_=ss, func=AF.Exp, scale=-0.5), r=[tmp], w=[tmp])

        def norm_only(xt, gain, junk, st, hb, ss_ap=None, ss_t=None):
            if ss_ap is None:
                ss_ap, ss_t = st[:, 0:1], st
            kb.op('act', lambda: A_.activation(out=junk[:], in_=xt[:], func=AF.Square, accum_out=ss_ap),
                  r=[xt], w=[junk, ss_t])
            rstd_from_ss(ss_ap, 1, 1024.0, ss_t)
            kb.op('dve', lambda: V_.scalar_tensor_tensor(out=hb[:], in0=xt[:], scalar=ss_ap, in1=gain[:],
                                                         op0=ALU.mult, op1=ALU.mult), r=[xt, ss_t, gain], w=[hb])

        def transp_T(hb, PT, hT_ap, hT_t):
            for c in range(8):
                kb.op('pe', lambda c=c: P_.transpose(out=PT[:, c, :], in_=hb[:, c * 128:(c + 1) * 128],
                                                     identity=ident[:]), r=[hb, ident], w=[PT])
            kb.op('act', lambda: A_.copy(out=hT_ap, in_=PT[:]), r=[PT], w=[hT_t])

        def norm_T(xt, gain, junk, st, hb, PT, hT_ap, hT_t):
            norm_only(xt, gain, junk, st, hb)
            transp_T(hb, PT, hT_ap, hT_t)

        def proj(hT_ap_fn, hT_t, W, col0, ncol, PJ_t, PJ_ap=None):
            o = PJ_t[:, 0:ncol] if PJ_ap is None else PJ_ap
            for c in range(8):
                kb.op('pe', lambda c=c: P_.matmul(o, lhsT=hT_ap_fn(c), rhs=W[:, c, col0:col0 + ncol],
                                                  start=(c == 0), stop=(c == 7)), r=[hT_t, W], w=[PJ_t])

        with ExitStack() as ph:
            g_mem = sb(ph, "g_memt", [128, 1024], F32)
            mkg4 = sb(ph, "mkg4", [128, 1024], F32)
            xt = sb(ph, "p0_x", [128, 1024], F32)
            junk = JunkRot([sb(ph, "p0_junk%d" % i, [128, 1024], BF16) for i in range(3)])
            st = sb(ph, "p0_st", [128, 8], F32)
            hb = sb(ph, "p0_hb", [128, 1024], BF16)
            mhT = sb(ph, "p0_mhT", [128, 8, 256], BF16)
            wkv = [sb(ph, "p0_wkv%d" % i, [128, 8, 512], BF16) for i in range(2)]
            kfs = [sb(ph, "p0_kf%d" % i, [128, 1024], F32) for i in range(2)]
            kbf = sb(ph, "p0_kbf", [128, 1024], BF16)
            PT = ps(ph, "p0_PT", [128, 8, 128], BF16)
            PJ = [ps(ph, "p0_PJ%d" % i, [128, 512]) for i in range(2)]
            kb.dma('sp', g_mem[:], bcast("g_mem", 1024), g_mem)
            for i in range(4):
                kb.dma('sp', mkg4[:, i * 256:(i + 1) * 256], bcast("mem_k_norm", 256), mkg4)
            for mb in range(2):
                kb.dma('sp', xt[:], mem[mb * 128:(mb + 1) * 128, :], xt)
                norm_T(xt, g_mem, junk, st, hb, PT, mhT[:, :, mb * 128:(mb + 1) * 128], mhT)
            wrot = Rot(wkv)
            pjrot = Rot(PJ)
            for nt in range(4):
                wt = wrot.next()
                kb.dma('pool', wt[:], wchunks(w_mem_kv, nt * 512, 512), wt)
                for mb in range(2):
                    pj = pjrot.next()
                    kf = kfs[mb]
                    proj(lambda c, mb=mb: mhT[:, c, mb * 128:(mb + 1) * 128], mhT, wt, 0, 512, pj)
                    if nt < 2:
                        kb.op('act', lambda pj=pj, nt=nt, kf=kf: A_.copy(out=kf[:, nt * 512:(nt + 1) * 512], in_=pj[:]),
                              r=[pj], w=[kf])
                        if nt == 1:
                            for h in range(4):
                                kb.op('act', lambda h=h, kf=kf: A_.activation(out=junk[:, 0:256], in_=kf[:, h * 256:(h + 1) * 256],
                                                                       func=AF.Square, accum_out=st[:, h:h + 1]),
                                      r=[kf], w=[junk, st])
                            rstd_from_ss(st[:, 0:4], 4, 256.0, st)
                            for h in range(4):
                                kb.op('dve', lambda h=h, kf=kf: V_.scalar_tensor_tensor(
                                    out=kbf[:, h * 256:(h + 1) * 256], in0=kf[:, h * 256:(h + 1) * 256],
                                    scalar=st[:, h:h + 1], in1=mkg4[:, h * 256:(h + 1) * 256],
                                    op0=ALU.mult, op1=ALU.mult), r=[kf, st, mkg4], w=[kbf])
                            for c8 in range(8):
                                kb.op('pe', lambda c8=c8: P_.transpose(out=PT[:, c8, :], in_=kbf[:, c8 * 128:(c8 + 1) * 128],
                                                                       identity=ident[:]), r=[kbf, ident], w=[PT])
                            kb.op('act', lambda mb=mb: A_.copy(out=memKT[:, :, mb * 128:(mb + 1) * 128], in_=PT[:]),
                                  r=[PT], w=[memKT])
                    else:
                        kb.op('act', lambda pj=pj, nt=nt, mb=mb: A_.copy(
                            out=memV[:, mb, (nt - 2) * 512:(nt - 1) * 512], in_=pj[:]), r=[pj], w=[memV])
            kb.barrier()


        with ExitStack() as ph:
            Wg = sb(ph, "g_W", [128, 8, 3088], BF16)
            caus4 = sb(ph, "caus4", [128, 512], F32)
            g_mix = sb(ph, "g_mixt", [128, 1024], F32)
            gnorm = sb(ph, "gnormt", [128, 256], F32)
            kb.dma('sp', caus4[:], c_caus4, caus4)
            load_gain(g_mix, "g_mix", 1024)
            load_gain(gnorm, "gla_norm", 256)
            wa2 = sb(ph, "g_wa2", [17, 512], BF16)
            lmt = sb(ph, "g_lmt", [128, 128], F32)
            umt = sb(ph, "g_umt", [128, 128], F32)
            negc = sb(ph, "g_negc", [128, 2], F32)
            S = sb(ph, "g_S", [128, 1024], F32)
            Ssel = sb(ph, "g_Ssel", [128, 1024], F32)
            Sbf = sb(ph, "g_Sbf", [128, 1024], BF16)
            xts = Rot([sb(ph, "g_x%d" % i, [128, 1024], F32) for i in range(4)])
            junk = JunkRot([sb(ph, "g_junk%d" % i, [128, 256], BF16) for i in range(3)])
            sts = Rot([sb(ph, "g_st%d" % i, [128, 8], F32) for i in range(4)])
            hbs = Rot([sb(ph, "g_hb%d" % i, [128, 1024], BF16) for i in range(4)])
            hTs = Rot([sb(ph, "g_hT%d" % i, [128, 8, 128], BF16) for i in range(4)])
            kfs = Rot([sb(ph, "g_kf%d" % i, [128, 512], F32) for i in range(4)])
            qf = sb(ph, "g_qf", [128, 512], F32)
            vbs = Rot([sb(ph, "g_vb%d" % i, [128, 1024], BF16) for i in range(4)])
            sg = sb(ph, "g_sg", [128, 1024], F32)
            aTs = Rot([sb(ph, "g_aT%d" % i, [17, 128], BF16) for i in range(4)])
            spbs = Rot([sb(ph, "g_sp%d" % i, [128, 512], F32) for i in range(4)])
            E1s = Rot([sb(ph, "g_E1%d" % i, [128, 512], F32) for i in range(4)])
            E2 = sb(ph, "g_E2", [128, 512], F32)
            decs = Rot([sb(ph, "g_dec%d" % i, [128, 4], F32) for i in range(4)])
            kends = Rot([sb(ph, "g_kend%d" % i, [128, 512], BF16) for i in range(4)])
            qdec = sb(ph, "g_qdec", [128, 512], BF16)
            qkT = sb(ph, "g_qkT", [128, 8, 128], BF16)
            attT = sb(ph, "g_attT", [128, 512], BF16)
            ogl = sb(ph, "g_ogl", [128, 1024], BF16)
            ogTs = Rot([sb(ph, "g_ogT%d" % i, [128, 8, 128], BF16) for i in range(2)])
            PJs = Rot([ps(ph, "g_PJ%d" % i, [128, 512]) for i in range(2)])
            PA = ps(ph, "g_PA", [128, 512])
            PB = ps(ph, "g_PB", [128, 512])
            PU = [ps(ph, "g_PU%d" % i, [128, 512]) for i in range(2)]
            PT = ps(ph, "g_PT", [128, 8, 128], BF16)
            PS = ps(ph, "g_PS", [128, 512])

            for i in range(7):
                c0 = i * 512
                n = min(512, 3088 - c0)
                kb.dma('pool', Wg[:, :, c0:c0 + n], wchunks(w_in, c0, n), Wg)
            kb.dma('pool', wa2[0:16, :], w_a2, wa2)
            kb.dma('pool', wa2[16:17, :], vecs["b_gla_a2"].ap(), wa2)
            kb.dma('sp', lmt[:], c_lmt, lmt)
            kb.dma('sp', umt[:], c_umt, umt)
            kb.op('dve', lambda: V_.memset(negc[:], -1.0 / 16.0), w=[negc])
            kb.op('dve', lambda: V_.memset(S[:], 0.0), w=[S])
            for aT_ in aTs.t:
                kb.op('pool', lambda aT_=aT_: G_.memset(aT_[:], 1.0), w=[aT_])

            def g_pre(xsrc, blk=None):
                def f():
                    xt = xts.next(); st = sts.next(); hb = hbs.next()
                    kb.dma('sp', xt[:], xsrc, xt)
                    if blk is None:
                        norm_only(xt, g_mix, hb, st, hb)
                    else:
                        norm_only(xt, g_mix, hb, st, hb, ss_ap=rstd_all[:, blk:blk + 1], ss_t=rstd_all)
                    return (st, hb)
                return f

            def gla_block(pre, own, p, slot):
                st, hb = pre
                hT = hTs.next()
                kf = kfs.next(); vb = vbs.next(); aT = aTs.next(); spb = spbs.next(); E1 = E1s.next(); E3 = E1
                dec = decs.next(); kend = kends.next(); kinv = kend
                transp_T(hb, PT, hT[:], hT)
                yield
                hf = lambda c: hT[:, c, :]
                for c in range(8):
                    kb.op('pe', lambda c=c: P_.matmul(PS[0:16, 0:128], lhsT=Wg[:, c, OFF['ga']:OFF['ga'] + 16],
                                                      rhs=hT[:, c, :], start=(c == 0), stop=(c == 7)),
                          r=[hT, Wg], w=[PS])
                kb.op('act', lambda: A_.copy(out=aT[0:16, :], in_=PS[0:16, 0:128]), r=[PS], w=[aT])
                pj = PJs.next()
                proj(hf, hT, Wg, OFF['gk'], 512, pj)
                kb.op('dve', lambda: V_.tensor_copy(out=kf[:], in_=pj[:]), r=[pj], w=[kf])
                yield
                kb.op('pe', lambda: P_.matmul(PA[:], lhsT=aT[:], rhs=wa2[:], start=True, stop=True),
                      r=[aT, wa2], w=[PA])
                pj = PJs.next()
                proj(hf, hT, Wg, OFF['gv'], 512, pj)
                kb.op('dve', lambda pj=pj: V_.tensor_copy(out=vb[:, 0:512], in_=pj[:]), r=[pj], w=[vb])
                yield
                kb.op('act', lambda: A_.activation(out=spb[:], in_=PA[:], func=AF.Exp, scale=-1.0), r=[PA], w=[spb])
                kb.op('act', lambda: A_.activation(out=spb[:], in_=spb[:], func=AF.Ln, bias=1.0), r=[spb], w=[spb])
                pj = PJs.next()
                proj(hf, hT, Wg, OFF['gv'] + 512, 512, pj)
                kb.op('dve', lambda pj=pj: V_.tensor_copy(out=vb[:, 512:1024], in_=pj[:]), r=[pj], w=[vb])
                yield
                if own:
                    pj = PJs.next()
                    proj(hf, hT, Wg, OFF['gq'], 512, pj)
                    kb.op('act', lambda: A_.copy(out=qf[:], in_=pj[:]), r=[pj], w=[qf])

                    def g_proj(nt):
                        pj = PJs.next()
                        proj(hf, hT, Wg, OFF['gg'] + nt * 512, 512, pj)
                        kb.op('act', lambda: A_.activation(out=sg[:, nt * 512:(nt + 1) * 512], in_=pj[:], func=AF.Silu),
                              r=[pj], w=[sg])
                yield
                if not own:
                    kb.op('pe', lambda: P_.matmul(PB[:], lhsT=umt[:], rhs=spb[:], start=True, stop=True),
                          r=[umt, spb], w=[PB])
                    for h in range(4):
                        kb.op('pe', lambda h=h: P_.matmul(PS[:, 128 + 2 * h:130 + 2 * h], lhsT=spb[:, h * 128:(h + 1) * 128],
                                                          rhs=negc[:], start=True, stop=True), r=[spb, negc], w=[PS])
                    yield
                    kb.op('act', lambda: A_.activation(out=E3[:], in_=PB[:], func=AF.Exp), r=[PB], w=[E3])
                    kb.op('act', lambda: A_.activation(out=dec[:], in_=PS[:, 128:136:2], func=AF.Exp), r=[PS], w=[dec])
                    kb.op('dve', lambda: V_.tensor_tensor(out=kend[:], in0=kf[:], in1=E3[:], op=ALU.mult),
                          r=[kf, E3], w=[kend])
                    for h in range(4):
                        kb.op('pe', lambda h=h: P_.matmul(PU[h // 2][:, (h % 2) * 256:(h % 2 + 1) * 256],
                                                          lhsT=kend[:, h * 128:(h + 1) * 128],
                                                          rhs=vb[:, h * 256:(h + 1) * 256], start=True, stop=True),
                              r=[kend, vb], w=[PU[h // 2]])
                    yield
                    if p == 0:
                        kb.op('dve', lambda: V_.tensor_scalar(out=Ssel[:], in0=S[:], scalar1=wsel[:, 0:1], scalar2=None,
                                                              op0=ALU.mult), r=[S, wsel], w=[Ssel])
                    else:
                        kb.op('dve', lambda: V_.scalar_tensor_tensor(out=Ssel[:], in0=S[:], scalar=wsel[:, p:p + 1],
                                                                     in1=Ssel[:], op0=ALU.mult, op1=ALU.add),
                              r=[S, wsel, Ssel], w=[Ssel])
                    for h in range(4):
                        kb.op('dve', lambda h=h: V_.scalar_tensor_tensor(
                            out=S[:, h * 256:(h + 1) * 256], in0=S[:, h * 256:(h + 1) * 256], scalar=dec[:, h:h + 1],
                            in1=PU[h // 2][:, (h % 2) * 256:(h % 2 + 1) * 256], op0=ALU.mult, op1=ALU.add),
                            r=[S, dec, PU[h // 2]], w=[S])
                else:
                    kb.op('pe', lambda: P_.matmul(PB[:], lhsT=lmt[:], rhs=spb[:], start=True, stop=True),
                          r=[lmt, spb], w=[PB])
                    yield
                    kb.op('act', lambda: A_.activation(out=E1[:], in_=PB[:], func=AF.Exp), r=[PB], w=[E1])
                    kb.op('act', lambda: A_.activation(out=E2[:], in_=PB[:], func=AF.Exp, scale=-1.0), r=[PB], w=[E2])
                    g_proj(0)
                    kb.op('dve', lambda: V_.scalar_tensor_tensor(out=qdec[:], in0=qf[:], scalar=128.0 ** -0.5, in1=E1[:],
                                                                 op0=ALU.mult, op1=ALU.mult), r=[qf, E1], w=[qdec])
                    kb.op('dve', lambda: V_.tensor_tensor(out=kinv[:], in0=kf[:], in1=E2[:], op=ALU.mult),
                          r=[kf, E2], w=[kinv])
                    g_proj(1)
                    for h in range(4):
                        kb.op('pe', lambda h=h: P_.transpose(out=PT[:, h, :], in_=qdec[:, h * 128:(h + 1) * 128],
                                                             identity=ident[:]), r=[qdec, ident], w=[PT])
                        kb.op('pe', lambda h=h: P_.transpose(out=PT[:, 4 + h, :], in_=kinv[:, h * 128:(h + 1) * 128],
                                                             identity=ident[:]), r=[kinv, ident], w=[PT])
                    yield
                    kb.op('act', lambda: A_.copy(out=qkT[:], in_=PT[:]), r=[PT], w=[qkT])
                    for h in range(4):
                        kb.op('pe', lambda h=h: P_.matmul(PB[:, h * 128:(h + 1) * 128], lhsT=qkT[:, 4 + h, :],
                                                          rhs=qkT[:, h, :], start=True, stop=True), r=[qkT], w=[PB])
                    kb.op('dve', lambda: V_.tensor_tensor(out=attT[:], in0=PB[:], in1=caus4[:], op=ALU.mult),
                          r=[PB, caus4], w=[attT])
                    yield
                    kb.op('pool', lambda: G_.tensor_copy(out=Sbf[:], in_=Ssel[:]), r=[Ssel], w=[Sbf])
                    for h in range(4):
                        o_ap = PU[h // 2][:, (h % 2) * 256:(h % 2 + 1) * 256]
                        kb.op('pe', lambda h=h, o_ap=o_ap: P_.matmul(o_ap, lhsT=attT[:, h * 128:(h + 1) * 128],
                                                                     rhs=vb[:, h * 256:(h + 1) * 256], start=True, stop=False),
                              r=[attT, vb], w=[PU[h // 2]])
                        kb.op('pe', lambda h=h, o_ap=o_ap: P_.matmul(o_ap, lhsT=qkT[:, h, :],
                                                                     rhs=Sbf[:, h * 256:(h + 1) * 256], start=False, stop=True),
                              r=[qkT, Sbf], w=[PU[h // 2]])
                    for h in range(4):
                        kb.op('act', lambda h=h: A_.activation(out=junk[:, 0:256], in_=PU[h // 2][:, (h % 2) * 256:(h % 2 + 1) * 256],
                                                               func=AF.Square, accum_out=st[:, 4 + h:5 + h]),
                              r=[PU[h // 2]], w=[junk, st])
                    yield
                    rstd_from_ss(st[:, 4:8], 4, 256.0, st)
                    for h in range(4):
                        kb.op('pool', lambda h=h: G_.tensor_tensor(out=sg[:, h * 256:(h + 1) * 256], in0=sg[:, h * 256:(h + 1) * 256],
                                                                   in1=gnorm[:], op=ALU.mult), r=[sg, gnorm], w=[sg])
                    for h in range(4):
                        kb.op('dve', lambda h=h: V_.scalar_tensor_tensor(
                            out=ogl[:, h * 256:(h + 1) * 256], in0=PU[h // 2][:, (h % 2) * 256:(h % 2 + 1) * 256],
                            scalar=st[:, 4 + h:5 + h], in1=sg[:, h * 256:(h + 1) * 256], op0=ALU.mult, op1=ALU.mult),
                            r=[PU[h // 2], st, sg], w=[ogl])
                    for c in range(8):
                        kb.op('pe', lambda c=c: P_.transpose(out=PT[:, c, :], in_=ogl[:, c * 128:(c + 1) * 128],
                                                             identity=ident[:]), r=[ogl, ident], w=[PT])
                    ogT = ogTs.next()
                    kb.op('act', lambda: A_.copy(out=ogT[:], in_=PT[:]), r=[PT], w=[ogT])
                    kb.dma('sp', ogla_scr[slot], ogT[:].rearrange("p a b -> p (a b)"), ogT, load=False)

            items = []
            for m in range(NOWN):
                for p in range(4):
                    blk = 4 * m + p
                    items.append((g_pre(x_full[blk * 128:(blk + 1) * 128, :], blk),
                                  lambda pre, p=p, m=m: gla_block(pre, False, p, m)))
                items.append((g_pre(x_own[m * 128:(m + 1) * 128, :]), lambda pre, m=m: gla_block(pre, True, 0, m)))
            pipeline2(items, depth=2, lag=4, ahead=2)
            kb.barrier()

        KT = sb(top, "KT", [128, 2, TT], BF16)
        VV = sb(top, "VV", [128, NB, 256], BF16)
        kidxT = sb(top, "kidxT", [128, TT // 2], BF16)
        with ExitStack() as ph:
            Wk = sb(ph, "k_W", [128, 8, 576], BF16)
            g_mix = sb(ph, "g_mixt", [128, 1024], F32)
            kg2 = sb(ph, "kg2", [128, 256], F32)
            ikg = sb(ph, "ikg", [128, 128], F32)
            load_gain(g_mix, "g_mix", 1024)
            load_gain(kg2, "dsa_k_norm", 128, rep=2)
            load_gain(ikg, "idx_k_norm", 64, rep=2)
            xts = Rot([sb(ph, "k_x%d" % i, [128, 1024], F32) for i in range(4)])
            junk = JunkRot([sb(ph, "k_junk%d" % i, [128, 1024], BF16) for i in range(3)])
            sts = Rot([sb(ph, "k_st%d" % i, [128, 8], F32) for i in range(4)])
            hbs = Rot([sb(ph, "k_hb%d" % i, [128, 1024], BF16) for i in range(4)])
            hTs = Rot([sb(ph, "k_hT%d" % i, [128, 8, 128], BF16) for i in range(3)])
            knbs = Rot([sb(ph, "k_knb%d" % i, [128, 256], BF16) for i in range(3)])
            ikbs = Rot([sb(ph, "k_ikb%d" % i, [128, 128], BF16) for i in range(3)])
            PJs = Rot([ps(ph, "k_PJ%d" % i, [128, 512]) for i in range(2)])
            PIs = Rot([ps(ph, "k_PI%d" % i, [128, 512]) for i in range(2)])
            PT = ps(ph, "k_PT", [128, 8, 128], BF16)
            PT2 = ps(ph, "k_PT2", [128, 4, 128], BF16)
            kb.dma('pool', Wk[:, :, 0:512], wchunks(w_in, OFF['dk'], 512), Wk)
            kb.dma('pool', Wk[:, :, 512:576], wchunks(w_in, OFF['ik'], 64), Wk)
            def k_pre(blk):
                def f():
                    xt = xts.next(); st = sts.next(); hb = hbs.next()
                    kb.dma('sp', xt[:], x_full[blk * 128:(blk + 1) * 128, :], xt)
                    kb.op('dve', lambda: V_.scalar_tensor_tensor(out=hb[:], in0=xt[:], scalar=rstd_all[:, blk:blk + 1], in1=g_mix[:],
                                                                 op0=ALU.mult, op1=ALU.mult), r=[xt, rstd_all, g_mix], w=[hb])
                    return (st, hb)
                return f

            def k_block(blk, pre):
                st, hb = pre
                hT = hTs.next(); knb = knbs.next(); ikb = ikbs.next(); PI = PIs.next()
                transp_T(hb, PT, hT[:], hT)
                yield
                hf = lambda c, hT=hT: hT[:, c, :]
                pj = PJs.next()
                proj(hf, hT, Wk, 0, 512, pj)
                proj(hf, hT, Wk, 512, 64, PI)
                yield
                kb.op('act', lambda pj=pj, blk=blk: A_.copy(out=VV[:, blk, :], in_=pj[:, 256:512]), r=[pj], w=[])
                for g in range(2):
                    kb.op('act', lambda g=g, pj=pj, st=st: A_.activation(out=junk[:, 0:128], in_=pj[:, g * 128:(g + 1) * 128],
                                                                         func=AF.Square, accum_out=st[:, 4 + g:5 + g]),
                          r=[pj], w=[junk, st])
                kb.op('act', lambda st=st: A_.activation(out=junk[:, 0:64], in_=PI[:, 0:64], func=AF.Square,
                                                         accum_out=st[:, 6:7]), r=[PI], w=[junk, st])
                yield
                rstd_from_ss(st[:, 4:6], 2, 128.0, st)
                rstd_from_ss(st[:, 6:7], 1, 64.0, st)
                for g in range(2):
                    kb.op('dve', lambda g=g, pj=pj, st=st: V_.scalar_tensor_tensor(
                        out=knb[:, g * 128:(g + 1) * 128], in0=pj[:, g * 128:(g + 1) * 128], scalar=st[:, 4 + g:5 + g],
                        in1=kg2[:, g * 128:(g + 1) * 128], op0=ALU.mult, op1=ALU.mult), r=[pj, st, kg2], w=[knb])
                for hh in range(2):
                    kb.op('dve', lambda st=st, hh=hh: V_.scalar_tensor_tensor(
                        out=ikb[:, hh * 64:(hh + 1) * 64], in0=PI[:, 0:64], scalar=st[:, 6:7], in1=ikg[:, 0:64],
                        op0=ALU.mult, op1=ALU.mult), r=[PI, st, ikg], w=[ikb])
                yield
                for g in range(2):
                    kb.op('pe', lambda g=g: P_.transpose(out=PT2[:, g, :], in_=knb[:, g * 128:(g + 1) * 128],
                                                         identity=ident[:]), r=[knb, ident], w=[PT2])
                kb.op('pe', lambda: P_.transpose(out=PT2[:, 2, :], in_=ikb[:], identity=ident[:]),
                      r=[ikb, ident], w=[PT2])
                kb.op('act', lambda blk=blk: A_.copy(out=KT[:, :, blk * 128:(blk + 1) * 128], in_=PT2[:, 0:2, :]),
                      r=[PT2], w=[])
                hp = 0 if blk < NB // 2 else 64
                bl = blk if blk < NB // 2 else blk - NB // 2
                kb.op('act', lambda bl=bl, hp=hp: A_.copy(out=kidxT[hp:hp + 64, bl * 128:(bl + 1) * 128],
                                                          in_=PT2[hp:hp + 64, 2, :]), r=[PT2], w=[])
            pipeline2([(k_pre(blk), lambda pre, blk=blk: k_block(blk, pre)) for blk in range(NB)], depth=2, lag=2, ahead=2, newest_first=True)
            if dbg:
                dbgs['KT'] = nc.dram_tensor("dbg_KT", [128, 2 * TT], BF16, kind="ExternalOutput")
                kb.dma('sp', dbgs['KT'].ap(), KT[:].rearrange("p a b -> p (a b)"), KT, load=False)
                dbgs['kidxT'] = nc.dram_tensor("dbg_kidxT", [128, TT // 2], BF16, kind="ExternalOutput")
                kb.dma('sp', dbgs['kidxT'].ap(), kidxT[:], kidxT, load=False)
            kb.barrier()

        U8 = mybir.dt.uint8
        for j in range(NTILE):
            with ExitStack() as tl:
                hTo = sb(tl, "c_hTo", [128, 8, 512], BF16)
                OdT = sb(tl, "c_OdT", [128, 8, 512], BF16)
                with ExitStack() as ph:
                    g_mix = sb(ph, "g_mixt", [128, 1024], F32)
                    xt = sb(ph, "c0_x", [128, 1024], F32)
                    junk = JunkRot([sb(ph, "c0_junk%d" % i, [128, 1024], BF16) for i in range(3)])
                    st = sb(ph, "c0_st", [128, 8], F32)
                    hbs4 = [sb(ph, "c0_hb%d" % i, [128, 1024], BF16) for i in range(4)]
                    xts2 = Rot([xt, sb(ph, "c0_x2", [128, 1024], F32)])
                    sts4 = [sb(ph, "c0_st%d" % i, [128, 8], F32) for i in range(4)]
                    PTs = Rot([ps(ph, "c0_PT%d" % i, [128, 8, 128], BF16) for i in range(2)])
                    load_gain(g_mix, "g_mix", 1024)
                    for s in range(4):
                        m = 4 * j + s
                        xt_ = xts2.next()
                        kb.dma('sp', xt_[:], x_own[m * 128:(m + 1) * 128, :], xt_)
                        norm_only(xt_, g_mix, junk, sts4[s], hbs4[s])
                    for s in range(4):
                        transp_T(hbs4[s], PTs.next(), hTo[:, :, s * 128:(s + 1) * 128], hTo)
                    kb.barrier()
                with ExitStack() as c1:
                    QT = sb(c1, "c1_QT", [128, 8, 512], BF16)
                    IQT = sb(c1, "c1_IQT", [128, 8, 512], BF16)
                    wt = sb(c1, "c1_wt", [128, 4, 8], F32)
                    wabs = sb(c1, "c1_wabs", [128, 4, 8], F32)
                    wsgn = sb(c1, "c1_wsgn", [128, 4, 8], F32)
                    with ExitStack() as ph:
                        qg8 = sb(ph, "qg8", [128, 1024], F32)
                        wbufs = Rot([sb(ph, "c1a_w%d" % i, [128, 8, 512], BF16) for i in range(2)])
                        wiw = sb(ph, "c1a_wiw", [128, 8, 8], BF16)
                        qn = sb(ph, "c1a_qn", [128, 512], BF16)
                        iqd = sb(ph, "c1a_iqd", [128, 8, 2, 64], BF16)
                        junk = JunkRot([sb(ph, "c1a_junk%d" % i, [128, 128], BF16) for i in range(3)])
                        st = sb(ph, "c1a_st", [128, 8], F32)
                        PJs = Rot([ps(ph, "c1a_PJ%d" % i, [128, 512]) for i in range(2)])
                        PJw = ps(ph, "c1a_PJw", [128, 512])
                        PT = ps(ph, "c1a_PT", [128, 8, 128], BF16)
                        load_gain(qg8, "dsa_q_norm", 128, rep=8, scale=128.0 ** -0.5)
                        kb.dma('pool', wiw[:], wchunks(w_in, OFF['iw'], 8), wiw)
                        pend = None
                        for wi, col0 in enumerate((OFF['dq'], OFF['dq'] + 512, OFF['iq'])):
                            wb = wbufs.next()
                            kb.dma('pool', wb[:], wchunks(w_in, col0, 512), wb)
                            for s in range(4):
                                hf = lambda c, s=s: hTo[:, c, s * 128:(s + 1) * 128]
                                pj = PJs.next()
                                proj(hf, hTo, wb, 0, 512, pj)
                                if pend is not None:
                                    pend()
                                def post(wi=wi, s=s, pj=pj, hf=hf):
                                    if wi < 2:
                                        for hh in range(4):
                                            kb.op('act', lambda hh=hh, pj=pj: A_.activation(
                                                out=junk[:], in_=pj[:, hh * 128:(hh + 1) * 128], func=AF.Square,
                                                accum_out=st[:, hh:hh + 1]), r=[pj], w=[junk, st])
                                        rstd_from_ss(st[:, 0:4], 4, 128.0, st)
                                        for hh in range(4):
                                            kb.op('dve', lambda hh=hh, pj=pj, wi=wi: V_.scalar_tensor_tensor(
                                                out=qn[:, hh * 128:(hh + 1) * 128], in0=pj[:, hh * 128:(hh + 1) * 128],
                                                scalar=st[:, hh:hh + 1], in1=qg8[:, (wi * 4 + hh) * 128:(wi * 4 + hh + 1) * 128],
                                                op0=ALU.mult, op1=ALU.mult), r=[pj, st, qg8], w=[qn])
                                        for hh in range(4):
                                            kb.op('pe', lambda hh=hh: P_.transpose(out=PT[:, hh, :], in_=qn[:, hh * 128:(hh + 1) * 128],
                                                                                   identity=ident[:]), r=[qn, ident], w=[PT])
                                        kb.op('act', lambda wi=wi, s=s: A_.copy(out=QT[:, wi * 4:wi * 4 + 4, s * 128:(s + 1) * 128],
                                                                                in_=PT[:, 0:4, :]), r=[PT], w=[QT])
                                    else:
                                        pv = pj[:, :].rearrange("p (h d) -> p h d", h=8)
                                        for dd in range(2):
                                            kb.op('act', lambda dd=dd, pv=pv: A_.copy(out=iqd[:, :, dd, :], in_=pv), r=[pj], w=[iqd])
                                        for h in range(8):
                                            kb.op('pe', lambda h=h: P_.transpose(
                                                out=PT[:, h, :], in_=iqd[:, h, :, :].rearrange("p a b -> p (a b)"),
                                                identity=ident[:]), r=[iqd, ident], w=[PT])
                                        kb.op('act', lambda s=s: A_.copy(out=IQT[:, :, s * 128:(s + 1) * 128], in_=PT[:]),
                                              r=[PT], w=[IQT])
                                        pj2 = PJw
                                        proj(hf, hTo, wiw, 0, 8, pj2)
                                        kb.op('dve', lambda s=s, pj2=pj2: V_.tensor_scalar(
                                            out=wt[:, s, :], in0=pj2[:, 0:8], scalar1=(8.0 ** -0.5) * (64.0 ** -0.5), scalar2=None,
                                            op0=ALU.mult), r=[pj2], w=[wt])
                                        kb.op('dve', lambda s=s: V_.tensor_scalar(out=wsgn[:, s, :], in0=wt[:, s, :], scalar1=0.0, scalar2=2.0,
                                                                                   op0=ALU.is_ge, op1=ALU.mult), r=[wt], w=[wsgn])
                                        kb.op('dve', lambda s=s: V_.tensor_scalar(out=wsgn[:, s, :], in0=wsgn[:, s, :], scalar1=-1.0, scalar2=None,
                                                                                   op0=ALU.add), r=[wsgn], w=[wsgn])
                                        kb.op('dve', lambda s=s: V_.tensor_tensor(out=wabs[:, s, :], in0=wt[:, s, :], in1=wsgn[:, s, :], op=ALU.mult),
                                              r=[wt, wsgn], w=[wabs])
                                pend = post
                        pend()
                        kb.barrier()
                    with ExitStack() as ph:
                        NKT = 4 * (j + 1)
                        scs = [sb(ph, "c1b_sc%d" % i, [128, NKT * 512], F32) for i in range(2)]
                        jk = sb(ph, "c1b_jk", [128, 2048], U8)
                        cmask = sb(ph, "cmaskt", [128, 512], F32)
                        cbis = sb(ph, "cbis", [128, NBIS], F32)
                        Rs = Rot([sb(ph, "c1b_R%d" % i, [128, 512], BF16) for i in range(2)])
                        Ps = Rot([sb(ph, "c1b_P%d" % i, [128, 512], BF16) for i in range(3)])
                        nmt = [sb(ph, "c1b_nm%d" % i, [128, 512], BF16) for i in range(NKT)]
                        Dm = sb(ph, "c1b_Dm", [128, 8, 128], BF16)
                        rec = sb(ph, "c1b_rec", [128, 512], F32)
                        bss = [sb(ph, "c1b_bs%d" % i, [128, 8], F32) for i in range(2)]
                        dl = sb(ph, "c1b_dl", [128, NBIS], F32)
                        PSIs = Rot([ps(ph, "c1b_PI%d" % i, [128, 512]) for i in range(2)])
                        PSC = ps(ph, "c1b_PC", [128, 512])
                        PSSs = Rot([ps(ph, "c1b_PS%d" % i, [128, 512]) for i in range(3)])
                        PSO = ps(ph, "c1b_PO", [128, 512])
                        PSM = ps(ph, "c1b_PM", [128, 512])
                        kb.dma('sp', cmask[:], cmask_d, cmask)
                        kb.dma('sp', cbis[:], c_bis, cbis)

                        def c1_index(s):
                            m = 4 * j + s
                            scores = scs[s % 2]
                            tok = slice(s * 128, (s + 1) * 128)
                            pend = None
                            for kt in range(m + 1):
                                hp = 0 if kt * 512 < TT // 2 else 64
                                kc = kt * 512 - (0 if hp == 0 else TT // 2)
                                for h in range(8):
                                    psi = PSIs.next()
                                    kb.op('pe', lambda: P_.matmul(
                                        psi[:], lhsT=IQT[hp:hp + 64, h, tok], rhs=kidxT[hp:hp + 64, kc:kc + 512],
                                        start=True, stop=True), r=[IQT, kidxT], w=[psi])
                                    R = Rs.next()
                                    kb.op('act', lambda: A_.activation(out=R[:], in_=psi[:], func=AF.Relu, scale=wabs[:, s, h:h + 1]),
                                          r=[psi, wabs], w=[R])
                                    if pend is not None:
                                        pend()
                                    def acc(h=h, R=R, kt=kt):
                                        kb.op('pe', lambda: P_.matmul(PSC[:], lhsT=Dm[:, h, :], rhs=R[:], start=(h == 0), stop=(h == 7)),
                                              r=[Dm, R], w=[PSC])
                                        if h == 7:
                                            kb.op('act', lambda: A_.copy(out=scores[:, kt * 512:(kt + 1) * 512], in_=PSC[:]),
                                                  r=[PSC], w=[scores])
                                    pend = acc
                                    if h % 4 == 3:
                                        yield 2.7
                            pend()
                            yield 0.5

                        def c1_dm(s):
                            for h in range(8):
                                kb.op('dve', lambda: V_.tensor_scalar(out=Dm[:, h, :], in0=ident[:], scalar1=wsgn[:, s, h:h + 1],
                                                                      scalar2=None, op0=ALU.mult), r=[ident, wsgn], w=[Dm])

                        def c1_bisect(s):
                            m = 4 * j + s
                            nk = 512 * (m + 1)
                            scores = scs[s % 2]
                            bs = bss[s % 2]
                            kb.op('dve', lambda: V_.tensor_reduce(out=bs[:, 0:1], in_=scores[:, 0:nk], axis=AX.X, op=ALU.max,
                                                                  apply_absolute_value=True), r=[scores], w=[bs])
                            kb.op('dve', lambda: V_.tensor_tensor(out=scores[:, m * 512:(m + 1) * 512],
                                                                  in0=scores[:, m * 512:(m + 1) * 512], in1=cmask[:], op=ALU.add),
                                  r=[scores, cmask], w=[scores])
                            kb.op('dve', lambda: V_.tensor_scalar(out=dl[:], in0=cbis[:], scalar1=bs[:, 0:1], scalar2=None,
                                                                  op0=ALU.mult), r=[cbis, bs], w=[dl])
                            kb.op('dve', lambda: V_.tensor_scalar(out=bs[:, 1:2], in0=bs[:, 0:1], scalar1=-1.0, scalar2=None,
                                                                  op0=ALU.mult), r=[bs], w=[bs])
                            yield nk / 850.0 + 1.5
                            for k in range(NBIS):
                                kb.op('dve', lambda: V_.tensor_tensor(out=bs[:, 2:3], in0=bs[:, 1:2], in1=dl[:, k:k + 1],
                                                                      op=ALU.add), r=[bs, dl], w=[bs])
                                for ci, c0 in enumerate(range(0, nk, 2048)):
                                    cw = min(2048, nk - c0)
                                    kb.op('dve', lambda: V_.tensor_scalar(
                                        out=jk[:, 0:cw], in0=scores[:, c0:c0 + cw], scalar1=bs[:, 2:3],
                                        scalar2=(None if ci == 0 else bs[:, 3:4]), op0=ALU.is_ge, op1=ALU.add,
                                        accum_out=bs[:, 3:4]), r=[scores, bs], w=[jk, bs])
                                kb.op('dve', lambda: V_.scalar_tensor_tensor(out=bs[:, 4:5], in0=bs[:, 3:4], scalar=255.5,
                                                                             in1=dl[:, k:k + 1], op0=ALU.is_ge, op1=ALU.mult),
                                      r=[bs, dl], w=[bs])
                                kb.op('dve', lambda: V_.tensor_tensor(out=bs[:, 1:2], in0=bs[:, 1:2], in1=bs[:, 4:5], op=ALU.add),
                                      r=[bs], w=[bs])
                                yield nk / 850.0 + 1.3

                        def c1_nm(s):
                            m = 4 * j + s
                            scores = scs[s % 2]
                            bs = bss[s % 2]
                            for kt in range(m + 1):
                                kb.op('dve', lambda: V_.tensor_scalar(
                                    out=nmt[kt][:], in0=scores[:, kt * 512:(kt + 1) * 512], scalar1=bs[:, 1:2], scalar2=NEG,
                                    op0=ALU.is_lt, op1=ALU.mult), r=[scores, bs], w=[nmt[kt]])

                        def c1_attn(s):
                            m = 4 * j + s
                            tok = slice(s * 128, (s + 1) * 128)
                            nkb = 4 * (m + 1)
                            for g in range(2):
                                pend = None
                                for kb_ in range(nkb):
                                    nm = nmt[kb_ // 4]
                                    pss = PSSs.next()
                                    kb.op('pe', lambda: P_.matmul(
                                        pss[:], lhsT=nm[:, (kb_ % 4) * 128:(kb_ % 4 + 1) * 128], rhs=i4[:], start=True, stop=False),
                                        r=[nm, i4], w=[pss])
                                    kb.op('pe', lambda: P_.matmul(
                                        pss[:], lhsT=KT[:, g, kb_ * 128:(kb_ + 1) * 128], rhs=QT[:, 4 * g:4 * g + 4, tok],
                                        start=False, stop=True), r=[KT, QT], w=[pss])
                                    Pb = Ps.next()
                                    kb.op('act', lambda: A_.activation(out=Pb[:], in_=pss[:], func=AF.Exp), r=[pss], w=[Pb])
                                    if pend is not None:
                                        pend()
                                    def pv(kb_=kb_, Pb=Pb, g=g):
                                        kb.op('pe', lambda: P_.matmul(
                                            PSO[:], lhsT=VV[:, kb_, g * 128:(g + 1) * 128], rhs=Pb[:],
                                            start=(kb_ == 0), stop=(kb_ == nkb - 1)), r=[VV, Pb], w=[PSO])
                                        kb.op('pe', lambda: P_.matmul(
                                            PSM[:], lhsT=ones[:], rhs=Pb[:], start=(kb_ == 0), stop=(kb_ == nkb - 1)),
                                            r=[ones, Pb], w=[PSM])
                                    pend = pv
                                    yield 1.1
                                pend()
                                kb.op('dve', lambda: V_.reciprocal(out=rec[:], in_=PSM[:]), r=[PSM], w=[rec])
                                kb.op('dve', lambda: V_.tensor_tensor(
                                    out=OdT[:, 4 * g:4 * g + 4, tok], in0=PSO[:, :].rearrange("p (h q) -> p h q", h=4),
                                    in1=rec[:, :].rearrange("p (h q) -> p h q", h=4), op=ALU.mult), r=[PSO, rec], w=[OdT])
                                yield 1.0

                        def weave3(gens):
                            acc_t = [0.0 for _ in gens]
                            live = [g is not None for g in gens]
                            while any(live):
                                i = min((k for k in range(len(gens)) if live[k]), key=lambda k: acc_t[k])
                                try:
                                    acc_t[i] += next(gens[i])
                                except StopIteration:
                                    live[i] = False

                        c1_dm(0)
                        for t in range(-2, 4):
                            gl = []
                            if 0 <= t + 2 < 4:
                                gl.append(c1_index(t + 2))
                            if 0 <= t + 1 < 4:
                                gl.append(c1_bisect(t + 1))
                            if 0 <= t < 4:
                                gl.append(c1_attn(t))
                            weave3(gl)
                            if 0 <= t + 3 < 4:
                                c1_dm(t + 3)
                            if 0 <= t + 1 < 4:
                                c1_nm(t + 1)
                        if dbg and j == 0:
                            dbgs['OdT'] = nc.dram_tensor("dbg_OdT", [128, 8 * 512], BF16, kind="ExternalOutput")
                            kb.dma('sp', dbgs['OdT'].ap(), OdT[:].rearrange("p a b -> p (a b)"), OdT, load=False)
                        kb.barrier()
                OmT = sb(tl, "c_OmT", [128, 8, 512], BF16)
                x1 = sb(tl, "c_x1", [128, 4, 1024], F32)
                s23 = ExitStack()
                c3wbufs = Rot([sb(s23, "c3_w%d" % i, [128, 8, 1024], BF16) for i in range(2)])
                c3bgs = Rot([sb(s23, "c3_bg%d" % i, [1, 512], BF16) for i in range(2)])
                def c3_load(br, nt):
                    wbuf = c3wbufs.next(); bg = c3bgs.next()
                    kb.dma('pool', wbuf[:, :, 0:512], wchunks(w_in, OFF['gates'] + br * 1024 + nt * 512, 512), wbuf)
                    kb.dma('pool', wbuf[:, :, 512:1024], wchunks(w_o[br], nt * 512, 512), wbuf)
                    kb.dma('pool', bg[:], vecs["b_gate"].ap()[:, br * 1024 + nt * 512:br * 1024 + (nt + 1) * 512], bg)
                    return wbuf, bg

                def c3_load_out():
                    wbuf = c3wbufs.next()
                    kb.dma('pool', wbuf[:, :, 0:512], wchunks(w_out, 0, 512), wbuf)
                    kb.dma('pool', wbuf[:, :, 512:1024], wchunks(w_out, 512, 512), wbuf)
                    return wbuf

                c3_first = c3_load(0, 0)
                with ExitStack() as ph:
                    mqg4 = sb(ph, "mqg4", [128, 1024], F32)
                    wbufs = Rot([sb(ph, "c2_w%d" % i, [128, 8, 512], BF16) for i in range(2)])
                    mqf = sb(ph, "c2_mqf", [128, 4, 1024], F32)
                    mqb = sb(ph, "c2_mqb", [128, 1024], BF16)
                    MQT = sb(ph, "c2_MQT", [128, 8, 512], BF16)
                    junk = JunkRot([sb(ph, "c2_junk%d" % i, [128, 256], BF16) for i in range(3)])
                    st = sb(ph, "c2_st", [128, 8], F32)
                    Pms = Rot([sb(ph, "c2_P%d" % i, [128, 512], BF16) for i in range(2)])
                    rec = sb(ph, "c2_rec", [128, 512], F32)
                    PJs = Rot([ps(ph, "c2_PJ%d" % i, [128, 512]) for i in range(2)])
                    PT = ps(ph, "c2_PT", [128, 8, 128], BF16)
                    PSs = Rot([ps(ph, "c2_PS%d" % i, [128, 512]) for i in range(2)])
                    PO2 = [ps(ph, "c2_PO%d" % i, [128, 512]) for i in range(2)]
                    PM2 = ps(ph, "c2_PM", [128, 512])
                    load_gain(mqg4, "mem_q_norm", 256, rep=4, scale=256.0 ** -0.5)
                    for nt in range(2):
                        wb = wbufs.next()
                        kb.dma('pool', wb[:], wchunks(w_in, OFF['mq'] + nt * 512, 512), wb)
                        for s in range(4):
                            pj = PJs.next()
                            proj(lambda c, s=s: hTo[:, c, s * 128:(s + 1) * 128], hTo, wb, 0, 512, pj)
                            kb.op('act', lambda pj=pj, s=s, nt=nt: A_.copy(out=mqf[:, s, nt * 512:(nt + 1) * 512], in_=pj[:]),
                                  r=[pj], w=[mqf])
                    mqbs = [mqb, mqb, mqb, mqb]
                    sts4 = [st] + [sb(ph, "c2_st%d" % i, [128, 8], F32) for i in range(1, 4)]

                    def c2_qnorm(s):
                        st_ = sts4[s]; mqb_ = mqbs[s]
                        for h in range(4):
                            kb.op('act', lambda h=h: A_.activation(out=junk[:], in_=mqf[:, s, h * 256:(h + 1) * 256],
                                                                   func=AF.Square, accum_out=st_[:, h:h + 1]),
                                  r=[mqf], w=[junk, st_])
                        yield
                        rstd_from_ss(st_[:, 0:4], 4, 256.0, st_)
                        yield
                        for h in range(4):
                            kb.op('dve', lambda h=h: V_.scalar_tensor_tensor(
                                out=mqb_[:, h * 256:(h + 1) * 256], in0=mqf[:, s, h * 256:(h + 1) * 256], scalar=st_[:, h:h + 1],
                                in1=mqg4[:, h * 256:(h + 1) * 256], op0=ALU.mult, op1=ALU.mult), r=[mqf, st_, mqg4], w=[mqb_])
                        yield
                        for c8 in range(8):
                            kb.op('pe', lambda c8=c8: P_.transpose(out=PT[:, c8, :], in_=mqb_[:, c8 * 128:(c8 + 1) * 128],
                                                                   identity=ident[:]), r=[mqb_, ident], w=[PT])
                        kb.op('act', lambda: A_.copy(out=MQT[:, :, s * 128:(s + 1) * 128], in_=PT[:]), r=[PT], w=[MQT])

                    pipeline([c2_qnorm(s) for s in range(4)], depth=4, lag=1)
                    for h in range(4):
                        pend = None
                        for mc in range(2):
                            pss = PSs.next()
                            for c in range(2):
                                kb.op('pe', lambda pss=pss, h=h, mc=mc, c=c: P_.matmul(
                                    pss[:], lhsT=memKT[:, h * 2 + c, mc * 128:(mc + 1) * 128], rhs=MQT[:, h * 2 + c, :],
                                    start=(c == 0), stop=(c == 1)), r=[memKT, MQT], w=[pss])
                            Pm = Pms.next()
                            kb.op('act', lambda pss=pss, Pm=Pm: A_.activation(out=Pm[:], in_=pss[:], func=AF.Exp),
                                  r=[pss], w=[Pm])
                            if pend is not None:
                                pend()
                            def pvm(Pm=Pm, h=h, mc=mc):
                                for vc in range(2):
                                    kb.op('pe', lambda vc=vc: P_.matmul(
                                        PO2[vc][:], lhsT=memV[:, mc, h * 256 + vc * 128:h * 256 + (vc + 1) * 128], rhs=Pm[:],
                                        start=(mc == 0), stop=(mc == 1)), r=[memV, Pm], w=[PO2[vc]])
                                kb.op('pe', lambda: P_.matmul(PM2[:], lhsT=ones[:], rhs=Pm[:], start=(mc == 0),
                                                              stop=(mc == 1)), r=[ones, Pm], w=[PM2])
                            pend = pvm
                        pend()
                        kb.op('dve', lambda: V_.reciprocal(out=rec[:], in_=PM2[:]), r=[PM2], w=[rec])
                        for vc in range(2):
                            kb.op('dve', lambda h=h, vc=vc: V_.tensor_tensor(out=OmT[:, h * 2 + vc, :], in0=PO2[vc][:], in1=rec[:],
                                                                             op=ALU.mult), r=[PO2[vc], rec], w=[OmT])
                    kb.barrier()
                with ExitStack() as ph:
                    gt = sb(ph, "c3_gt", [128, 512], F32)
                    tmp = sb(ph, "c3_tmp", [128, 512], F32)
                    mbf = sb(ph, "c3_mbf", [128, 1024], BF16)
                    mT = sb(ph, "c3_mT", [128, 8, 128], BF16)
                    xo = sb(ph, "c3_xo", [128, 1024], F32)
                    ogs = [sb(ph, "c3_og%d" % i, [128, 8, 128], BF16) for i in range(4)]
                    for s in range(4):
                        kb.dma('sp', ogs[s][:].rearrange("p a b -> p (a b)"), ogla_scr[4 * j + s], ogs[s])
                    PGs = Rot([ps(ph, "c3_PG%d" % i, [128, 512]) for i in range(2)])
                    PPs = Rot([ps(ph, "c3_PP%d" % i, [128, 512]) for i in range(2)])
                    PT = ps(ph, "c3_PT", [128, 8, 128], BF16)
                    chunks = [(br, nt) for br in range(3) for nt in range(2)]
                    nxt = c3_first
                    for ci, (br, nt) in enumerate(chunks):
                        if True:
                            wbuf, bg = nxt
                            nxt = c3_load(*chunks[ci + 1]) if ci + 1 < len(chunks) else (c3_load_out(), None)
                            for s in range(4):
                                tok = slice(s * 128, (s + 1) * 128)
                                pg = PGs.next()
                                for c in range(8):
                                    kb.op('pe', lambda pg=pg, c=c, tok=tok: P_.matmul(pg[:], lhsT=hTo[:, c, tok], rhs=wbuf[:, c, 0:512],
                                                                                      start=(c == 0), stop=False), r=[hTo, wbuf], w=[pg])
                                kb.op('pe', lambda pg=pg: P_.matmul(pg[:], lhsT=ones[0:1, :], rhs=bg[:], start=False, stop=True),
                                      r=[ones, bg], w=[pg])
                                kb.op('act', lambda pg=pg: A_.activation(out=gt[:], in_=pg[:], func=AF.Sigmoid), r=[pg], w=[gt])
                                pp = PPs.next()
                                for c in range(8):
                                    if br == 0:
                                        lh = ogs[s][:, c, :]
                                        lt = ogs[s]
                                    elif br == 1:
                                        lh = OdT[:, c, tok]
                                        lt = OdT
                                    else:
                                        lh = OmT[:, c, tok]
                                        lt = OmT
                                    kb.op('pe', lambda pp=pp, c=c, lh=lh: P_.matmul(pp[:], lhsT=lh, rhs=wbuf[:, c, 512:1024],
                                                                                    start=(c == 0), stop=(c == 7)), r=[lt, wbuf], w=[pp])
                                dst = x1[:, s, nt * 512:(nt + 1) * 512]
                                if br == 0:
                                    kb.op('dve', lambda pp=pp, dst=dst: V_.tensor_tensor(out=dst, in0=gt[:], in1=pp[:], op=ALU.mult),
                                          r=[gt, pp], w=[x1])
                                else:
                                    kb.op('dve', lambda pp=pp: V_.tensor_tensor(out=tmp[:], in0=gt[:], in1=pp[:], op=ALU.mult),
                                          r=[gt, pp], w=[tmp])
                                    kb.op('dve', lambda dst=dst: V_.tensor_tensor(out=dst, in0=dst, in1=tmp[:], op=ALU.add),
                                          r=[tmp, x1], w=[x1])
                    wbuf = nxt[0]
                    mbfs = [mbf, sb(ph, "c3_mbf2", [128, 1024], BF16)]
                    mTs = [mT, sb(ph, "c3_mT2", [128, 8, 128], BF16)]

                    def c3_prep(s):
                        mb_, mt_ = mbfs[s % 2], mTs[s % 2]
                        kb.op('act', lambda: A_.copy(out=mb_[:], in_=x1[:, s, :]), r=[x1], w=[mb_])
                        for c in range(8):
                            kb.op('pe', lambda c=c: P_.transpose(out=PT[:, c, :], in_=mb_[:, c * 128:(c + 1) * 128],
                                                                 identity=ident[:]), r=[mb_, ident], w=[PT])
                        kb.op('act', lambda: A_.copy(out=mt_[:], in_=PT[:]), r=[PT], w=[mt_])

                    c3_prep(0)
                    for s in range(4):
                        m = 4 * j + s
                        if s + 1 < 4:
                            c3_prep(s + 1)
                        mt_ = mTs[s % 2]
                        kb.dma('sp', xo[:], x_own[m * 128:(m + 1) * 128, :], xo)
                        for nt in range(2):
                            pp = PPs.next()
                            for c in range(8):
                                kb.op('pe', lambda pp=pp, c=c, nt=nt: P_.matmul(pp[:], lhsT=mt_[:, c, :], rhs=wbuf[:, c, nt * 512:(nt + 1) * 512],
                                                                                start=(c == 0), stop=(c == 7)), r=[mt_, wbuf], w=[pp])
                            kb.op('dve', lambda pp=pp, s=s, nt=nt: V_.tensor_tensor(
                                out=x1[:, s, nt * 512:(nt + 1) * 512], in0=xo[:, nt * 512:(nt + 1) * 512], in1=pp[:], op=ALU.add),
                                r=[xo, pp], w=[x1])
                    if dbg and j == 0:
                        dbgs['x1'] = nc.dram_tensor("dbg_x1", [128, 4096], F32, kind="ExternalOutput")
                        kb.dma('sp', dbgs['x1'].ap(), x1[:].rearrange("p a b -> p (a b)"), x1, load=False)
                    kb.barrier()
                s23.close()
                with ExitStack() as ph:
                    g_ffn = sb(ph, "g_ffnt", [128, 1024], F32)
                    xnT = sb(ph, "c4_xnT", [128, 8, 512], BF16)
                    junk = JunkRot([sb(ph, "c4_junk%d" % i, [128, 1024], BF16) for i in range(3)])
                    hbs4 = [sb(ph, "c4_hb%d" % i, [128, 1024], BF16) for i in range(4)]
                    sts4 = [sb(ph, "c4_st%d" % i, [128, 8], F32) for i in range(4)]
                    wr = sb(ph, "c4_wr", [128, 8, 20], BF16)
                    brow = sb(ph, "c4_brow", [1, 20], BF16)
                    lgs4 = [sb(ph, "c4_lg%d" % i, [128, 20], F32) for i in range(4)]
                    rts4 = [sb(ph, "c4_rt%d" % i, [128, 64], F32) for i in range(4)]
                    comb = sb(ph, "c4_comb", [128, 4, 16], F32)
                    wgs = Rot([sb(ph, "c4_wg%d" % i, [128, 8, 256], BF16) for i in range(3)])
                    wus = Rot([sb(ph, "c4_wu%d" % i, [128, 8, 256], BF16) for i in range(3)])
                    wds = Rot([sb(ph, "c4_wd%d" % i, [128, 2, 1024], BF16) for i in range(3)])
                    sgs = Rot([sb(ph, "c4_sg%d" % i, [128, 512], F32) for i in range(2)])
                    hid = [sb(ph, "c4_hid%d" % i, [128, 512], BF16) for i in range(2)]
                    PGs = Rot([ps(ph, "c4_PG%d" % i, [128, 512]) for i in range(2)])
                    PUs = Rot([ps(ph, "c4_PU%d" % i, [128, 512]) for i in range(2)])
                    PDs = Rot([ps(ph, "c4_PD%d" % i, [128, 512]) for i in range(2)])
                    PT = ps(ph, "c4_PT", [128, 8, 128], BF16)
                    PR = ps(ph, "c4_PR", [128, 512])
                    load_gain(g_ffn, "g_ffn", 1024)
                    kb.dma('pool', wr[:, :, 0:4], wchunks(w_r1, 0, 4), wr)
                    kb.dma('pool', wr[:, :, 4:20], wchunks(w_r2, 0, 16), wr)
                    kb.dma('pool', brow[:, 0:4], vecs["b_r1"].ap(), brow)
                    kb.dma('pool', brow[:, 4:20], vecs["b_r2"].ap(), brow)
                    BIG = 1.0e4

                    def c4_head(s):
                        hb = hbs4[s]; st = sts4[s]; lg = lgs4[s]; rt = rts4[s]
                        PRs = PR[:, 32 * s:32 * s + 20]
                        tok = slice(s * 128, (s + 1) * 128)
                        kb.op('act', lambda s=s: A_.activation(out=junk[:], in_=x1[:, s, :], func=AF.Square, accum_out=st[:, 0:1]),
                              r=[x1], w=[junk, st])
                        rstd_from_ss(st[:, 0:1], 1, 1024.0, st)
                        yield
                        kb.op('dve', lambda s=s: V_.scalar_tensor_tensor(out=hb[:], in0=x1[:, s, :], scalar=st[:, 0:1], in1=g_ffn[:],
                                                                         op0=ALU.mult, op1=ALU.mult), r=[x1, st, g_ffn], w=[hb])
                        yield
                        for c in range(8):
                            kb.op('pe', lambda c=c: P_.transpose(out=PT[:, c, :], in_=hb[:, c * 128:(c + 1) * 128],
                                                                 identity=ident[:]), r=[hb, ident], w=[PT])
                        kb.op('act', lambda tok=tok: A_.copy(out=xnT[:, :, tok], in_=PT[:]), r=[PT], w=[xnT])
                        yield
                        for c in range(8):
                            kb.op('pe', lambda c=c, tok=tok: P_.matmul(PRs, lhsT=xnT[:, c, tok], rhs=wr[:, c, :],
                                                                       start=(c == 0), stop=False), r=[xnT, wr], w=[PR])
                        kb.op('pe', lambda: P_.matmul(PRs, lhsT=ones[0:1, :], rhs=brow[:], start=False, stop=True),
                              r=[ones, brow], w=[PR])
                        kb.op('dve', lambda: V_.tensor_copy(out=lg[:], in_=PRs), r=[PR], w=[lg])
                        yield
                        dv = lambda fn, r=(), w=(): kb.op('dve', fn, r=[lg, rt] + list(r), w=[rt] + list(w))
                        dv(lambda: V_.tensor_reduce(out=rt[:, 0:1], in_=lg[:, 0:4], axis=AX.X, op=ALU.max))
                        yield
                        dv(lambda: V_.tensor_scalar(out=rt[:, 1:2], in0=rt[:, 0:1], scalar1=-1.0, scalar2=None, op0=ALU.mult))
                        yield
                        kb.op('act', lambda: A_.activation(out=rt[:, 48:52], in_=lg[:, 0:4], func=AF.Exp, bias=rt[:, 1:2],
                                                           accum_out=rt[:, 2:3]), r=[lg, rt], w=[rt])
                        yield
                        dv(lambda: V_.reciprocal(out=rt[:, 3:4], in_=rt[:, 2:3]))
                        yield
                        dv(lambda: V_.tensor_scalar(out=rt[:, 4:8], in0=lg[:, 0:4], scalar1=rt[:, 0:1], scalar2=None, op0=ALU.is_ge))
                        yield
                        dv(lambda: V_.tensor_scalar(out=rt[:, 8:12], in0=rt[:, 4:8], scalar1=BIG, scalar2=-BIG, op0=ALU.mult,
                                                    op1=ALU.add))
                        yield
                        for g in range(4):
                            dv(lambda g=g: V_.tensor_scalar(out=rt[:, 12 + 4 * g:16 + 4 * g], in0=lg[:, 4 + 4 * g:8 + 4 * g],
                                                            scalar1=rt[:, 8 + g:9 + g], scalar2=None, op0=ALU.add))
                            yield
                        dv(lambda: V_.tensor_reduce(out=rt[:, 28:29], in_=rt[:, 12:28], axis=AX.X, op=ALU.max))
                        yield
                        dv(lambda: V_.tensor_scalar(out=rt[:, 29:30], in0=rt[:, 28:29], scalar1=-1.0, scalar2=None, op0=ALU.mult))
                        yield
                        dv(lambda: V_.tensor_scalar(out=rt[:, 30:46], in0=rt[:, 12:28], scalar1=rt[:, 28:29], scalar2=-BIG,
                                                    op0=ALU.is_ge, op1=ALU.mult))
                        yield
                        dv(lambda: V_.tensor_tensor(out=rt[:, 30:46], in0=rt[:, 30:46], in1=rt[:, 12:28], op=ALU.add))
                        yield
                        dv(lambda: V_.tensor_reduce(out=rt[:, 46:47], in_=rt[:, 30:46], axis=AX.X, op=ALU.max))
                        yield
                        kb.op('act', lambda: A_.activation(out=rt[:, 48:64], in_=rt[:, 12:28], func=AF.Exp, bias=rt[:, 29:30]),
                              r=[rt], w=[rt])
                        yield
                        dv(lambda: V_.scalar_tensor_tensor(out=rt[:, 48:64], in0=rt[:, 12:28], scalar=rt[:, 46:47], in1=rt[:, 48:64],
                                                           op0=ALU.is_ge, op1=ALU.mult))
                        yield
                        dv(lambda: V_.tensor_reduce(out=rt[:, 47:48], in_=rt[:, 48:64], axis=AX.X, op=ALU.add))
                        yield
                        dv(lambda: V_.reciprocal(out=rt[:, 47:48], in_=rt[:, 47:48]))
                        yield
                        dv(lambda: V_.tensor_tensor(out=rt[:, 47:48], in0=rt[:, 47:48], in1=rt[:, 3:4], op=ALU.mult))
                        yield
                        dv(lambda s=s: V_.tensor_scalar(out=comb[:, s, :], in0=rt[:, 48:64], scalar1=rt[:, 47:48], scalar2=None,
                                                        op0=ALU.mult), w=[comb])
                        yield
                    pipeline([c4_head(s) for s in range(4)], depth=4, lag=0)
                    hidA = [hid, [sb(ph, "c4_hidB%d" % i, [128, 512], BF16) for i in range(2)]]
                    wsets = {}

                    def c4_load(e):
                        wg_ = wgs.next(); wu_ = wus.next(); wd_ = wds.next()
                        kb.dma('pool', wg_[:], w_gate[e].rearrange("(c p) n -> p c n", p=128), wg_)
                        kb.dma('pool', wu_[:], w_up[e].rearrange("(c p) n -> p c n", p=128), wu_)
                        kb.dma('pool', wd_[:], w_down[e].rearrange("(c p) n -> p c n", p=128), wd_)
                        wsets[e] = (wg_, wu_, wd_)

                    def c4_gu(e, fc):
                        wg_, wu_, _ = wsets[e]
                        hd = hidA[e % 2][fc]
                        pg = PGs.next(); pu = PUs.next()
                        for c in range(8):
                            kb.op('pe', lambda c=c: P_.matmul(
                                pg[:], lhsT=wg_[:, c, fc * 128:(fc + 1) * 128], rhs=xnT[:, c, :], start=(c == 0), stop=(c == 7)),
                                r=[wg_, xnT], w=[pg])
                        for c in range(8):
                            kb.op('pe', lambda c=c: P_.matmul(
                                pu[:], lhsT=wu_[:, c, fc * 128:(fc + 1) * 128], rhs=xnT[:, c, :], start=(c == 0), stop=(c == 7)),
                                r=[wu_, xnT], w=[pu])
                        sg_ = sgs.next()
                        kb.op('act', lambda: A_.activation(out=sg_[:], in_=pg[:], func=AF.Silu), r=[pg], w=[sg_])
                        kb.op('dve', lambda: V_.tensor_tensor(out=hd[:], in0=sg_[:], in1=pu[:], op=ALU.mult),
                              r=[sg_, pu], w=[hd])

                    def c4_down(e, slots):
                        _, _, wd_ = wsets[e]
                        for s in slots:
                            tok = slice(s * 128, (s + 1) * 128)
                            for nt in range(2):
                                pd = PDs.next()
                                for fc in range(2):
                                    hd = hidA[e % 2][fc]
                                    kb.op('pe', lambda fc=fc, hd=hd: P_.matmul(
                                        pd[:], lhsT=hd[:, tok], rhs=wd_[:, fc, nt * 512:(nt + 1) * 512], start=(fc == 0), stop=(fc == 1)),
                                        r=[hd, wd_], w=[pd])
                                kb.op('dve', lambda s=s, nt=nt: V_.scalar_tensor_tensor(
                                    out=x1[:, s, nt * 512:(nt + 1) * 512], in0=pd[:], scalar=comb[:, s, e:e + 1],
                                    in1=x1[:, s, nt * 512:(nt + 1) * 512], op0=ALU.mult, op1=ALU.add), r=[pd, comb, x1], w=[x1])

                    c4_load(0)
                    c4_load(1)
                    c4_gu(0, 0)
                    c4_gu(0, 1)
                    for e in range(16):
                        if e + 2 < 16:
                            c4_load(e + 2)
                        if e + 1 < 16:
                            c4_gu(e + 1, 0)
                        c4_down(e, [0, 1])
                        if e + 1 < 16:
                            c4_gu(e + 1, 1)
                        c4_down(e, [2, 3])
                    for s in range(4):
                        m = 4 * j + s
                        kb.dma('sp', out_own[m * 128:(m + 1) * 128, :], x1[:, s, :], x1, load=False)
                    kb.barrier()
    return nc, kb, dbgs


def _consts():
    bf = ml_dtypes.bfloat16
    c = {}
    c["c_ident"] = np.eye(128, dtype=np.float32).astype(bf)
    c["c_i4"] = np.tile(np.eye(128, dtype=np.float32), (1, 4)).astype(bf)
    c["c_ones"] = np.ones((128, 128), np.float32).astype(bf)
    j = np.arange(128)[:, None]
    i = np.arange(128)[None, :]
    c["c_lmt"] = np.where(j <= i, -1.0 / 16.0, 0.0).astype(np.float32)
    c["c_umt"] = np.where(j > i, -1.0 / 16.0, 0.0).astype(np.float32)
    c["c_caus4"] = np.tile(np.where(j <= i, 1.0, 0.0), (1, 4)).astype(np.float32)
    sel = np.zeros((16, 16, 128), np.float32)
    for e in range(16):
        sel[e, e, :] = 1.0
    c["c_sel16"] = sel.reshape(16, 2048).astype(bf)
    c["c_bis"] = np.tile((2.0 ** (1.0 - np.arange(1, NBIS + 1)))[None, :], (128, 1)).astype(np.float32)
    return c


_CACHE = {}


def kernel(x, mem, g_mix, g_mem, w_in, w_gla_a2, b_gla_a2, gla_norm, w_mem_kv,
           dsa_q_norm, dsa_k_norm, idx_k_norm, mem_q_norm, mem_k_norm, b_gate,
           w_o_gla, w_o_dsa, w_o_mem, w_out, g_ffn, w_r1, b_r1, w_r2, b_r2,
           w_gate, w_up, w_down, _dbg=False):
    f = lambda a: np.ascontiguousarray(np.asarray(a, dtype=np.float32))
    x = f(x)
    B, TT, _ = x.shape
    key = (TT, _dbg)
    if key not in _CACHE:
        _CACHE[key] = build_nc(TT, dbg=_dbg)
    nc, kb, dbgs = _CACHE[key]
    NB = TT // 128
    consts = _consts()
    shared = {
        "w_in": f(w_in)[0], "w_gla_a2": f(w_gla_a2)[0], "w_mem_kv": f(w_mem_kv)[0],
        "w_o_gla": f(w_o_gla)[0], "w_o_dsa": f(w_o_dsa)[0], "w_o_mem": f(w_o_mem)[0], "w_out": f(w_out)[0],
        "w_r1": f(w_r1)[0], "w_r2": f(w_r2)[0], "w_gate": f(w_gate)[0], "w_up": f(w_up)[0], "w_down": f(w_down)[0],
        "g_mix": f(g_mix), "g_mem": f(g_mem), "g_ffn": f(g_ffn), "gla_norm": f(gla_norm), "dsa_q_norm": f(dsa_q_norm),
        "dsa_k_norm": f(dsa_k_norm), "idx_k_norm": f(idx_k_norm), "mem_q_norm": f(mem_q_norm), "mem_k_norm": f(mem_k_norm),
        "b_gla_a2": f(b_gla_a2), "b_gate": f(b_gate), "b_r1": f(b_r1), "b_r2": f(b_r2),
    }
    shared.update(consts)
    memf = f(mem)
    in_maps = []
    tpos = np.arange(128)[:, None]
    spos = np.arange(128)[None, :]
    for c in range(8):
        b, r = c // 4, c % 4
        xb = x[b].reshape(NB, 128, D)
        own = np.ascontiguousarray(xb[r::4].reshape(-1, D))
        cm = np.zeros((128, 512), np.float32)
        for p in range(4):
            if p == r:
                cm[:, p * 128:(p + 1) * 128] = np.where(spos <= tpos, 0.0, -1e30)
            elif p > r:
                cm[:, p * 128:(p + 1) * 128] = -1e30
        ws = np.zeros((128, 4), np.float32)
        ws[:, r] = 1.0
        d = dict(shared)
        d.update({"x_full": x[b], "x_own": own, "mem": memf[b], "cmask": cm, "wsel": ws})
        in_maps.append(d)
    res = run_bass_kernel_spmd(nc, in_maps, core_ids=list(range(8)))
    out = np.zeros((B, NB, 128, D), np.float32)
    for c in range(8):
        b, r = c // 4, c % 4
        out[b, r::4] = res.results[c]["out_own"].reshape(NB // 4, 128, D)
    if _dbg:
        kernel.last = res
    return out.reshape(B, TT, D)
```
